# Optimizing a Trainium2 kernel written in Bass

```python
import math
import jax, jax.numpy as jnp
from jax import lax
import numpy as np

D_MODEL = 1024
BATCH = 4
SEQ = 4096
DEPTH = 1

MIX_WIDTH = D_MODEL
POOL_WIDTH = MIX_WIDTH // 2
POOL_WINDOWS = (2, 4, 8, 16)
N_POOL_GROUPS = len(POOL_WINDOWS)
POOL_GROUP_DIM = POOL_WIDTH // N_POOL_GROUPS
ATTN_WIDTH = MIX_WIDTH - POOL_WIDTH
HEAD_DIM = 64
N_ATTN_HEADS = ATTN_WIDTH // HEAD_DIM
DILATED_BRANCHES = ((128, 1), (512, 4), (2048, 16))
ATTN_BLOCK = 128
IN_WIDTH = POOL_WIDTH + 3 * ATTN_WIDTH
N_REL_BUCKETS = 32
REL_MAX_EXACT = N_REL_BUCKETS // 2
REL_MAX_DISTANCE = 2048
N_EXPERTS = 32
TOP_K = 4
D_EXPERT = D_MODEL
SWIGLU_LIMIT = 7.0
SWIGLU_ALPHA = 1.702
MOE_BLOCK = 128
PLE_DIM = 256
NORM_EPS = 1e-6
NEG_INF = -1e30

kernel_name = "hybrid_pool_dilated_attn_moe_ple"


def rmsnorm(x, g):
    xf = x.astype(jnp.float32)
    y = xf * lax.rsqrt(jnp.mean(xf * xf, axis=-1, keepdims=True) + NORM_EPS)
    return (y * g.astype(jnp.float32)).astype(x.dtype)


def pool_mixer(u, w_pool, pool_scale):
    B, S, _ = u.shape
    ug = u.reshape(B, S, N_POOL_GROUPS, POOL_GROUP_DIM)
    t = jnp.arange(S)
    outs = []
    for gi, w in enumerate(POOL_WINDOWS):
        ch = ug[:, :, gi].astype(jnp.float32)
        cs = jnp.cumsum(ch, axis=1)
        prev = jnp.pad(cs, ((0, 0), (w, 0), (0, 0)))[:, :S]
        cnt = jnp.minimum(t + 1, w).astype(jnp.float32)[None, :, None]
        outs.append(((cs - prev) / cnt - ch).astype(u.dtype))
    pooled = jnp.stack(outs, axis=2)
    mixed = jnp.einsum('bsgc,gcd->bsgd', pooled, w_pool)
    return mixed.reshape(B, S, POOL_WIDTH) * pool_scale


def t5_bucket(dist):
    n = jnp.maximum(dist, 1).astype(jnp.float32)
    large = REL_MAX_EXACT + (jnp.log(n / REL_MAX_EXACT) / math.log(REL_MAX_DISTANCE / REL_MAX_EXACT)
                             * (N_REL_BUCKETS - REL_MAX_EXACT)).astype(jnp.int32)
    large = jnp.minimum(large, N_REL_BUCKETS - 1)
    return jnp.where(dist < REL_MAX_EXACT, dist, large)


def dilated_branch(q, k, v, rel_bias, window, dil):
    B, S, H, Dh = q.shape
    w_sub = window // dil
    blk = ATTN_BLOCK
    span = dil * blk
    Sp = -(-S // span) * span
    L = Sp // dil
    nb = L // blk

    def to_sub(t):
        t = jnp.pad(t, ((0, 0), (0, Sp - S), (0, 0), (0, 0)))
        t = t.reshape(B, nb, blk, dil, H, Dh)
        return t.transpose(0, 3, 4, 1, 2, 5)

    qs, ks, vs = to_sub(q), to_sub(k), to_sub(v)
    shift = lambda t: jnp.concatenate([jnp.zeros_like(t[:, :, :, :1]), t[:, :, :, :-1]], axis=3)
    keys = jnp.concatenate([shift(ks), ks], axis=4)
    vals = jnp.concatenate([shift(vs), vs], axis=4)

    s = jnp.einsum('brhnqc,brhnkc->brhnqk', qs, keys).astype(jnp.float32) * (1.0 / math.sqrt(Dh))
    qi = jnp.arange(blk)[:, None]
    ki = jnp.arange(2 * blk)[None, :]
    rel = blk + qi - ki
    bias = rel_bias[t5_bucket(jnp.maximum(rel, 0) * dil)]
    s = s + bias.transpose(2, 0, 1)[:, None].astype(jnp.float32)
    band = (rel >= 0) & (rel <= w_sub)
    first_ok = (jnp.arange(nb)[:, None, None] > 0) | (ki[None] >= blk)
    mask = band[None] & first_ok
    s = jnp.where(mask, s, NEG_INF)
    m = jnp.max(s, axis=-1)
    e = jnp.exp(s - m[..., None])
    l = jnp.sum(e, axis=-1)
    o = jnp.einsum('brhnqk,brhnkc->brhnqc', e, vals.astype(jnp.float32)) / l[..., None]
    lse = m + jnp.log(l)
    o = o.transpose(0, 3, 4, 1, 2, 5).reshape(B, Sp, H, Dh)[:, :S]
    lse = lse.transpose(0, 3, 4, 1, 2).reshape(B, Sp, H)[:, :S]
    return o, lse


def dilated_attention(q, k, v, rel_bias):
    outs, lses = [], []
    for window, dil in DILATED_BRANCHES:
        o, lse = dilated_branch(q, k, v, rel_bias, window, dil)
        outs.append(o)
        lses.append(lse)
    w = jax.nn.softmax(jnp.stack(lses, axis=0), axis=0)
    out = jnp.sum(w[..., None] * jnp.stack(outs, axis=0), axis=0)
    return out.astype(q.dtype)


def moe(h, w_router, b_router, w_gate_up, b_gate_up, w_down, b_down):
    B, S, D = h.shape
    T = B * S
    hf = h.reshape(T, D)
    logits = (hf @ w_router + b_router).astype(jnp.float32)
    topv, topi = lax.top_k(logits, TOP_K)
    gates = jax.nn.softmax(topv, axis=-1)

    expert_ids = topi.reshape(-1)
    token_ids = jnp.repeat(jnp.arange(T), TOP_K)
    gate_flat = gates.reshape(-1)
    order = jnp.argsort(expert_ids)
    sorted_e = expert_ids[order]
    group_sizes = jnp.bincount(expert_ids, length=N_EXPERTS)
    group_starts = jnp.cumsum(group_sizes) - group_sizes
    padded_sizes = (group_sizes + MOE_BLOCK - 1) // MOE_BLOCK * MOE_BLOCK
    padded_ends = jnp.cumsum(padded_sizes)
    padded_starts = padded_ends - padded_sizes
    rank = jnp.arange(T * TOP_K) - group_starts[sorted_e]
    dest = padded_starts[sorted_e] + rank

    n_rows = T * TOP_K + N_EXPERTS * MOE_BLOCK
    n_blocks = n_rows // MOE_BLOCK
    row_token = jnp.zeros((n_rows,), jnp.int32).at[dest].set(token_ids[order])
    row_gate = jnp.zeros((n_rows,), h.dtype).at[dest].set(gate_flat[order].astype(h.dtype))
    block_expert = jnp.minimum(
        jnp.searchsorted(padded_ends, jnp.arange(n_blocks) * MOE_BLOCK, side='right'),
        N_EXPERTS - 1)
    xs = hf[row_token].reshape(n_blocks, MOE_BLOCK, D)

    def expert_block(args):
        xb, e = args
        gu = xb @ w_gate_up[e] + b_gate_up[e]
        g, u = gu[:, :D_EXPERT], gu[:, D_EXPERT:]
        g = jnp.minimum(g, SWIGLU_LIMIT)
        u = jnp.clip(u, -SWIGLU_LIMIT, SWIGLU_LIMIT)
        glu = g * jax.nn.sigmoid(SWIGLU_ALPHA * g)
        return ((u + 1.0) * glu) @ w_down[e] + b_down[e]

    ys = lax.map(expert_block, (xs, block_expert)).reshape(n_rows, D)
    out = jnp.zeros((T, D), h.dtype).at[row_token].add(ys * row_gate[:, None])
    return out.reshape(B, S, D)


def setup_inputs(seed: int = 0) -> dict:
    key = jax.random.key(seed)
    ks = jax.random.split(key, 24)
    f32 = jnp.float32
    nrm = lambda k, shape, s: jax.random.normal(k, shape, f32) * s
    gain = lambda k, shape: 1.0 + 0.05 * jax.random.normal(k, shape, f32)
    return {
        "x": nrm(ks[0], (BATCH, SEQ, D_MODEL), 1.0),
        "p": nrm(ks[1], (DEPTH, BATCH, SEQ, PLE_DIM), 1.0),
        "g_mix": gain(ks[2], (DEPTH, D_MODEL)),
        "w_in": nrm(ks[3], (DEPTH, D_MODEL, IN_WIDTH), D_MODEL ** -0.5),
        "w_pool": nrm(ks[4], (DEPTH, N_POOL_GROUPS, POOL_GROUP_DIM, POOL_GROUP_DIM), POOL_GROUP_DIM ** -0.5),
        "pool_scale": gain(ks[5], (DEPTH, POOL_WIDTH)),
        "rel_bias": nrm(ks[6], (N_REL_BUCKETS, N_ATTN_HEADS), 0.5),
        "w_out": nrm(ks[7], (DEPTH, MIX_WIDTH, D_MODEL), MIX_WIDTH ** -0.5),
        "g_ffn": gain(ks[8], (DEPTH, D_MODEL)),
        "w_router": nrm(ks[9], (DEPTH, D_MODEL, N_EXPERTS), D_MODEL ** -0.5),
        "b_router": nrm(ks[10], (DEPTH, N_EXPERTS), 0.01),
        "w_gate_up": nrm(ks[11], (DEPTH, N_EXPERTS, D_MODEL, 2 * D_EXPERT), D_MODEL ** -0.5),
        "b_gate_up": nrm(ks[12], (DEPTH, N_EXPERTS, 2 * D_EXPERT), 0.01),
        "w_down": nrm(ks[13], (DEPTH, N_EXPERTS, D_EXPERT, D_MODEL), D_EXPERT ** -0.5),
        "b_down": nrm(ks[14], (DEPTH, N_EXPERTS, D_MODEL), 0.01),
        "g_ple": gain(ks[15], (DEPTH, D_MODEL)),
        "w_ple_gate": nrm(ks[16], (DEPTH, D_MODEL, D_MODEL), D_MODEL ** -0.5),
        "w_ple_proj": nrm(ks[17], (DEPTH, PLE_DIM, D_MODEL), PLE_DIM ** -0.5),
        "g_final": gain(ks[18], (D_MODEL,)),
    }


def reference(x, p, g_mix, w_in, w_pool, pool_scale, rel_bias, w_out, g_ffn,
              w_router, b_router, w_gate_up, b_gate_up, w_down, b_down,
              g_ple, w_ple_gate, w_ple_proj, g_final):
    B, S, _ = x.shape
    for i in range(DEPTH):
        h = rmsnorm(x, g_mix[i])
        z = jnp.einsum('bsd,de->bse', h, w_in[i])
        u = z[..., :POOL_WIDTH]
        qkv = z[..., POOL_WIDTH:].reshape(B, S, 3, N_ATTN_HEADS, HEAD_DIM)
        q, k, v = qkv[:, :, 0], qkv[:, :, 1], qkv[:, :, 2]
        a_out = pool_mixer(u, w_pool[i], pool_scale[i])
        b_out = dilated_attention(q, k, v, rel_bias).reshape(B, S, ATTN_WIDTH)
        mix = jnp.concatenate([a_out, b_out], axis=-1)
        x = x + jnp.einsum('bse,ed->bsd', mix, w_out[i])
        x = x + moe(rmsnorm(x, g_ffn[i]), w_router[i], b_router[i], w_gate_up[i],
                    b_gate_up[i], w_down[i], b_down[i])
        gate = jax.nn.sigmoid(jnp.einsum('bsd,de->bse', rmsnorm(x, g_ple[i]), w_ple_gate[i]))
        x = x + jnp.einsum('bsp,pd->bsd', p[i], w_ple_proj[i]) * gate
    return rmsnorm(x, g_final)
```

```python
import math
from contextlib import ExitStack

import numpy as np
import concourse.bass as bass
import concourse.mybir as mybir
from concourse.bass_utils import run_bass_kernel_spmd

F32 = mybir.dt.float32
BF16 = mybir.dt.bfloat16
AF = mybir.ActivationFunctionType
ALU = mybir.AluOpType

NCORES = 8
D = 1024
S_OWN = 2048
NT = 16
NE = 32
NEG = -1e30
CAP = 384
BIGSLOT = 1.0e6
NCONV = 9
OPT_ACT_RECIP = True
OPT_ATT_NEW = False
EPS = 1e-6
BRANCHES = ((128, 1), (512, 4), (2048, 16))


class Op:
    __slots__ = ("eng", "fn", "deps", "signal", "count", "dma_sem", "is_dma")

    def __init__(self, eng, fn, dma_sem=None):
        self.eng = eng
        self.fn = fn
        self.deps = []
        self.signal = False
        self.count = 0
        self.dma_sem = dma_sem
        self.is_dma = dma_sem is not None


class Prog:
    ENGS = ("pe", "act", "dve", "pool", "sp")

    def __init__(self, nc, stack):
        self.nc = nc
        self.stack = stack
        self.esem = {e: stack.enter_context(nc.semaphore("sem_" + e)) for e in self.ENGS}
        self.ecount = {e: 0 for e in self.ENGS}
        self.dsem = {}
        self.dcount = {}
        self.waited = {e: {} for e in self.ENGS}
        self.begin()

    def bc_reg(self, E, val):
        if self._bc is None:
            self._bc = E.to_reg(val)
        return self._bc

    def begin(self):
        self._bc = None
        self.ops = []
        self.res_w = {}
        self.res_r = {}

    def _dma_sem(self, key):
        if key not in self.dsem:
            self.dsem[key] = self.stack.enter_context(self.nc.semaphore("dsem_%d" % len(self.dsem)))
            self.dcount[key] = 0
        return key

    def op(self, eng, fn, reads=(), writes=(), dma_key=None):
        o = Op(eng, fn, self._dma_sem(dma_key) if dma_key is not None else None)
        if dma_key is not None:
            writes = list(writes) + [("__key", dma_key)]
        deps = {}
        for r in reads:
            w = self.res_w.get(r)
            if w is not None:
                deps[id(w)] = w
        for r in writes:
            w = self.res_w.get(r)
            if w is not None:
                deps[id(w)] = w
            for rd in self.res_r.get(r, ()):
                deps[id(rd)] = rd
        for d in deps.values():
            if d is o:
                continue
            if d.eng == "pe" and eng == "pe" and not d.is_dma and not o.is_dma:
                continue
            o.deps.append(d)
            d.signal = True
        for r in reads:
            lst = self.res_r.setdefault(r, [])
            if not o.is_dma:
                lst[:] = [x for x in lst if x.is_dma or x.eng != eng]
            lst.append(o)
        for r in writes:
            self.res_w[r] = o
            self.res_r[r] = []
        self.ops.append(o)
        return o

    def dma(self, eng, out, in_, reads=(), writes=(), key=None):
        assert key is not None
        return self.op(eng, lambda E: E.dma_start(out=out, in_=in_), reads, writes, dma_key=key)

    def ckey(self):
        self._ck = (getattr(self, "_ck", 0) + 1) % 6
        return ("const", self._ck)

    def emit(self, defer_cv=False):
        nc = self.nc
        last = {}
        for o in self.ops:
            if not o.is_dma:
                last[o.eng] = o
        for o in last.values():
            o.signal = True
        for o in self.ops:
            if o.is_dma:
                o.signal = True
                self.dcount[o.dma_sem] += 16
                o.count = self.dcount[o.dma_sem]
            elif o.signal:
                self.ecount[o.eng] += 1
                o.count = self.ecount[o.eng]
        by_eng = {e: [o for o in self.ops if o.eng == e] for e in self.ENGS}
        final_e = dict(self.ecount)
        final_d = dict(self.dcount)

        def run(ename, E):
            waited = self.waited[ename]
            for o in by_eng[ename]:
                need = {}
                for d in o.deps:
                    if d.is_dma:
                        k = ("d", d.dma_sem)
                    else:
                        k = ("e", d.eng)
                    if d.count > need.get(k, 0):
                        need[k] = d.count
                for k, v in need.items():
                    if waited.get(k, 0) < v:
                        sem = self.dsem[k[1]] if k[0] == "d" else self.esem[k[1]]
                        E.wait_ge(sem, v)
                        waited[k] = v
                ins = o.fn(E)
                if o.signal:
                    if o.is_dma:
                        ins.then_inc(self.dsem[o.dma_sem], 16)
                    else:
                        ins.then_inc(self.esem[o.eng], 1)
            for e2, v in final_e.items():
                if v > 0 and waited.get(("e", e2), 0) < v:
                    E.wait_ge(self.esem[e2], v)
                    waited[("e", e2)] = v
            for k2, v in final_d.items():
                if defer_cv and isinstance(k2, tuple) and k2[0] == "cv":
                    continue
                if v > 0 and waited.get(("d", k2), 0) < v:
                    E.wait_ge(self.dsem[k2], v)
                    waited[("d", k2)] = v

        with nc.Block() as block:
            @block.tensor
            def _(E):
                run("pe", E)

            @block.scalar
            def _(E):
                run("act", E)

            @block.vector
            def _(E):
                run("dve", E)

            @block.gpsimd
            def _(E):
                run("pool", E)

            @block.sync
            def _(E):
                run("sp", E)
        self.begin()


def build_program(debug=False):
    nc = bass.Bass("TRN2", target_bir_lowering=False)

    def din(name, shape, dt=F32):
        return nc.dram_tensor(name, list(shape), dt, kind="ExternalInput").ap()

    xh = din("xh", [4096, D])
    p_in = din("p", [S_OWN, 256])
    ident_in = din("ident", [128, 128])
    btab_in = din("btab", [128, 3 * 8 * 256])
    bhalo_in = din("bhalo", [128, 3 * 8 * 128])
    invcnt_in = din("invcnt", [128, 4 * 16])
    g_mix_in = din("g_mix_b", [128, D])
    g_ffn_in = din("g_ffn_b", [128, D])
    g_ple_in = din("g_ple_b", [128, D])
    g_fin_in = din("g_fin_b", [128, D])
    w_in = din("w_in", [D, 2048])
    w_pool = din("w_pool", [4, 128, 128])
    pscale_in = din("pscale", [128, 4])
    w_out = din("w_out", [D, D])
    w_router = din("w_router", [D, NE])
    brouter_in = din("b_router_b", [128, NE])
    w_gu = din("w_gate_up", [NE, D, 2 * D])
    bgu_in = din("bgu", [128, NE * 16])
    w_down = din("w_down", [NE, D, D])
    b_down = din("b_down", [NE, D])
    w_pg = din("w_ple_gate", [D, D])
    w_pp = din("w_ple_proj", [256, D])
    ltri_in = din("ltri", [128, 128])
    iota4_in = din("iota4", [128, 4 * NE])
    ebase_in = din("ebase", [128, NE])
    y = nc.dram_tensor("y", [S_OWN, D], F32, kind="ExternalOutput").ap()
    xs = nc.dram_tensor("xs_scratch", [NE * CAP, D], BF16).ap()
    ys = nc.dram_tensor("ys_scratch", [NE * CAP, D], F32).ap()
    x1_spill = nc.dram_tensor("x1_spill", [128, NT * D], F32).ap()
    wgu_bf = nc.dram_tensor("wgu_bf16", [NCONV, D, 2 * D], BF16).ap()
    wdn_bf = nc.dram_tensor("wdn_bf16", [NCONV, D, D], BF16).ap()
    dbg = {}
    if debug:
        dbg["mixT"] = nc.dram_tensor("dbg_mixT", [128, 8 * 2048], BF16, kind="ExternalOutput").ap()
        dbg["mixA"] = nc.dram_tensor("dbg_mixA", [64, 8 * 2048], BF16, kind="ExternalOutput").ap()
        dbg["x1"] = nc.dram_tensor("dbg_x1", [128, NT * D], F32, kind="ExternalOutput").ap()
        dbg["gates"] = nc.dram_tensor("dbg_gates", [128, NT * NE], F32, kind="ExternalOutput").ap()
        dbg["x2"] = nc.dram_tensor("dbg_x2", [128, NT * D], F32, kind="ExternalOutput").ap()

    w_in_v = w_in.rearrange("(kc p) n -> p kc n", p=128)
    w_out_v = w_out.rearrange("(kc p) n -> p kc n", p=128)
    w_pg_v = w_pg.rearrange("(kc p) n -> p kc n", p=128)
    w_pp_v = w_pp.rearrange("(kc p) n -> p kc n", p=128)
    w_router_v = w_router.rearrange("(kc p) n -> p kc n", p=128)

    with ExitStack() as stack:
        P = Prog(nc, stack)
        T = lambda name, shape, dt: stack.enter_context(nc.sbuf_tensor("sb_" + name, list(shape), dt))
        ident = T("ident_sb", [128, 128], BF16)
        ones_bf = T("ones_bf", [128, 64], BF16)
        sAB = ExitStack()
        TAB = lambda name, shape, dt: sAB.enter_context(nc.sbuf_tensor("sb_" + name, list(shape), dt))
        epsb = T("epsb", [128, 1], F32)
        c119 = T("c119", [128, 1], F32)
        slot_i32 = T("slot_i32", [128, NT, 4], mybir.dt.int32)
        g4n = T("g4n", [128, NT, 4], F32)
        actT = TAB("actT", [128, 4, 2048], BF16)
        mixA = TAB("mixA", [64, 8, 2048], BF16)

        def rms_tiles(x_ap_of, ntiles, g_b, dstT, dst_col0, pfx, xt_res, stats, hn_bufs, ps_tr,
                      pre=None, post=None):
            junk, ssq, rstd = stats
            for i in range(ntiles):
                if pre is not None:
                    pre(i)
                xa, xres = x_ap_of(i)
                P.op("act", lambda E, xa=xa, i=i: E.activation(out=junk[:], in_=xa, func=AF.Square,
                                                                accum_out=ssq[:, i:i + 1]),
                     reads=[xres], writes=[pfx + "junk", (pfx + "ssq", i)])
                P.op("act", lambda E, i=i: E.activation(out=rstd[:, i:i + 1], in_=ssq[:, i:i + 1], func=AF.Sqrt,
                                                        bias=epsb[:], scale=1.0 / D),
                     reads=[(pfx + "ssq", i), "epsb"], writes=[(pfx + "rstd", i)])
                P.op("dve", lambda E, i=i: E.reciprocal(out=rstd[:, i:i + 1], in_=rstd[:, i:i + 1]),
                     reads=[(pfx + "rstd", i)], writes=[(pfx + "rstd", i)])
                hb = hn_bufs[i % len(hn_bufs)]
                hres = (pfx + "hn", i % len(hn_bufs))
                P.op("dve", lambda E, xa=xa, i=i, hb=hb: E.scalar_tensor_tensor(
                    out=hb[:], in0=xa, scalar=rstd[:, i:i + 1], in1=g_b[:], op0=ALU.mult, op1=ALU.mult),
                     reads=[xres, (pfx + "rstd", i), "gains"], writes=[hres])
                pt = ps_tr[i % len(ps_tr)]
                pres = (pfx + "pstr", i % len(ps_tr))
                for kc in range(8):
                    P.op("pe", lambda E, kc=kc, hb=hb, pt=pt: E.transpose(pt[:, kc, :], hb[:, kc * 128:(kc + 1) * 128],
                                                                          ident[:]),
                         reads=[hres, "ident"], writes=[pres])
                c0 = dst_col0 + 128 * i
                P.op("act", lambda E, pt=pt, c0=c0: E.copy(out=dstT[:, :, c0:c0 + 128], in_=pt[:]),
                     reads=[pres], writes=[("T", c0 // 128)])
                if post is not None:
                    post(i, hb, hres)

        P.dma("pool", ident[:], ident_in[:, :], writes=["ident"], key=P.ckey())
        P.op("pool", lambda E: E.memset(ones_bf[:], 1.0), writes=["ones"])
        P.op("pool", lambda E: E.memset(epsb[:], EPS), writes=["epsb"])
        P.op("pool", lambda E: E.memset(c119[:], 7.0 * 1.702), writes=["c119"])
        zero_jobs = list(range(NE * CAP // 1024))

        with ExitStack() as sA:
            TA = lambda name, shape, dt: sA.enter_context(nc.sbuf_tensor("sb_" + name, list(shape), dt))
            hnT = TA("hnT", [128, 8, 4096], BF16)

            with ExitStack() as s1:
                T1 = lambda name, shape, dt: s1.enter_context(nc.sbuf_tensor("sb_" + name, list(shape), dt))
                g_b = T1("g_b", [128, D], F32)
                NXT = 4
                xts = [T1("xt%d" % i, [128, D], F32) for i in range(NXT)]
                hns = [T1("hn%d" % i, [128, D], BF16) for i in range(4)]
                junk = T1("junk", [128, D], F32)
                ssq = T1("ssq", [128, 32], F32)
                rstd = T1("rstd", [128, 32], F32)
                ps_tr = [s1.enter_context(nc.psum_tensor("pstrA%d" % i, [128, 8, 128], BF16)) for i in range(4)]
                P.dma("sp", g_b[:], g_mix_in[:, :], writes=["gains"], key=P.ckey())
                def ld(i):
                    P.dma("sp", xts[i % NXT][:], xh[128 * i:128 * (i + 1), :], writes=[("xt", i % NXT)],
                          key=("xt", i % NXT))

                zt = T1("zeros", [128, 8192], BF16)
                P.op("pool", lambda E: E.memset(zt[:], 0.0), writes=["zt"])

                def pre(i):
                    if i == 0:
                        ld(0)
                        ld(1)
                        ld(2)
                    if i + 3 < 32:
                        ld(i + 3)
                    if i >= 2 and zero_jobs:
                        c = zero_jobs.pop()
                        P.dma("sp", xs[1024 * c:1024 * (c + 1), :].rearrange("(p r) d -> p (r d)", p=128), zt[:],
                              reads=["zt"], key=("zx", c % 4))
                rms_tiles(lambda i: (xts[i % NXT][:], ("xt", i % NXT)), 32, g_b, hnT, 0, "A1", None,
                          (junk, ssq, rstd), hns, ps_tr, pre=pre)
                P.emit()

            with ExitStack() as s2:
                T2 = lambda name, shape, dt: s2.enter_context(nc.sbuf_tensor("sb_" + name, list(shape), dt))
                wpc = T2("wpc", [128, 8, 512], BF16)
                wpl = T2("wpl", [128, 4, 128], BF16)
                psc = T2("psc", [128, 4], F32)
                icn = T2("icn", [128, 4, 16], F32)
                u = T2("u", [128, 2064], F32)
                sa = T2("sa", [128, 2064], F32)
                sb = T2("sb", [128, 2064], F32)
                pooled = T2("pooled", [128, 2048], BF16)
                t16 = T2("t16", [128, 16], F32)
                ps = [s2.enter_context(nc.psum_tensor("psA2_%d" % i, [128, 512], F32)) for i in range(2)]
                P.dma("pool", wpc[:], w_in_v[:, :, 0:512], writes=["wpc"], key="wpc")
                P.dma("pool", wpl[:], w_pool.rearrange("g c d -> c g d"), writes=["wpl"], key="wpl")
                P.dma("sp", psc[:], pscale_in[:, :], writes=["psc"], key=P.ckey())
                P.dma("sp", icn[:], invcnt_in.rearrange("p (g t) -> p g t", g=4), writes=["icn"], key=P.ckey())
                pi = 0
                for g in range(4):
                    w = (2, 4, 8, 16)[g]
                    for blk in range(5):
                        pp = ps[pi % 2]
                        pres = ("psA2", pi % 2)
                        pi += 1
                        if blk == 0:
                            c0, n, o0 = 2032, 16, 0
                        else:
                            c0, n, o0 = 2048 + 512 * (blk - 1), 512, 16 + 512 * (blk - 1)
                        for kc in range(8):
                            P.op("pe", lambda E, pp=pp, kc=kc, g=g, c0=c0, n=n: E.matmul(
                                pp[:, 0:n], lhsT=wpc[:, kc, g * 128:(g + 1) * 128], rhs=hnT[:, kc, c0:c0 + n],
                                start=(kc == 0), stop=(kc == 7)), reads=["wpc", "hnT"], writes=[pres])
                        P.op("act", lambda E, pp=pp, n=n, o0=o0: E.copy(out=u[:, o0:o0 + n], in_=pp[:, 0:n]),
                             reads=[pres], writes=["u"])
                    src, srcres = u, "u"
                    sh = 1
                    bufs = [(sa, "sa"), (sb, "sb")]
                    bi = 0
                    while sh < w:
                        dst, dres = bufs[bi % 2]
                        bi += 1
                        P.op("dve", lambda E, dst=dst, src=src, sh=sh: E.tensor_tensor(
                            out=dst[:, sh:2064], in0=src[:, sh:2064], in1=src[:, 0:2064 - sh], op=ALU.add),
                             reads=[srcres], writes=[dres])
                        src, srcres = dst, dres
                        sh *= 2
                    P.op("dve", lambda E, src=src, w=w: E.scalar_tensor_tensor(
                        out=pooled[:], in0=src[:, 16:2064], scalar=1.0 / w, in1=u[:, 16:2064],
                        op0=ALU.mult, op1=ALU.subtract), reads=[srcres, "u"], writes=["pooled"])
                    P.op("dve", lambda E, src=src, g=g: E.tensor_tensor(
                        out=t16[:], in0=src[:, 16:32], in1=icn[:, g, :], op=ALU.mult),
                         reads=[srcres, "icn"], writes=["t16"])
                    P.op("dve", lambda E: E.tensor_tensor(out=pooled[:, 0:16], in0=t16[:], in1=u[:, 16:32],
                                                          op=ALU.subtract),
                         reads=["t16", "u", "pooled"], writes=["pooled"])
                    for blk in range(4):
                        pp = ps[pi % 2]
                        pres = ("psA2", pi % 2)
                        pi += 1
                        P.op("pe", lambda E, pp=pp, g=g, blk=blk: E.matmul(
                            pp[:], lhsT=wpl[:, g, :], rhs=pooled[:, 512 * blk:512 * (blk + 1)], start=True, stop=True),
                             reads=["wpl", "pooled"], writes=[pres])
                        P.op("dve", lambda E, pp=pp, g=g, blk=blk: E.tensor_scalar(
                            out=actT[:, g, 512 * blk:512 * (blk + 1)], in0=pp[:], scalar1=psc[:, g:g + 1],
                            scalar2=None, op0=ALU.mult), reads=[pres, "psc"], writes=[("mixT", g, blk)])
                P.emit()

            with ExitStack() as s3:
                T3 = lambda name, shape, dt: s3.enter_context(nc.sbuf_tensor("sb_" + name, list(shape), dt))
                btabs = [T3("btab%d" % i, [128, 3, 2, 256], BF16) for i in range(2)]
                bhalos = [T3("bhalo%d" % i, [128, 3, 2, 128], BF16) for i in range(2)]
                wqs = [T3("wq%d" % i, [128, 8, 128], BF16) for i in range(2)]
                wks = [T3("wk%d" % i, [128, 8, 128], BF16) for i in range(2)]
                wvs = [T3("wv%d" % i, [128, 8, 128], BF16) for i in range(2)]
                QT = T3("QTz", [128, 2, 2048], BF16)
                KT = T3("KT", [128, 4096], BF16)
                VT = T3("VT", [128, 4096], BF16)
                V = T3("V", [128, 69, 2, 65], BF16)
                acc = T3("acc", [65, 2, 2048], F32)
                onesf = T3("onesf", [65, 64], F32)
                NSB, NOB = 4, 3
                Pb = [T3("Pb%d" % i, [128, 2, 256], BF16) for i in range(NSB)]
                ps_tr = s3.enter_context(nc.psum_tensor("pstrV", [128, 8, 128], BF16))
                ps_s = [s3.enter_context(nc.psum_tensor("pss%d" % i, [128, 2, 256], F32)) for i in range(NSB)]
                ps_o = [s3.enter_context(nc.psum_tensor("pso%d" % i, [128, 2, 256], F32)) for i in range(NOB)]
                ps_pr = [t[:].rearrange("p h q -> p (h q)") for t in ps_o]
                btab_v = btab_in.rearrange("p (b h q) -> p b h q", b=3, h=8)
                bhalo_v = bhalo_in.rearrange("p (b h q) -> p b h q", b=3, h=8)
                P.op("dve", lambda E: E.memset(V[:], 1.0), writes=["V"])
                P.op("dve", lambda E: E.memset(QT[:], 0.0), writes=["QT"])
                P.op("dve", lambda E: E.memset(onesf[:], 1.0), writes=["onesf"])

                def tok_slice(start, dil, n=128):
                    return slice(start, start + (n - 1) * dil + 1, dil)

                ktiles = []
                for j in range(15, 32):
                    ks = tok_slice(128 * j, 1)
                    if j == 15:
                        ktiles.append((0, ks, tok_slice(0, 1), 128, "halo", 0))
                    elif j == 31:
                        ktiles.append((0, ks, tok_slice(128 * 15, 1), 128, "tab", 0))
                    else:
                        ktiles.append((0, ks, tok_slice(128 * (j - 16), 1, 256), 256, "tab", 0))
                for n in range(3, 8):
                    for r in range(4):
                        ks = tok_slice(512 * n + r, 4)
                        if n == 3:
                            ktiles.append((1, ks, tok_slice(r, 4), 128, "halo", 0))
                        elif n == 7:
                            ktiles.append((1, ks, tok_slice(512 * 3 + r, 4), 128, "tab", 0))
                        else:
                            ktiles.append((1, ks, tok_slice(512 * (n - 4) + r, 4, 256), 256, "tab", 0))
                for n in range(2):
                    for r in range(16):
                        ks = tok_slice(2048 * n + r, 16)
                        ktiles.append((2, ks, tok_slice(r, 16), 128, "halo" if n == 0 else "tab", 0))
                assert len(ktiles) == 69

                def load_hp(hp):
                    cq = 512 + 128 * hp
                    w = hp % 2
                    P.dma("pool", wqs[w][:], w_in_v[:, :, cq:cq + 128], writes=[("wq", w)], key=("wq", w))
                    P.dma("pool", wks[w][:], w_in_v[:, :, 512 + cq:512 + cq + 128], writes=[("wk", w)], key=("wk", w))
                    P.dma("pool", wvs[w][:], w_in_v[:, :, 1024 + cq:1024 + cq + 128], writes=[("wv", w)], key=("wv", w))
                    P.dma("pool", btabs[w][:], btab_v[:, :, 2 * hp:2 * hp + 2, :], writes=[("btab", w)], key=("btab", w))
                    P.dma("pool", bhalos[w][:], bhalo_v[:, :, 2 * hp:2 * hp + 2, :], writes=[("bhalo", w)],
                          key=("bhalo", w))

                conv_jobs = []
                for e in range(NCONV):
                    for r in range(8):
                        conv_jobs.append((wgu_bf[e][128 * r:128 * (r + 1), :], w_gu[e][128 * r:128 * (r + 1), :]))
                    for r in range(8):
                        conv_jobs.append((wdn_bf[e][128 * r:128 * (r + 1), :], w_down[e][128 * r:128 * (r + 1), :]))
                conv_jobs.reverse()
                conv_n = [0]

                def conv_issue():
                    if conv_jobs:
                        dst, src = conv_jobs.pop()
                        P.dma("pool", dst, src, key=("cv", conv_n[0] % 10))
                        conv_n[0] += 1

                ppi = 0
                load_hp(0)
                for hp in range(4):
                    if hp + 1 < 4:
                        load_hp(hp + 1)
                    w = hp % 2
                    wq, wk, wv, btab, bhalo = wqs[w], wks[w], wvs[w], btabs[w], bhalos[w]
                    P.op("dve", lambda E: E.memset(acc[:], 0.0), writes=["acc"])
                    for (wt, wres, dst, dres, t0, nblk, scale) in ((wq, ("wq", w), QT, "QT", 2048, 4, 0.125),
                                                                    (wk, ("wk", w), KT, "KT", 0, 8, None),
                                                                    (wv, ("wv", w), VT, "VT", 0, 8, None)):
                        for blk in range(nblk):
                            pp = ps_pr[ppi % NOB]
                            pres = ("pso", ppi % NOB)
                            ppi += 1
                            for kc in range(8):
                                P.op("pe", lambda E, pp=pp, wt=wt, kc=kc, c0=t0 + 512 * blk: E.matmul(
                                    pp[:], lhsT=wt[:, kc, :], rhs=hnT[:, kc, c0:c0 + 512], start=(kc == 0),
                                    stop=(kc == 7)), reads=[wres, "hnT"], writes=[pres])
                            if scale is not None:
                                for h2 in range(2):
                                    P.op("act", lambda E, pp=pp, dst=dst, blk=blk, scale=scale, h2=h2: E.mul(
                                        out=dst[64 * h2:64 * (h2 + 1), h2, 512 * blk:512 * (blk + 1)],
                                        in_=pp[64 * h2:64 * (h2 + 1), :], mul=scale),
                                         reads=[pres], writes=[dres])
                            else:
                                P.op("act", lambda E, pp=pp, dst=dst, blk=blk: E.copy(
                                    out=dst[:, 512 * blk:512 * (blk + 1)], in_=pp[:]), reads=[pres], writes=[dres])
                    for t0 in range(0, 69, 8):
                        nt = min(8, 69 - t0)
                        for k in range(nt):
                            P.op("pe", lambda E, k=k, sl=ktiles[t0 + k][1]: E.transpose(ps_tr[:, k, :], VT[:, sl], ident[:]),
                                 reads=["VT", "ident"], writes=["pstrV"])
                        P.op("dve", lambda E, t0=t0, nt=nt: E.tensor_copy(
                            out=V[:, t0:t0 + nt, :, 0:64],
                            in_=ps_tr[:, 0:nt, :].rearrange("p t (h d) -> p t h d", h=2)),
                             reads=["pstrV"], writes=["V"])

                    def emit_bias(ti):
                        br, ks, qs, nq, kind, c0 = ktiles[ti]
                        b = ti % 2
                        for h2 in range(2):
                            if kind == "halo":
                                bt = bhalo[:, br, h2, :]
                            else:
                                bt = btab[:, br, h2, c0:c0 + nq]
                            P.op("pe", lambda E, b=b, bt=bt, nq=nq, h2=h2: E.matmul(
                                ps_s[b][:, h2, 0:nq], lhsT=ident[:], rhs=bt, start=(h2 == 0), stop=False,
                                skip_group_check=True),
                                 reads=["ident", "btab", "bhalo"], writes=[("pss", b)])

                    def emit_QK(ti):
                        br, ks, qs, nq, kind, c0 = ktiles[ti]
                        b = ti % 2
                        for h2 in range(2):
                            r0 = 64 * h2
                            P.op("pe", lambda E, b=b, h2=h2, nq=nq, ks=ks, qs=qs, r0=r0: E.matmul(
                                ps_s[b][:, h2, 0:nq], lhsT=KT[r0:r0 + 64, ks], rhs=QT[r0:r0 + 64, qs], start=False,
                                stop=(h2 == 1), tile_position=(r0, 0), skip_group_check=True),
                                 reads=["KT", "QT"], writes=[("pss", b)])
                        P.op("act", lambda E, b=b, nq=nq: E.activation(out=Pb[b][:, :, 0:nq], in_=ps_s[b][:, :, 0:nq],
                                                                      func=AF.Exp),
                             reads=[("pss", b)], writes=[("Pb", b)])

                    def emit_PV(ti):
                        br, ks, qs, nq, kind, c0 = ktiles[ti]
                        b = ti % NSB
                        ob = ti % NOB
                        for h2 in range(2):
                            P.op("pe", lambda E, h2=h2, b=b, ob=ob, nq=nq, ti=ti: E.matmul(
                                ps_o[ob][0:65, h2, 0:nq], lhsT=V[:, ti, h2, :], rhs=Pb[b][:, h2, 0:nq], start=True,
                                stop=True), reads=["V", ("Pb", b)], writes=[("pso", ob)])
                        P.op("dve", lambda E, ob=ob, qs=qs, nq=nq: E.tensor_tensor(
                            out=acc[:, :, qs], in0=acc[:, :, qs], in1=ps_o[ob][0:65, :, 0:nq], op=ALU.add),
                             reads=[("pso", ob), "acc"], writes=["acc"])

                    def emit_S_old(ti):
                        br, ks, qs, nq, kind, c0 = ktiles[ti]
                        b = ti % NSB
                        out = ps_s[b][:, :, 0:nq]
                        P.op("pe", lambda E, out=out, ks=ks, qs=qs: E.matmul(
                            out, lhsT=KT[:, ks], rhs=QT[:, :, qs], start=True, stop=False),
                             reads=["KT", "QT"], writes=[("pss", b)])
                        if kind == "halo":
                            bt = bhalo[:, br, :, :]
                        else:
                            bt = btab[:, br, :, c0:c0 + nq]
                        P.op("pe", lambda E, out=out, bt=bt: E.matmul(
                            out, lhsT=ident[:], rhs=bt, start=False, stop=True),
                             reads=["ident", ("btab", w), ("bhalo", w)], writes=[("pss", b)])
                        P.op("act", lambda E, b=b, nq=nq: E.activation(out=Pb[b][:, :, 0:nq], in_=ps_s[b][:, :, 0:nq],
                                                                      func=AF.Exp),
                             reads=[("pss", b)], writes=[("Pb", b)])

                    if OPT_ATT_NEW:
                        emit_bias(0)
                        emit_QK(0)
                        for ti in range(len(ktiles)):
                            if ti + 1 < len(ktiles):
                                emit_bias(ti + 1)
                            emit_PV(ti)
                            if ti + 1 < len(ktiles):
                                emit_QK(ti + 1)
                    else:
                        LA = NSB - 1
                        for t in range(LA):
                            emit_S_old(t)
                        for ti in range(len(ktiles)):
                            if ti + LA < len(ktiles):
                                emit_S_old(ti + LA)
                            emit_PV(ti)
                            if ti % 2 == 0:
                                conv_issue()
                    for h2 in range(2):
                        h = 2 * hp + h2
                        if OPT_ACT_RECIP:
                            P.op("act", lambda E, h2=h2: E.activation(out=acc[64:65, h2, :], in_=acc[64:65, h2, :], func=AF.Ln),
                                 reads=["acc"], writes=["acc"])
                            P.op("act", lambda E, h2=h2: E.activation(out=acc[64:65, h2, :], in_=acc[64:65, h2, :],
                                                                      func=AF.Exp, scale=-1.0),
                                 reads=["acc"], writes=["acc"])
                        else:
                            P.op("dve", lambda E, h2=h2: E.reciprocal(out=acc[64:65, h2, :], in_=acc[64:65, h2, :]),
                                 reads=["acc"], writes=["acc"])
                        for blk in range(4):
                            pp = ps_pr[ppi % NOB]
                            pres = ("pso", ppi % NOB)
                            ppi += 1
                            P.op("pe", lambda E, pp=pp, blk=blk, h2=h2: E.matmul(
                                pp[0:64, :], lhsT=onesf[64:65, :], rhs=acc[64:65, h2, 512 * blk:512 * (blk + 1)],
                                start=True, stop=True, tile_position=(64, 0)), reads=["onesf", "acc"], writes=[pres])
                            P.op("dve", lambda E, pp=pp, blk=blk, h=h, h2=h2: E.tensor_tensor(
                                out=mixA[:, h, 512 * blk:512 * (blk + 1)], in0=acc[0:64, h2, 512 * blk:512 * (blk + 1)],
                                in1=pp[0:64, :], op=ALU.mult), reads=[pres, "acc"], writes=[("mixA", h, blk)])
                while conv_jobs:
                    conv_issue()
                P.emit(defer_cv=True)
        if debug:
            P.dma("sp", dbg["mixT"].rearrange("p (c t) -> p c t", c=8)[:, 0:4, :], actT[:], key="dbg")
            P.emit()
            P.dma("sp", dbg["mixA"].rearrange("p (c t) -> p c t", c=8), mixA[:], key="dbg")
            P.emit()

        with ExitStack() as sR:
            TR = lambda name, shape, dt: sR.enter_context(nc.sbuf_tensor("sb_" + name, list(shape), dt))
            x1 = TR("x1", [128, NT, D], F32)
            g_b = TR("g_b2", [128, D], F32)
            junk = TR("junk2", [128, D], F32)
            ssq = TR("ssq2", [128, NT], F32)
            rstd = TR("rstd2", [128, NT], F32)
            hns = [TR("hnb%d" % i, [128, D], BF16) for i in range(4)]

            with ExitStack() as sB:
                TB = lambda name, shape, dt: sB.enter_context(nc.sbuf_tensor("sb_" + name, list(shape), dt))
                wo = TB("wo", [128, 4, D], BF16)
                woA = TB("woA", [64, 8, D], BF16)
                ps = [sB.enter_context(nc.psum_tensor("psB%d" % i, [128, 512], F32)) for i in range(4)]
                P.dma("pool", wo[:], w_out_v[:, 0:4, :], writes=["wo"], key="wo")
                P.dma("pool", woA[:], w_out[512:1024, :].rearrange("(h d) n -> d h n", d=64), writes=["wo"], key="wk")
                for i in range(NT):
                    P.dma("sp", x1[:, i, :], xh[2048 + 128 * i:2048 + 128 * (i + 1), :], writes=[("x1", i)],
                          key=("x1", i % 4))
                pi = 0
                for i in range(NT):
                    for half in range(2):
                        pp = ps[pi % 4]
                        pres = ("psB", pi % 4)
                        pi += 1
                        for kc in range(4):
                            P.op("pe", lambda E, pp=pp, kc=kc, i=i, half=half: E.matmul(
                                pp[:], lhsT=actT[:, kc, 128 * i:128 * (i + 1)], rhs=wo[:, kc, 512 * half:512 * (half + 1)],
                                start=(kc == 0), stop=False), reads=["wo", "mixT"], writes=[pres])
                        for h in range(8):
                            P.op("pe", lambda E, pp=pp, h=h, i=i, half=half: E.matmul(
                                pp[:], lhsT=mixA[:, h, 128 * i:128 * (i + 1)], rhs=woA[:, h, 512 * half:512 * (half + 1)],
                                start=False, stop=(h == 7)), reads=["wo", "mixT"], writes=[pres])
                        P.op("dve", lambda E, pp=pp, i=i, half=half: E.tensor_tensor(
                            out=x1[:, i, 512 * half:512 * (half + 1)], in0=x1[:, i, 512 * half:512 * (half + 1)],
                            in1=pp[:], op=ALU.add), reads=[pres, ("x1", i)], writes=[("x1", i)])
                P.emit(defer_cv=True)
            if debug:
                P.dma("sp", dbg["x1"].rearrange("p (c t) -> p c t", c=NT), x1[:], key="dbg")
                P.emit()

            with ExitStack() as sC1:
                TC1 = lambda name, shape, dt: sC1.enter_context(nc.sbuf_tensor("sb_" + name, list(shape), dt))
                hn2T = TC1("hn2T", [128, 8, 2048], BF16)
                gates = TC1("gates", [128, NT, NE], F32)
                gates_bf = TC1("gates_bf", [128, NT, NE], BF16)
                gT = TC1("gT", [NE, NT, 128], BF16)
                bdn = TC1("bdn", [NE, D], BF16)
                wr = TC1("wr", [128, 8, NE], BF16)
                brb = TC1("brb", [128, NE], F32)
                ltri = TC1("ltri", [128, 128], BF16)
                ones128 = TC1("ones128", [128, 128], BF16)
                iota4 = TC1("iota4", [128, 4, NE], F32)
                ebase = TC1("ebase", [128, NE], F32)
                carry = TC1("carry", [128, NE], F32)
                lg = TC1("lg", [128, NE], F32)
                m8 = TC1("m8", [128, 8], F32)
                idx8 = TC1("idx8", [128, 8], mybir.dt.uint32)
                ef = TC1("ef", [128, 4], F32)
                negm = TC1("negm", [128, 1], F32)
                ex = TC1("ex", [128, NE], F32)
                msk = TC1("msk", [128, NE], F32)
                mskb = TC1("mskb", [128, NE], BF16)
                ssum = TC1("ssum", [128, 1], F32)
                posf = TC1("posf", [128, NE], F32)
                ovf = TC1("ovf", [128, NE], F32)
                oh4 = TC1("oh4", [128, 4, NE], F32)
                pr4 = TC1("pr4", [128, 4, NE], F32)
                slotf = TC1("slotf", [128, 4], F32)
                g4 = TC1("g4", [128, 4], F32)
                ps_tr = [sC1.enter_context(nc.psum_tensor("pstrC%d" % i, [128, 8, 128], BF16)) for i in range(2)]
                ps_l = sC1.enter_context(nc.psum_tensor("psl", [128, NE], F32))
                ps_pos = sC1.enter_context(nc.psum_tensor("pspos", [128, NE], F32))
                ps_cnt = sC1.enter_context(nc.psum_tensor("pscnt", [128, NE], F32))
                ps_g = sC1.enter_context(nc.psum_tensor("psgT", [NE, 128], BF16))
                ps_b = [sC1.enter_context(nc.psum_tensor("psbd%d" % i, [128, 512], F32)) for i in range(2)]
                P.dma("sp", g_b[:], g_ffn_in[:, :], writes=["gains"], key=P.ckey())
                P.dma("pool", wr[:], w_router_v, writes=["wr"], key="wr")
                P.dma("sp", brb[:], brouter_in[:, :], writes=["brb"], key=P.ckey())
                P.dma("pool", bdn[:], b_down[:, :], writes=["bdn"], key=P.ckey())
                P.dma("pool", ltri[:], ltri_in[:, :], writes=["ltri"], key=P.ckey())
                P.dma("sp", iota4[:], iota4_in.rearrange("p (k e) -> p k e", k=4), writes=["iota4"], key=P.ckey())
                P.dma("sp", ebase[:], ebase_in[:, :], writes=["ebase"], key=P.ckey())
                P.op("pool", lambda E: E.memset(ones128[:], 1.0), writes=["ones128"])
                P.op("pool", lambda E: E.memset(carry[:], 0.0), writes=["carry"])

                def route(i, hb, hres):
                    for kc in range(8):
                        P.op("pe", lambda E, kc=kc, i=i: E.matmul(
                            ps_l[:], lhsT=hn2T[:, kc, 128 * i:128 * (i + 1)], rhs=wr[:, kc, :],
                            start=(kc == 0), stop=(kc == 7)), reads=["wr", ("T", i)], writes=["psl"])
                    P.op("dve", lambda E: E.tensor_tensor(out=lg[:], in0=ps_l[:], in1=brb[:], op=ALU.add),
                         reads=["psl", "brb"], writes=["lg"])
                    P.op("dve", lambda E: E.max(out=m8[:], in_=lg[:]), reads=["lg"], writes=["m8"])
                    P.op("dve", lambda E: E.max_index(out=idx8[:], in_max=m8[:], in_values=lg[:]),
                         reads=["lg", "m8"], writes=["idx8"])
                    P.op("dve", lambda E: E.tensor_copy(out=ef[:], in_=idx8[:, 0:4]), reads=["idx8"], writes=["ef"])
                    P.op("dve", lambda E: E.tensor_scalar(out=negm[:], in0=m8[:, 0:1], scalar1=-1.0, scalar2=None,
                                                          op0=ALU.mult), reads=["m8"], writes=["negm"])
                    P.op("dve", lambda E: E.tensor_scalar(out=msk[:], in0=lg[:], scalar1=m8[:, 3:4], scalar2=None,
                                                          op0=ALU.is_ge), reads=["lg", "m8"], writes=["msk"])
                    P.op("dve", lambda E: E.tensor_copy(out=mskb[:], in_=msk[:]), reads=["msk"], writes=["mskb"])
                    P.op("act", lambda E: E.activation(out=ex[:], in_=lg[:], func=AF.Exp, bias=negm[:], scale=1.0),
                         reads=["lg", "negm"], writes=["ex"])
                    P.op("dve", lambda E: E.tensor_tensor(out=ex[:], in0=ex[:], in1=msk[:], op=ALU.mult),
                         reads=["ex", "msk"], writes=["ex"])
                    P.op("dve", lambda E: E.reduce_sum(out=ssum[:], in_=ex[:], axis=mybir.AxisListType.X),
                         reads=["ex"], writes=["ssum"])
                    P.op("dve", lambda E: E.reciprocal(out=ssum[:], in_=ssum[:]), reads=["ssum"], writes=["ssum"])
                    P.op("dve", lambda E, i=i: E.tensor_scalar(out=gates[:, i, :], in0=ex[:], scalar1=ssum[:, 0:1],
                                                               scalar2=None, op0=ALU.mult),
                         reads=["ex", "ssum"], writes=[("gates", i)])
                    P.op("dve", lambda E, i=i: E.tensor_copy(out=gates_bf[:, i, :], in_=gates[:, i, :]),
                         reads=[("gates", i)], writes=[("gates_bf", i)])
                    P.op("pe", lambda E, i=i: E.transpose(ps_g[:], gates_bf[:, i, :], ident[:]),
                         reads=[("gates_bf", i), "ident"], writes=["psgT"])
                    P.op("act", lambda E, i=i: E.copy(out=gT[:, i, :], in_=ps_g[:]), reads=["psgT"],
                         writes=[("gT", i)])
                    P.op("pe", lambda E: E.matmul(ps_pos[:], lhsT=ltri[:], rhs=mskb[:], start=True, stop=True),
                         reads=["ltri", "mskb"], writes=["pspos"])
                    P.op("pe", lambda E: E.matmul(ps_cnt[:], lhsT=ones128[:], rhs=mskb[:], start=True, stop=True),
                         reads=["ones128", "mskb"], writes=["pscnt"])
                    P.op("dve", lambda E: E.tensor_tensor(out=posf[:], in0=ps_pos[:], in1=carry[:], op=ALU.add),
                         reads=["pspos", "carry"], writes=["posf"])
                    P.op("dve", lambda E: E.tensor_tensor(out=carry[:], in0=ps_cnt[:], in1=carry[:], op=ALU.add),
                         reads=["pscnt", "carry", "posf"], writes=["carry"])
                    P.op("dve", lambda E: E.tensor_scalar(out=ovf[:], in0=posf[:], scalar1=float(CAP), scalar2=BIGSLOT,
                                                          op0=ALU.is_ge, op1=ALU.mult), reads=["posf"], writes=["ovf"])
                    P.op("dve", lambda E: E.tensor_tensor(out=posf[:], in0=posf[:], in1=ebase[:], op=ALU.add),
                         reads=["posf", "ebase", "ovf"], writes=["posf"])
                    P.op("dve", lambda E: E.tensor_tensor(out=posf[:], in0=posf[:], in1=ovf[:], op=ALU.add),
                         reads=["posf", "ovf"], writes=["posf"])
                    P.op("dve", lambda E: E.tensor_tensor(
                        out=oh4[:], in0=iota4[:], in1=ef[:].unsqueeze(2).to_broadcast([128, 4, NE]), op=ALU.is_equal),
                         reads=["iota4", "ef"], writes=["oh4"])
                    P.op("dve", lambda E: E.tensor_tensor(
                        out=pr4[:], in0=oh4[:], in1=posf[:].unsqueeze(1).to_broadcast([128, 4, NE]), op=ALU.mult),
                         reads=["oh4", "posf"], writes=["pr4"])
                    P.op("dve", lambda E: E.reduce_sum(out=slotf[:], in_=pr4[:], axis=mybir.AxisListType.X),
                         reads=["pr4"], writes=["slotf"])
                    P.op("dve", lambda E, i=i: E.tensor_copy(out=slot_i32[:, i, :], in_=slotf[:]),
                         reads=["slotf"], writes=[("slot", i)])
                    P.op("dve", lambda E, i=i: E.tensor_tensor(
                        out=pr4[:], in0=oh4[:], in1=gates[:, i, :].unsqueeze(1).to_broadcast([128, 4, NE]), op=ALU.mult),
                         reads=["oh4", ("gates", i), "pr4"], writes=["pr4"])
                    P.op("dve", lambda E: E.reduce_sum(out=g4[:], in_=pr4[:], axis=mybir.AxisListType.X),
                         reads=["pr4"], writes=["g4"])
                    P.op("dve", lambda E, i=i: E.tensor_scalar(out=g4n[:, i, :], in0=g4[:], scalar1=-1.0, scalar2=None,
                                                               op0=ALU.mult), reads=["g4"], writes=[("g4n", i)])
                    for k in range(4):
                        P.op("pool", lambda E, i=i, k=k, hb=hb: E.indirect_dma_start(
                            out=xs[:, :], out_offset=bass.IndirectOffsetOnAxis(ap=slot_i32[:, i, k:k + 1], axis=0),
                            in_=hb[:], in_offset=None, bounds_check=P.bc_reg(E, NE * CAP - 1), oob_is_err=False),
                             reads=[hres, ("slot", i)], writes=[], dma_key=("scat", i % 2, k))

                rms_tiles(lambda i: (x1[:, i, :], ("x1", i)), NT, g_b, hn2T, 0, "C1", None,
                          (junk, ssq, rstd), hns, ps_tr, post=route)
                for i in range(NT):
                    for half in range(2):
                        pp = ps_b[(2 * i + half) % 2]
                        pres = ("psbd", (2 * i + half) % 2)
                        P.op("pe", lambda E, pp=pp, i=i, half=half: E.matmul(
                            pp[:], lhsT=gT[:, i, :], rhs=bdn[:, 512 * half:512 * (half + 1)], start=True, stop=True),
                             reads=[("gT", i), "bdn"], writes=[pres])
                        P.op("dve", lambda E, pp=pp, i=i, half=half: E.tensor_tensor(
                            out=x1[:, i, 512 * half:512 * (half + 1)], in0=x1[:, i, 512 * half:512 * (half + 1)],
                            in1=pp[:], op=ALU.add), reads=[pres, ("x1", i)], writes=[("x1", i)])
                    P.dma("sp", x1_spill[:, D * i:D * (i + 1)], x1[:, i, :], reads=[("x1", i)], key=("spill", i % 4))
                P.emit()
                if debug:
                    P.dma("sp", dbg["gates"].rearrange("p (c t) -> p c t", c=NT), gates[:], key="dbg")
                    P.emit()

        sAB.close()
        with ExitStack() as sC2:
            TC2 = lambda name, shape, dt: sC2.enter_context(nc.sbuf_tensor("sb_" + name, list(shape), dt))
            NRING = 8
            ring = [TC2("ring%d" % i, [128, 8, 512], BF16) for i in range(NRING)]
            bgu = TC2("bgu", [128, NE, 16], F32)
            xes = [TC2("xe%d" % i, [128, CAP // 128, D], BF16) for i in range(2)]
            xeTs = [TC2("xeT%d" % i, [128, 8, CAP], BF16) for i in range(2)]
            act_es = [TC2("act_e%d" % i, [128, 8, CAP], BF16) for i in range(2)]
            rs = [TC2("r%d" % i, [128, CAP], F32) for i in range(2)]
            sgs = [TC2("sg%d" % i, [128, CAP], F32) for i in range(2)]
            ucs = [TC2("uc%d" % i, [128, CAP], F32) for i in range(2)]
            yts = [TC2("yt%d" % i, [128, D], F32) for i in range(3)]
            ps_gu = [sC2.enter_context(nc.psum_tensor("psgu%d" % i, [128, 512], F32)) for i in range(4)]
            ps_d = [sC2.enter_context(nc.psum_tensor("psd%d" % i, [128, 512], F32)) for i in range(2)]
            ps_tr = [sC2.enter_context(nc.psum_tensor("pstrE%d" % i, [128, 8, 128], BF16)) for i in range(2)]
            P.dma("sp", bgu[:], bgu_in.rearrange("p (e c) -> p e c", e=NE), writes=["bgu"], key=P.ckey())
            P.op("dve", lambda E: E.tensor_scalar(out=bgu[:, :, 0:8], in0=bgu[:, :, 0:8], scalar1=-1.0, scalar2=7.0,
                                                  op0=ALU.mult, op1=ALU.add), reads=["bgu"], writes=["bgu"])
            P.op("dve", lambda E: E.tensor_scalar(out=bgu[:, :, 8:16], in0=bgu[:, :, 8:16], scalar1=1.0, scalar2=None,
                                                  op0=ALU.add), reads=["bgu"], writes=["bgu"])
            pieces = []
            for e in range(NE):
                for j in range(2):
                    pieces.append((e, "g", j))
                    pieces.append((e, "u", j))
                for half in range(2):
                    pieces.append((e, "dn", half))

            def piece_dma(n):
                e, kind, j = pieces[n]
                slot = n % NRING
                wg_e = wgu_bf[e] if e < NCONV else w_gu[e]
                wd_e = wdn_bf[e] if e < NCONV else w_down[e]
                for part in range(2):
                    if kind in ("g", "u"):
                        c0 = (0 if kind == "g" else 1024) + 512 * j
                        src = wg_e.rearrange("(kc p) f -> p kc f", p=128)[:, 4 * part:4 * (part + 1), c0:c0 + 512]
                    else:
                        src = wd_e.rearrange("(kc p) n -> p kc n", p=128)[
                            :, 4 * part:4 * (part + 1), 512 * j:512 * (j + 1)]
                    dst = ring[slot][:, 4 * part:4 * (part + 1), :]
                    P.dma("pool", dst, src, writes=[("ring", slot, part)], key=("ring", slot, part))

            def xe_load(e):
                b = e % 2
                P.dma("sp", xes[b][:], xs[e * CAP:(e + 1) * CAP, :].rearrange("(j p) d -> p j d", p=128),
                      writes=[("xe", b)], key=("xe", b))

            LOOK = NRING - 2
            for n in range(min(LOOK, len(pieces))):
                piece_dma(n)
            xe_load(0)
            gi = 0
            di = 0
            ei = 0
            ti = 0
            yi = 0
            for n, (e, kind, j) in enumerate(pieces):
                if n + LOOK < len(pieces):
                    piece_dma(n + LOOK)
                slot = n % NRING
                rg = ring[slot]
                b = e % 2
                xeT, act_e = xeTs[b], act_es[b]
                if kind == "g" and j == 0:
                    if e + 1 < NE:
                        xe_load(e + 1)
                    for jj in range(CAP // 128):
                        pt, ptres = ps_tr[ti % 2], ("pstrE", ti % 2)
                        ti += 1
                        for kc in range(8):
                            P.op("pe", lambda E, pt=pt, kc=kc, jj=jj, b=b: E.transpose(
                                pt[:, kc, :], xes[b][:, jj, 128 * kc:128 * (kc + 1)], ident[:]),
                                 reads=[("xe", b), "ident"], writes=[ptres])
                        P.op("act", lambda E, pt=pt, jj=jj, xeT=xeT: E.copy(out=xeT[:, :, 128 * jj:128 * (jj + 1)], in_=pt[:]),
                             reads=[ptres], writes=[("xeT", b)])
                if kind == "g":
                    continue
                if kind == "u":
                    slot_g = (n - 1) % NRING
                    rgg = ring[slot_g]
                    for c in range(4):
                        fc = 4 * j + c
                        pg, pgres = ps_gu[gi % 4], ("psgu", gi % 4)
                        gi += 1
                        pu, pures = ps_gu[gi % 4], ("psgu", gi % 4)
                        gi += 1
                        for (pp, pres, wt, wslot) in ((pg, pgres, rgg, slot_g), (pu, pures, rg, slot)):
                            for kc in range(8):
                                P.op("pe", lambda E, pp=pp, wt=wt, kc=kc, c=c, xeT=xeT: E.matmul(
                                    pp[:, 0:CAP], lhsT=wt[:, kc, 128 * c:128 * (c + 1)],
                                    rhs=xeT[:, kc, :], start=(kc == 0), stop=(kc == 7)),
                                     reads=[("ring", wslot, kc // 4), ("xeT", b)], writes=[pres])
                        k2 = ei % 2
                        ei += 1
                        r, sg, uc = rs[k2], sgs[k2], ucs[k2]
                        P.op("act", lambda E, pg=pg, r=r, e=e, fc=fc: E.activation(
                            out=r[:], in_=pg[:, 0:CAP], func=AF.Relu, bias=bgu[:, e, fc:fc + 1], scale=-1.0),
                             reads=[pgres, "bgu"], writes=[("r", k2)])
                        P.op("act", lambda E, r=r, sg=sg: E.activation(out=sg[:], in_=r[:], func=AF.Sigmoid, bias=c119[:],
                                                                       scale=-1.702),
                             reads=[("r", k2), "c119"], writes=[("sg", k2)])
                        P.op("dve", lambda E, r=r, sg=sg: E.scalar_tensor_tensor(
                            out=r[:], in0=r[:], scalar=7.0, in1=sg[:], op0=ALU.subtract, op1=ALU.mult),
                             reads=[("r", k2), ("sg", k2)], writes=[("r", k2)])
                        P.op("dve", lambda E, pu=pu, uc=uc, e=e, fc=fc: E.tensor_scalar(
                            out=uc[:], in0=pu[:, 0:CAP], scalar1=bgu[:, e, 8 + fc:9 + fc], scalar2=8.0, op0=ALU.add,
                            op1=ALU.min), reads=[pures, "bgu"], writes=[("uc", k2)])
                        P.op("dve", lambda E, r=r, uc=uc, fc=fc, act_e=act_e: E.scalar_tensor_tensor(
                            out=act_e[:, fc, :], in0=uc[:], scalar=-6.0, in1=r[:], op0=ALU.max, op1=ALU.mult),
                             reads=[("r", k2), ("uc", k2)], writes=[("act_e", b, fc)])
                else:
                    half = j
                    for jj in range(CAP // 128):
                        pp, pres = ps_d[di % 2], ("psd", di % 2)
                        di += 1
                        for fc in range(8):
                            P.op("pe", lambda E, pp=pp, rg=rg, fc=fc, jj=jj, act_e=act_e: E.matmul(
                                pp[:], lhsT=act_e[:, fc, 128 * jj:128 * (jj + 1)], rhs=rg[:, fc, :],
                                start=(fc == 0), stop=(fc == 7)),
                                 reads=[("ring", slot, fc // 4), ("act_e", b, fc)], writes=[pres])
                        yt = yts[jj]
                        P.op("act", lambda E, pp=pp, yt=yt, half=half: E.copy(out=yt[:, 512 * half:512 * (half + 1)],
                                                                              in_=pp[:]),
                             reads=[pres], writes=[("yt", jj, half)])
                        if half == 1:
                            P.dma("sp", ys[e * CAP + 128 * jj:e * CAP + 128 * (jj + 1), :], yt[:],
                                  reads=[("yt", jj, 0), ("yt", jj, 1)], key=("ys", jj))
            P.emit()

        with ExitStack() as sR:
            TR = lambda name, shape, dt: sR.enter_context(nc.sbuf_tensor("sb_" + name, list(shape), dt))
            x1 = TR("x1b", [128, NT, D], F32)
            g_b = TR("g_b3", [128, D], F32)
            junk = TR("junk3", [128, D], F32)
            ssq = TR("ssq3", [128, NT], F32)
            rstd = TR("rstd3", [128, NT], F32)
            hns = [TR("hnc%d" % i, [128, D], BF16) for i in range(4)]
            with ExitStack() as sD:
                TD = lambda name, shape, dt: sD.enter_context(nc.sbuf_tensor("sb_" + name, list(shape), dt))
                hn3T = TD("hn3T", [128, 8, 2048], BF16)
                wpg = TD("wpg", [128, 8, D], BF16)
                wpp = TD("wpp", [128, 2, D], BF16)
                gfin = TD("gfin", [128, D], F32)
                pts = [TD("pt%d" % i, [128, 256], BF16) for i in range(NT)]
                pTs = [TD("pT%d" % i, [128, 2, 128], BF16) for i in range(2)]
                sgs = [TD("sgD%d" % i, [128, 512], F32) for i in range(2)]
                outs = [TD("outD%d" % i, [128, D], F32) for i in range(2)]
                ssq2 = TD("ssqD", [128, NT], F32)
                rstd2 = TD("rstdD", [128, NT], F32)
                ps_tr = [sD.enter_context(nc.psum_tensor("pstrD%d" % i, [128, 8, 128], BF16)) for i in range(3)]
                ps_pt = sD.enter_context(nc.psum_tensor("pspt", [128, 2, 128], BF16))
                ps_g = [sD.enter_context(nc.psum_tensor("psDg%d" % i, [128, 512], F32)) for i in range(2)]
                ps_p = [sD.enter_context(nc.psum_tensor("psDp%d" % i, [128, 512], F32)) for i in range(2)]
                P.dma("sp", g_b[:], g_ple_in[:, :], writes=["gains"], key=P.ckey())
                P.dma("sp", gfin[:], g_fin_in[:, :], writes=["gfin"], key=P.ckey())
                P.dma("pool", wpg[:], w_pg_v, writes=["wpg"], key="wpg")
                P.dma("pool", wpp[:], w_pp_v, writes=["wpp"], key="wpp")
                for i in range(NT):
                    P.dma("pool", pts[i][:], p_in[128 * i:128 * (i + 1), :], writes=[("pt", i)], key=("pt", i % 2))
                yks = [TD("yk%d" % i, [128, D], F32) for i in range(4)]
                for i in range(4):
                    P.op("pool", lambda E, i=i: E.memset(yks[i][:], 0.0), writes=[("yk", i)])
                for i in range(NT):
                    P.dma("sp", x1[:, i, :], x1_spill[:, D * i:D * (i + 1)], writes=[("x1", i)], key=("x1", i % 4))
                for i in range(NT):
                    for k in range(4):
                        q = (4 * i + k) % 4
                        P.op("pool", lambda E, i=i, k=k, q=q: E.indirect_dma_start(
                            out=yks[q][:], out_offset=None, in_=ys[:, :],
                            in_offset=bass.IndirectOffsetOnAxis(ap=slot_i32[:, i, k:k + 1], axis=0),
                            bounds_check=P.bc_reg(E, NE * CAP - 1), oob_is_err=False),
                             reads=[], writes=[("yk", q)], dma_key=("yk", q))
                        P.op("dve", lambda E, i=i, k=k, q=q: E.scalar_tensor_tensor(
                            out=x1[:, i, :], in0=yks[q][:], scalar=g4n[:, i, k:k + 1], in1=x1[:, i, :],
                            op0=ALU.mult, op1=ALU.add), reads=[("yk", q), ("x1", i)], writes=[("x1", i)])
                if debug:
                    P.emit()
                if debug:
                    P.dma("sp", dbg["x2"].rearrange("p (c t) -> p c t", c=NT), x1[:], key="dbg")
                    P.emit()

                rms_tiles(lambda i: (x1[:, i, :], ("x1", i)), NT, g_b, hn3T, 0, "D", None,
                          (junk, ssq, rstd), hns, ps_tr)
                pi = 0
                for i in range(NT):
                    b = i % 2
                    for c in range(2):
                        P.op("pe", lambda E, i=i, c=c: E.transpose(ps_pt[:, c, :], pts[i][:, 128 * c:128 * (c + 1)], ident[:]),
                             reads=[("pt", i), "ident"], writes=["pspt"])
                    P.op("act", lambda E, b=b: E.copy(out=pTs[b][:], in_=ps_pt[:]), reads=["pspt"], writes=[("pT", b)])
                    for half in range(2):
                        k2 = pi % 2
                        pi += 1
                        pg, pgres = ps_g[k2], ("psDg", k2)
                        pq, pqres = ps_p[k2], ("psDp", k2)
                        for kc in range(8):
                            P.op("pe", lambda E, pg=pg, kc=kc, i=i, half=half: E.matmul(
                                pg[:], lhsT=hn3T[:, kc, 128 * i:128 * (i + 1)], rhs=wpg[:, kc, 512 * half:512 * (half + 1)],
                                start=(kc == 0), stop=(kc == 7)), reads=["wpg", ("T", i)], writes=[pgres])
                        for c in range(2):
                            P.op("pe", lambda E, pq=pq, c=c, b=b, half=half: E.matmul(
                                pq[:], lhsT=pTs[b][:, c, :], rhs=wpp[:, c, 512 * half:512 * (half + 1)],
                                start=(c == 0), stop=(c == 1)), reads=["wpp", ("pT", b)], writes=[pqres])
                        sg = sgs[k2]
                        P.op("act", lambda E, pg=pg, sg=sg: E.activation(out=sg[:], in_=pg[:], func=AF.Sigmoid),
                             reads=[pgres], writes=[("sgD", k2)])
                        P.op("dve", lambda E, pq=pq, sg=sg: E.tensor_tensor(out=sg[:], in0=sg[:], in1=pq[:], op=ALU.mult),
                             reads=[pqres, ("sgD", k2)], writes=[("sgD", k2)])
                        P.op("dve", lambda E, sg=sg, i=i, half=half: E.tensor_tensor(
                            out=x1[:, i, 512 * half:512 * (half + 1)], in0=x1[:, i, 512 * half:512 * (half + 1)],
                            in1=sg[:], op=ALU.add), reads=[("sgD", k2), ("x1", i)], writes=[("x1", i)])
                    ob = outs[b]
                    P.op("act", lambda E, i=i: E.activation(out=junk[:], in_=x1[:, i, :], func=AF.Square,
                                                            accum_out=ssq2[:, i:i + 1]),
                         reads=[("x1", i)], writes=["Djunk2", ("ssqD", i)])
                    P.op("act", lambda E, i=i: E.activation(out=rstd2[:, i:i + 1], in_=ssq2[:, i:i + 1], func=AF.Sqrt,
                                                            bias=epsb[:], scale=1.0 / D),
                         reads=[("ssqD", i), "epsb"], writes=[("rstdD", i)])
                    P.op("dve", lambda E, i=i: E.reciprocal(out=rstd2[:, i:i + 1], in_=rstd2[:, i:i + 1]),
                         reads=[("rstdD", i)], writes=[("rstdD", i)])
                    P.op("dve", lambda E, i=i, ob=ob: E.scalar_tensor_tensor(
                        out=ob[:], in0=x1[:, i, :], scalar=rstd2[:, i:i + 1], in1=gfin[:], op0=ALU.mult, op1=ALU.mult),
                         reads=[("x1", i), ("rstdD", i), "gfin"], writes=[("outD", b)])
                    P.dma("sp", y[128 * i:128 * (i + 1), :], ob[:], reads=[("outD", b)], key=("outD", b))
                P.emit()
    return nc


def _t5_bucket_np(dist):
    n = np.maximum(dist, 1).astype(np.float32)
    large = 16 + (np.log(n / 16) / math.log(2048 / 16) * 16).astype(np.int32)
    large = np.minimum(large, 31)
    return np.where(dist < 16, dist, large)


def _bias_tables(rel_bias, first_half):
    ki = np.arange(128)[:, None]
    qi = np.arange(128)[None, :]
    btab = np.full((128, 3, 8, 256), NEG, np.float32)
    for br, (window, dil) in enumerate(BRANCHES):
        relp = 128 + qi - ki
        idxp = _t5_bucket_np(np.maximum(relp, 0) * dil)
        relc = qi - ki
        idxc = _t5_bucket_np(np.maximum(relc, 0) * dil)
        for h in range(8):
            bp = rel_bias[idxp, h]
            bc = rel_bias[idxc, h]
            btab[:, br, h, 128:256] = np.where((relp >= 0) & (relp <= 128), bp, NEG)
            btab[:, br, h, 0:128] = np.where((relc >= 0) & (relc <= 128), bc, NEG)
    bhalo = btab[:, :, :, 128:256].copy()
    if first_half:
        bhalo[:] = NEG
    return btab.reshape(128, -1), np.ascontiguousarray(bhalo).reshape(128, -1)


_NC_CACHE = {}


def _prepare_in_maps(inputs):
    f = lambda a: np.ascontiguousarray(np.asarray(a, dtype=np.float32))
    x = f(inputs["x"])
    p = f(inputs["p"])[0]
    rb = f(inputs["rel_bias"])
    bc = lambda v: np.ascontiguousarray(np.broadcast_to(f(v).reshape(1, -1), (128, f(v).size)))
    shared = {
        "ident": np.eye(128, dtype=np.float32),
        "ltri": np.triu(np.ones((128, 128), np.float32), 1),
        "iota4": np.ascontiguousarray(np.broadcast_to(np.tile(np.arange(NE, dtype=np.float32), 4)[None, :], (128, 4 * NE))),
        "ebase": np.ascontiguousarray(np.broadcast_to((np.arange(NE, dtype=np.float32) * CAP)[None, :], (128, NE))),
        "g_mix_b": bc(inputs["g_mix"][0]),
        "g_ffn_b": bc(inputs["g_ffn"][0]),
        "g_ple_b": bc(inputs["g_ple"][0]),
        "g_fin_b": bc(inputs["g_final"]),
        "w_in": f(inputs["w_in"])[0],
        "w_pool": f(inputs["w_pool"])[0],
        "pscale": np.ascontiguousarray(f(inputs["pool_scale"])[0].reshape(4, 128).T),
        "w_out": f(inputs["w_out"])[0],
        "w_router": f(inputs["w_router"])[0],
        "b_router_b": bc(inputs["b_router"][0]),
        "w_gate_up": f(inputs["w_gate_up"])[0],
        "bgu": np.ascontiguousarray(f(inputs["b_gate_up"])[0].reshape(NE, 16, 128).transpose(2, 0, 1)).reshape(128, -1),
        "w_down": f(inputs["w_down"])[0],
        "b_down": f(inputs["b_down"])[0],
        "w_ple_gate": f(inputs["w_ple_gate"])[0],
        "w_ple_proj": f(inputs["w_ple_proj"])[0],
    }
    tabs = {fh: _bias_tables(rb, fh) for fh in (True, False)}
    in_maps = []
    for c in range(NCORES):
        b, half = c // 2, c % 2
        base = half * S_OWN
        xh = np.zeros((4096, D), np.float32)
        if half == 1:
            xh[0:2048] = x[b, 0:2048]
        xh[2048:4096] = x[b, base:base + S_OWN]
        pos = base + np.arange(16)
        invcnt = np.stack([1.0 / np.minimum(pos + 1, w) for w in (2, 4, 8, 16)]).astype(np.float32)
        m = dict(shared)
        m["xh"] = xh
        m["p"] = np.ascontiguousarray(p[b, base:base + S_OWN])
        m["btab"], m["bhalo"] = tabs[half == 0]
        m["invcnt"] = np.ascontiguousarray(np.broadcast_to(invcnt.reshape(1, -1), (128, 64)))
        in_maps.append(m)
    return in_maps


def kernel(**inputs):
    in_maps = _prepare_in_maps(inputs)
    if "nc" not in _NC_CACHE:
        _NC_CACHE["nc"] = build_program(debug=False)
    nc = _NC_CACHE["nc"]
    res = run_bass_kernel_spmd(nc, in_maps, core_ids=list(range(NCORES)))
    out = np.zeros((4, 4096, D), np.float32)
    for c in range(NCORES):
        b, half = c // 2, c % 2
        out[b, half * S_OWN:(half + 1) * S_OWN] = np.asarray(res.results[c]["y"], dtype=np.float32)
    return out
```

```python
import math
from contextlib import ExitStack

import numpy as np
import concourse.bass as bass
import concourse.mybir as mybir
from concourse.bass_utils import run_bass_kernel_spmd

F32 = mybir.dt.float32
BF16 = mybir.dt.bfloat16
AF = mybir.ActivationFunctionType
ALU = mybir.AluOpType

NCORES = 8
D = 1024
S_OWN = 2048
NT = 16
NE = 32
NEG = -1e30
CAP = 384
BIGSLOT = 1.0e6
NCONV = 9
OPT_ACT_RECIP = True
OPT_ATT_NEW = False
EPS = 1e-6
BRANCHES = ((128, 1), (512, 4), (2048, 16))


class Op:
    __slots__ = ("eng", "fn", "deps", "signal", "count", "dma_sem", "is_dma")

    def __init__(self, eng, fn, dma_sem=None):
        self.eng = eng
        self.fn = fn
        self.deps = []
        self.signal = False
        self.count = 0
        self.dma_sem = dma_sem
        self.is_dma = dma_sem is not None


class Prog:
    ENGS = ("pe", "act", "dve", "pool", "sp")

    def __init__(self, nc, stack):
        self.nc = nc
        self.stack = stack
        self.esem = {e: stack.enter_context(nc.semaphore("sem_" + e)) for e in self.ENGS}
        self.ecount = {e: 0 for e in self.ENGS}
        self.dsem = {}
        self.dcount = {}
        self.waited = {e: {} for e in self.ENGS}
        self.begin()

    def bc_reg(self, E, val):
        if self._bc is None:
            self._bc = E.to_reg(val)
        return self._bc

    def begin(self):
        self._bc = None
        self.ops = []
        self.res_w = {}
        self.res_r = {}

    def _dma_sem(self, key):
        if key not in self.dsem:
            self.dsem[key] = self.stack.enter_context(self.nc.semaphore("dsem_%d" % len(self.dsem)))
            self.dcount[key] = 0
        return key

    def op(self, eng, fn, reads=(), writes=(), dma_key=None):
        o = Op(eng, fn, self._dma_sem(dma_key) if dma_key is not None else None)
        if dma_key is not None:
            writes = list(writes) + [("__key", dma_key)]
        deps = {}
        for r in reads:
            w = self.res_w.get(r)
            if w is not None:
                deps[id(w)] = w
        for r in writes:
            w = self.res_w.get(r)
            if w is not None:
                deps[id(w)] = w
            for rd in self.res_r.get(r, ()):
                deps[id(rd)] = rd
        for d in deps.values():
            if d is o:
                continue
            if d.eng == "pe" and eng == "pe" and not d.is_dma and not o.is_dma:
                continue
            o.deps.append(d)
            d.signal = True
        for r in reads:
            lst = self.res_r.setdefault(r, [])
            if not o.is_dma:
                lst[:] = [x for x in lst if x.is_dma or x.eng != eng]
            lst.append(o)
        for r in writes:
            self.res_w[r] = o
            self.res_r[r] = []
        self.ops.append(o)
        return o

    def dma(self, eng, out, in_, reads=(), writes=(), key=None):
        assert key is not None
        return self.op(eng, lambda E: E.dma_start(out=out, in_=in_), reads, writes, dma_key=key)

    def ckey(self):
        self._ck = (getattr(self, "_ck", 0) + 1) % 6
        return ("const", self._ck)

    def emit(self, defer_cv=False):
        nc = self.nc
        last = {}
        for o in self.ops:
            if not o.is_dma:
                last[o.eng] = o
        for o in last.values():
            o.signal = True
        for o in self.ops:
            if o.is_dma:
                o.signal = True
                self.dcount[o.dma_sem] += 16
                o.count = self.dcount[o.dma_sem]
            elif o.signal:
                self.ecount[o.eng] += 1
                o.count = self.ecount[o.eng]
        by_eng = {e: [o for o in self.ops if o.eng == e] for e in self.ENGS}
        final_e = dict(self.ecount)
        final_d = dict(self.dcount)

        def run(ename, E):
            waited = self.waited[ename]
            for o in by_eng[ename]:
                need = {}
                for d in o.deps:
                    if d.is_dma:
                        k = ("d", d.dma_sem)
                    else:
                        k = ("e", d.eng)
                    if d.count > need.get(k, 0):
                        need[k] = d.count
                for k, v in need.items():
                    if waited.get(k, 0) < v:
                        sem = self.dsem[k[1]] if k[0] == "d" else self.esem[k[1]]
                        E.wait_ge(sem, v)
                        waited[k] = v
                ins = o.fn(E)
                if o.signal:
                    if o.is_dma:
                        ins.then_inc(self.dsem[o.dma_sem], 16)
                    else:
                        ins.then_inc(self.esem[o.eng], 1)
            for e2, v in final_e.items():
                if v > 0 and waited.get(("e", e2), 0) < v:
                    E.wait_ge(self.esem[e2], v)
                    waited[("e", e2)] = v
            for k2, v in final_d.items():
                if defer_cv and isinstance(k2, tuple) and k2[0] == "cv":
                    continue
                if v > 0 and waited.get(("d", k2), 0) < v:
                    E.wait_ge(self.dsem[k2], v)
                    waited[("d", k2)] = v

        with nc.Block() as block:
            @block.tensor
            def _(E):
                run("pe", E)

            @block.scalar
            def _(E):
                run("act", E)

            @block.vector
            def _(E):
                run("dve", E)

            @block.gpsimd
            def _(E):
                run("pool", E)

            @block.sync
            def _(E):
                run("sp", E)
        self.begin()


def build_program(debug=False):
    nc = bass.Bass("TRN2", target_bir_lowering=False)

    def din(name, shape, dt=F32):
        return nc.dram_tensor(name, list(shape), dt, kind="ExternalInput").ap()

    xh = din("xh", [4096, D])
    p_in = din("p", [S_OWN, 256])
    ident_in = din("ident", [128, 128])
    btab_in = din("btab", [128, 3 * 8 * 256])
    bhalo_in = din("bhalo", [128, 3 * 8 * 128])
    invcnt_in = din("invcnt", [128, 4 * 16])
    g_mix_in = din("g_mix_b", [128, D])
    g_ffn_in = din("g_ffn_b", [128, D])
    g_ple_in = din("g_ple_b", [128, D])
    g_fin_in = din("g_fin_b", [128, D])
    w_in = din("w_in", [D, 2048])
    w_pool = din("w_pool", [4, 128, 128])
    pscale_in = din("pscale", [128, 4])
    w_out = din("w_out", [D, D])
    w_router = din("w_router", [D, NE])
    brouter_in = din("b_router_b", [128, NE])
    w_gu = din("w_gate_up", [NE, D, 2 * D])
    bgu_in = din("bgu", [128, NE * 16])
    w_down = din("w_down", [NE, D, D])
    b_down = din("b_down", [NE, D])
    w_pg = din("w_ple_gate", [D, D])
    w_pp = din("w_ple_proj", [256, D])
    ltri_in = din("ltri", [128, 128])
    iota4_in = din("iota4", [128, 4 * NE])
    ebase_in = din("ebase", [128, NE])
    y = nc.dram_tensor("y", [S_OWN, D], F32, kind="ExternalOutput").ap()
    xs = nc.dram_tensor("xs_scratch", [NE * CAP, D], BF16).ap()
    ys = nc.dram_tensor("ys_scratch", [NE * CAP, D], F32).ap()
    x1_spill = nc.dram_tensor("x1_spill", [128, NT * D], F32).ap()
    wgu_bf = nc.dram_tensor("wgu_bf16", [NCONV, D, 2 * D], BF16).ap()
    wdn_bf = nc.dram_tensor("wdn_bf16", [NCONV, D, D], BF16).ap()
    dbg = {}
    if debug:
        dbg["mixT"] = nc.dram_tensor("dbg_mixT", [128, 8 * 2048], BF16, kind="ExternalOutput").ap()
        dbg["mixA"] = nc.dram_tensor("dbg_mixA", [64, 8 * 2048], BF16, kind="ExternalOutput").ap()
        dbg["x1"] = nc.dram_tensor("dbg_x1", [128, NT * D], F32, kind="ExternalOutput").ap()
        dbg["gates"] = nc.dram_tensor("dbg_gates", [128, NT * NE], F32, kind="ExternalOutput").ap()
        dbg["x2"] = nc.dram_tensor("dbg_x2", [128, NT * D], F32, kind="ExternalOutput").ap()

    w_in_v = w_in.rearrange("(kc p) n -> p kc n", p=128)
    w_out_v = w_out.rearrange("(kc p) n -> p kc n", p=128)
    w_pg_v = w_pg.rearrange("(kc p) n -> p kc n", p=128)
    w_pp_v = w_pp.rearrange("(kc p) n -> p kc n", p=128)
    w_router_v = w_router.rearrange("(kc p) n -> p kc n", p=128)

    with ExitStack() as stack:
        P = Prog(nc, stack)
        T = lambda name, shape, dt: stack.enter_context(nc.sbuf_tensor("sb_" + name, list(shape), dt))
        ident = T("ident_sb", [128, 128], BF16)
        ones_bf = T("ones_bf", [128, 64], BF16)
        sAB = ExitStack()
        TAB = lambda name, shape, dt: sAB.enter_context(nc.sbuf_tensor("sb_" + name, list(shape), dt))
        epsb = T("epsb", [128, 1], F32)
        c119 = T("c119", [128, 1], F32)
        slot_i32 = T("slot_i32", [128, NT, 4], mybir.dt.int32)
        g4n = T("g4n", [128, NT, 4], F32)
        actT = TAB("actT", [128, 4, 2048], BF16)
        mixA = TAB("mixA", [64, 8, 2048], BF16)

        def rms_tiles(x_ap_of, ntiles, g_b, dstT, dst_col0, pfx, xt_res, stats, hn_bufs, ps_tr,
                      pre=None, post=None):
            junk, ssq, rstd = stats
            for i in range(ntiles):
                if pre is not None:
                    pre(i)
                xa, xres = x_ap_of(i)
                P.op("act", lambda E, xa=xa, i=i: E.activation(out=junk[:], in_=xa, func=AF.Square,
                                                                accum_out=ssq[:, i:i + 1]),
                     reads=[xres], writes=[pfx + "junk", (pfx + "ssq", i)])
                P.op("act", lambda E, i=i: E.activation(out=rstd[:, i:i + 1], in_=ssq[:, i:i + 1], func=AF.Sqrt,
                                                        bias=epsb[:], scale=1.0 / D),
                     reads=[(pfx + "ssq", i), "epsb"], writes=[(pfx + "rstd", i)])
                P.op("dve", lambda E, i=i: E.reciprocal(out=rstd[:, i:i + 1], in_=rstd[:, i:i + 1]),
                     reads=[(pfx + "rstd", i)], writes=[(pfx + "rstd", i)])
                hb = hn_bufs[i % len(hn_bufs)]
                hres = (pfx + "hn", i % len(hn_bufs))
                P.op("dve", lambda E, xa=xa, i=i, hb=hb: E.scalar_tensor_tensor(
                    out=hb[:], in0=xa, scalar=rstd[:, i:i + 1], in1=g_b[:], op0=ALU.mult, op1=ALU.mult),
                     reads=[xres, (pfx + "rstd", i), "gains"], writes=[hres])
                pt = ps_tr[i % len(ps_tr)]
                pres = (pfx + "pstr", i % len(ps_tr))
                for kc in range(8):
                    P.op("pe", lambda E, kc=kc, hb=hb, pt=pt: E.transpose(pt[:, kc, :], hb[:, kc * 128:(kc + 1) * 128],
                                                                          ident[:]),
                         reads=[hres, "ident"], writes=[pres])
                c0 = dst_col0 + 128 * i
                P.op("act", lambda E, pt=pt, c0=c0: E.copy(out=dstT[:, :, c0:c0 + 128], in_=pt[:]),
                     reads=[pres], writes=[("T", c0 // 128)])
                if post is not None:
                    post(i, hb, hres)

        P.dma("pool", ident[:], ident_in[:, :], writes=["ident"], key=P.ckey())
        P.op("pool", lambda E: E.memset(ones_bf[:], 1.0), writes=["ones"])
        P.op("pool", lambda E: E.memset(epsb[:], EPS), writes=["epsb"])
        P.op("pool", lambda E: E.memset(c119[:], 7.0 * 1.702), writes=["c119"])
        zero_jobs = list(range(NE * CAP // 1024))

        with ExitStack() as sA:
            TA = lambda name, shape, dt: sA.enter_context(nc.sbuf_tensor("sb_" + name, list(shape), dt))
            hnT = TA("hnT", [128, 8, 4096], BF16)

            with ExitStack() as s1:
                T1 = lambda name, shape, dt: s1.enter_context(nc.sbuf_tensor("sb_" + name, list(shape), dt))
                g_b = T1("g_b", [128, D], F32)
                NXT = 4
                xts = [T1("xt%d" % i, [128, D], F32) for i in range(NXT)]
                hns = [T1("hn%d" % i, [128, D], BF16) for i in range(4)]
                junk = T1("junk", [128, D], F32)
                ssq = T1("ssq", [128, 32], F32)
                rstd = T1("rstd", [128, 32], F32)
                ps_tr = [s1.enter_context(nc.psum_tensor("pstrA%d" % i, [128, 8, 128], BF16)) for i in range(4)]
                P.dma("sp", g_b[:], g_mix_in[:, :], writes=["gains"], key=P.ckey())
                def ld(i):
                    P.dma("sp", xts[i % NXT][:], xh[128 * i:128 * (i + 1), :], writes=[("xt", i % NXT)],
                          key=("xt", i % NXT))

                zt = T1("zeros", [128, 8192], BF16)
                P.op("pool", lambda E: E.memset(zt[:], 0.0), writes=["zt"])

                def pre(i):
                    if i == 0:
                        ld(0)
                        ld(1)
                        ld(2)
                    if i + 3 < 32:
                        ld(i + 3)
                    if i >= 2 and zero_jobs:
                        c = zero_jobs.pop()
                        P.dma("sp", xs[1024 * c:1024 * (c + 1), :].rearrange("(p r) d -> p (r d)", p=128), zt[:],
                              reads=["zt"], key=("zx", c % 4))
                rms_tiles(lambda i: (xts[i % NXT][:], ("xt", i % NXT)), 32, g_b, hnT, 0, "A1", None,
                          (junk, ssq, rstd), hns, ps_tr, pre=pre)
                P.emit()

            with ExitStack() as s2:
                T2 = lambda name, shape, dt: s2.enter_context(nc.sbuf_tensor("sb_" + name, list(shape), dt))
                wpc = T2("wpc", [128, 8, 512], BF16)
                wpl = T2("wpl", [128, 4, 128], BF16)
                psc = T2("psc", [128, 4], F32)
                icn = T2("icn", [128, 4, 16], F32)
                u = T2("u", [128, 2064], F32)
                sa = T2("sa", [128, 2064], F32)
                sb = T2("sb", [128, 2064], F32)
                pooled = T2("pooled", [128, 2048], BF16)
                t16 = T2("t16", [128, 16], F32)
                ps = [s2.enter_context(nc.psum_tensor("psA2_%d" % i, [128, 512], F32)) for i in range(2)]
                P.dma("pool", wpc[:], w_in_v[:, :, 0:512], writes=["wpc"], key="wpc")
                P.dma("pool", wpl[:], w_pool.rearrange("g c d -> c g d"), writes=["wpl"], key="wpl")
                P.dma("sp", psc[:], pscale_in[:, :], writes=["psc"], key=P.ckey())
                P.dma("sp", icn[:], invcnt_in.rearrange("p (g t) -> p g t", g=4), writes=["icn"], key=P.ckey())
                pi = 0
                for g in range(4):
                    w = (2, 4, 8, 16)[g]
                    for blk in range(5):
                        pp = ps[pi % 2]
                        pres = ("psA2", pi % 2)
                        pi += 1
                        if blk == 0:
                            c0, n, o0 = 2032, 16, 0
                        else:
                            c0, n, o0 = 2048 + 512 * (blk - 1), 512, 16 + 512 * (blk - 1)
                        for kc in range(8):
                            P.op("pe", lambda E, pp=pp, kc=kc, g=g, c0=c0, n=n: E.matmul(
                                pp[:, 0:n], lhsT=wpc[:, kc, g * 128:(g + 1) * 128], rhs=hnT[:, kc, c0:c0 + n],
                                start=(kc == 0), stop=(kc == 7)), reads=["wpc", "hnT"], writes=[pres])
                        P.op("act", lambda E, pp=pp, n=n, o0=o0: E.copy(out=u[:, o0:o0 + n], in_=pp[:, 0:n]),
                             reads=[pres], writes=["u"])
                    src, srcres = u, "u"
                    sh = 1
                    bufs = [(sa, "sa"), (sb, "sb")]
                    bi = 0
                    while sh < w:
                        dst, dres = bufs[bi % 2]
                        bi += 1
                        P.op("dve", lambda E, dst=dst, src=src, sh=sh: E.tensor_tensor(
                            out=dst[:, sh:2064], in0=src[:, sh:2064], in1=src[:, 0:2064 - sh], op=ALU.add),
                             reads=[srcres], writes=[dres])
                        src, srcres = dst, dres
                        sh *= 2
                    P.op("dve", lambda E, src=src, w=w: E.scalar_tensor_tensor(
                        out=pooled[:], in0=src[:, 16:2064], scalar=1.0 / w, in1=u[:, 16:2064],
                        op0=ALU.mult, op1=ALU.subtract), reads=[srcres, "u"], writes=["pooled"])
                    P.op("dve", lambda E, src=src, g=g: E.tensor_tensor(
                        out=t16[:], in0=src[:, 16:32], in1=icn[:, g, :], op=ALU.mult),
                         reads=[srcres, "icn"], writes=["t16"])
                    P.op("dve", lambda E: E.tensor_tensor(out=pooled[:, 0:16], in0=t16[:], in1=u[:, 16:32],
                                                          op=ALU.subtract),
                         reads=["t16", "u", "pooled"], writes=["pooled"])
                    for blk in range(4):
                        pp = ps[pi % 2]
                        pres = ("psA2", pi % 2)
                        pi += 1
                        P.op("pe", lambda E, pp=pp, g=g, blk=blk: E.matmul(
                            pp[:], lhsT=wpl[:, g, :], rhs=pooled[:, 512 * blk:512 * (blk + 1)], start=True, stop=True),
                             reads=["wpl", "pooled"], writes=[pres])
                        P.op("dve", lambda E, pp=pp, g=g, blk=blk: E.tensor_scalar(
                            out=actT[:, g, 512 * blk:512 * (blk + 1)], in0=pp[:], scalar1=psc[:, g:g + 1],
                            scalar2=None, op0=ALU.mult), reads=[pres, "psc"], writes=[("mixT", g, blk)])
                P.emit()

            with ExitStack() as s3:
                T3 = lambda name, shape, dt: s3.enter_context(nc.sbuf_tensor("sb_" + name, list(shape), dt))
                btabs = [T3("btab%d" % i, [128, 3, 2, 256], BF16) for i in range(2)]
                bhalos = [T3("bhalo%d" % i, [128, 3, 2, 128], BF16) for i in range(2)]
                wqs = [T3("wq%d" % i, [128, 8, 128], BF16) for i in range(2)]
                wks = [T3("wk%d" % i, [128, 8, 128], BF16) for i in range(2)]
                wvs = [T3("wv%d" % i, [128, 8, 128], BF16) for i in range(2)]
                QT = T3("QTz", [128, 2, 2048], BF16)
                KT = T3("KT", [128, 4096], BF16)
                VT = T3("VT", [128, 4096], BF16)
                V = T3("V", [128, 69, 2, 65], BF16)
                acc = T3("acc", [65, 2, 2048], F32)
                onesf = T3("onesf", [65, 64], F32)
                NSB, NOB = 4, 3
                Pb = [T3("Pb%d" % i, [128, 2, 256], BF16) for i in range(NSB)]
                ps_tr = s3.enter_context(nc.psum_tensor("pstrV", [128, 8, 128], BF16))
                ps_s = [s3.enter_context(nc.psum_tensor("pss%d" % i, [128, 2, 256], F32)) for i in range(NSB)]
                ps_o = [s3.enter_context(nc.psum_tensor("pso%d" % i, [128, 2, 256], F32)) for i in range(NOB)]
                ps_pr = [t[:].rearrange("p h q -> p (h q)") for t in ps_o]
                btab_v = btab_in.rearrange("p (b h q) -> p b h q", b=3, h=8)
                bhalo_v = bhalo_in.rearrange("p (b h q) -> p b h q", b=3, h=8)
                P.op("dve", lambda E: E.memset(V[:], 1.0), writes=["V"])
                P.op("dve", lambda E: E.memset(QT[:], 0.0), writes=["QT"])
                P.op("dve", lambda E: E.memset(onesf[:], 1.0), writes=["onesf"])

                def tok_slice(start, dil, n=128):
                    return slice(start, start + (n - 1) * dil + 1, dil)

                ktiles = []
                for j in range(15, 32):
                    ks = tok_slice(128 * j, 1)
                    if j == 15:
                        ktiles.append((0, ks, tok_slice(0, 1), 128, "halo", 0))
                    elif j == 31:
                        ktiles.append((0, ks, tok_slice(128 * 15, 1), 128, "tab", 0))
                    else:
                        ktiles.append((0, ks, tok_slice(128 * (j - 16), 1, 256), 256, "tab", 0))
                for n in range(3, 8):
                    for r in range(4):
                        ks = tok_slice(512 * n + r, 4)
                        if n == 3:
                            ktiles.append((1, ks, tok_slice(r, 4), 128, "halo", 0))
                        elif n == 7:
                            ktiles.append((1, ks, tok_slice(512 * 3 + r, 4), 128, "tab", 0))
                        else:
                            ktiles.append((1, ks, tok_slice(512 * (n - 4) + r, 4, 256), 256, "tab", 0))
                for n in range(2):
                    for r in range(16):
                        ks = tok_slice(2048 * n + r, 16)
                        ktiles.append((2, ks, tok_slice(r, 16), 128, "halo" if n == 0 else "tab", 0))
                assert len(ktiles) == 69

                def load_hp(hp):
                    cq = 512 + 128 * hp
                    w = hp % 2
                    P.dma("pool", wqs[w][:], w_in_v[:, :, cq:cq + 128], writes=[("wq", w)], key=("wq", w))
                    P.dma("pool", wks[w][:], w_in_v[:, :, 512 + cq:512 + cq + 128], writes=[("wk", w)], key=("wk", w))
                    P.dma("pool", wvs[w][:], w_in_v[:, :, 1024 + cq:1024 + cq + 128], writes=[("wv", w)], key=("wv", w))
                    P.dma("pool", btabs[w][:], btab_v[:, :, 2 * hp:2 * hp + 2, :], writes=[("btab", w)], key=("btab", w))
                    P.dma("pool", bhalos[w][:], bhalo_v[:, :, 2 * hp:2 * hp + 2, :], writes=[("bhalo", w)],
                          key=("bhalo", w))

                conv_jobs = []
                for e in range(NCONV):
                    for r in range(8):
                        conv_jobs.append((wgu_bf[e][128 * r:128 * (r + 1), :], w_gu[e][128 * r:128 * (r + 1), :]))
                    for r in range(8):
                        conv_jobs.append((wdn_bf[e][128 * r:128 * (r + 1), :], w_down[e][128 * r:128 * (r + 1), :]))
                conv_jobs.reverse()
                conv_n = [0]

                def conv_issue():
                    if conv_jobs:
                        dst, src = conv_jobs.pop()
                        P.dma("pool", dst, src, key=("cv", conv_n[0] % 10))
                        conv_n[0] += 1

                ppi = 0
                load_hp(0)
                for hp in range(4):
                    if hp + 1 < 4:
                        load_hp(hp + 1)
                    w = hp % 2
                    wq, wk, wv, btab, bhalo = wqs[w], wks[w], wvs[w], btabs[w], bhalos[w]
                    P.op("dve", lambda E: E.memset(acc[:], 0.0), writes=["acc"])
                    for (wt, wres, dst, dres, t0, nblk, scale) in ((wq, ("wq", w), QT, "QT", 2048, 4, 0.125),
                                                                    (wk, ("wk", w), KT, "KT", 0, 8, None),
                                                                    (wv, ("wv", w), VT, "VT", 0, 8, None)):
                        for blk in range(nblk):
                            pp = ps_pr[ppi % NOB]
                            pres = ("pso", ppi % NOB)
                            ppi += 1
                            for kc in range(8):
                                P.op("pe", lambda E, pp=pp, wt=wt, kc=kc, c0=t0 + 512 * blk: E.matmul(
                                    pp[:], lhsT=wt[:, kc, :], rhs=hnT[:, kc, c0:c0 + 512], start=(kc == 0),
                                    stop=(kc == 7)), reads=[wres, "hnT"], writes=[pres])
                            if scale is not None:
                                for h2 in range(2):
                                    P.op("act", lambda E, pp=pp, dst=dst, blk=blk, scale=scale, h2=h2: E.mul(
                                        out=dst[64 * h2:64 * (h2 + 1), h2, 512 * blk:512 * (blk + 1)],
                                        in_=pp[64 * h2:64 * (h2 + 1), :], mul=scale),
                                         reads=[pres], writes=[dres])
                            else:
                                P.op("act", lambda E, pp=pp, dst=dst, blk=blk: E.copy(
                                    out=dst[:, 512 * blk:512 * (blk + 1)], in_=pp[:]), reads=[pres], writes=[dres])
                    for t0 in range(0, 69, 8):
                        nt = min(8, 69 - t0)
                        for k in range(nt):
                            P.op("pe", lambda E, k=k, sl=ktiles[t0 + k][1]: E.transpose(ps_tr[:, k, :], VT[:, sl], ident[:]),
                                 reads=["VT", "ident"], writes=["pstrV"])
                        P.op("dve", lambda E, t0=t0, nt=nt: E.tensor_copy(
                            out=V[:, t0:t0 + nt, :, 0:64],
                            in_=ps_tr[:, 0:nt, :].rearrange("p t (h d) -> p t h d", h=2)),
                             reads=["pstrV"], writes=["V"])

                    def emit_bias(ti):
                        br, ks, qs, nq, kind, c0 = ktiles[ti]
                        b = ti % 2
                        for h2 in range(2):
                            if kind == "halo":
                                bt = bhalo[:, br, h2, :]
                            else:
                                bt = btab[:, br, h2, c0:c0 + nq]
                            P.op("pe", lambda E, b=b, bt=bt, nq=nq, h2=h2: E.matmul(
                                ps_s[b][:, h2, 0:nq], lhsT=ident[:], rhs=bt, start=(h2 == 0), stop=False,
                                skip_group_check=True),
                                 reads=["ident", "btab", "bhalo"], writes=[("pss", b)])

                    def emit_QK(ti):
                        br, ks, qs, nq, kind, c0 = ktiles[ti]
                        b = ti % 2
                        for h2 in range(2):
                            r0 = 64 * h2
                            P.op("pe", lambda E, b=b, h2=h2, nq=nq, ks=ks, qs=qs, r0=r0: E.matmul(
                                ps_s[b][:, h2, 0:nq], lhsT=KT[r0:r0 + 64, ks], rhs=QT[r0:r0 + 64, qs], start=False,
                                stop=(h2 == 1), tile_position=(r0, 0), skip_group_check=True),
                                 reads=["KT", "QT"], writes=[("pss", b)])
                        P.op("act", lambda E, b=b, nq=nq: E.activation(out=Pb[b][:, :, 0:nq], in_=ps_s[b][:, :, 0:nq],
                                                                      func=AF.Exp),
                             reads=[("pss", b)], writes=[("Pb", b)])

                    def emit_PV(ti):
                        br, ks, qs, nq, kind, c0 = ktiles[ti]
                        b = ti % NSB
                        ob = ti % NOB
                        for h2 in range(2):
                            P.op("pe", lambda E, h2=h2, b=b, ob=ob, nq=nq, ti=ti: E.matmul(
                                ps_o[ob][0:65, h2, 0:nq], lhsT=V[:, ti, h2, :], rhs=Pb[b][:, h2, 0:nq], start=True,
                                stop=True), reads=["V", ("Pb", b)], writes=[("pso", ob)])
                        P.op("dve", lambda E, ob=ob, qs=qs, nq=nq: E.tensor_tensor(
                            out=acc[:, :, qs], in0=acc[:, :, qs], in1=ps_o[ob][0:65, :, 0:nq], op=ALU.add),
                             reads=[("pso", ob), "acc"], writes=["acc"])

                    def emit_S_old(ti):
                        br, ks, qs, nq, kind, c0 = ktiles[ti]
                        b = ti % NSB
                        out = ps_s[b][:, :, 0:nq]
                        P.op("pe", lambda E, out=out, ks=ks, qs=qs: E.matmul(
                            out, lhsT=KT[:, ks], rhs=QT[:, :, qs], start=True, stop=False),
                             reads=["KT", "QT"], writes=[("pss", b)])
                        if kind == "halo":
                            bt = bhalo[:, br, :, :]
                        else:
                            bt = btab[:, br, :, c0:c0 + nq]
                        P.op("pe", lambda E, out=out, bt=bt: E.matmul(
                            out, lhsT=ident[:], rhs=bt, start=False, stop=True),
                             reads=["ident", ("btab", w), ("bhalo", w)], writes=[("pss", b)])
                        P.op("act", lambda E, b=b, nq=nq: E.activation(out=Pb[b][:, :, 0:nq], in_=ps_s[b][:, :, 0:nq],
                                                                      func=AF.Exp),
                             reads=[("pss", b)], writes=[("Pb", b)])

                    if OPT_ATT_NEW:
                        emit_bias(0)
                        emit_QK(0)
                        for ti in range(len(ktiles)):
                            if ti + 1 < len(ktiles):
                                emit_bias(ti + 1)
                            emit_PV(ti)
                            if ti + 1 < len(ktiles):
                                emit_QK(ti + 1)
                    else:
                        LA = NSB - 1
                        for t in range(LA):
                            emit_S_old(t)
                        for ti in range(len(ktiles)):
                            if ti + LA < len(ktiles):
                                emit_S_old(ti + LA)
                            emit_PV(ti)
                            if ti % 2 == 0:
                                conv_issue()
                    for h2 in range(2):
                        h = 2 * hp + h2
                        if OPT_ACT_RECIP:
                            P.op("act", lambda E, h2=h2: E.activation(out=acc[64:65, h2, :], in_=acc[64:65, h2, :], func=AF.Ln),
                                 reads=["acc"], writes=["acc"])
                            P.op("act", lambda E, h2=h2: E.activation(out=acc[64:65, h2, :], in_=acc[64:65, h2, :],
                                                                      func=AF.Exp, scale=-1.0),
                                 reads=["acc"], writes=["acc"])
                        else:
                            P.op("dve", lambda E, h2=h2: E.reciprocal(out=acc[64:65, h2, :], in_=acc[64:65, h2, :]),
                                 reads=["acc"], writes=["acc"])
                        for blk in range(4):
                            pp = ps_pr[ppi % NOB]
                            pres = ("pso", ppi % NOB)
                            ppi += 1
                            P.op("pe", lambda E, pp=pp, blk=blk, h2=h2: E.matmul(
                                pp[0:64, :], lhsT=onesf[64:65, :], rhs=acc[64:65, h2, 512 * blk:512 * (blk + 1)],
                                start=True, stop=True, tile_position=(64, 0)), reads=["onesf", "acc"], writes=[pres])
                            P.op("dve", lambda E, pp=pp, blk=blk, h=h, h2=h2: E.tensor_tensor(
                                out=mixA[:, h, 512 * blk:512 * (blk + 1)], in0=acc[0:64, h2, 512 * blk:512 * (blk + 1)],
                                in1=pp[0:64, :], op=ALU.mult), reads=[pres, "acc"], writes=[("mixA", h, blk)])
                while conv_jobs:
                    conv_issue()
                P.emit(defer_cv=True)
        if debug:
            P.dma("sp", dbg["mixT"].rearrange("p (c t) -> p c t", c=8)[:, 0:4, :], actT[:], key="dbg")
            P.emit()
            P.dma("sp", dbg["mixA"].rearrange("p (c t) -> p c t", c=8), mixA[:], key="dbg")
            P.emit()

        with ExitStack() as sR:
            TR = lambda name, shape, dt: sR.enter_context(nc.sbuf_tensor("sb_" + name, list(shape), dt))
            x1 = TR("x1", [128, NT, D], F32)
            g_b = TR("g_b2", [128, D], F32)
            junk = TR("junk2", [128, D], F32)
            ssq = TR("ssq2", [128, NT], F32)
            rstd = TR("rstd2", [128, NT], F32)
            hns = [TR("hnb%d" % i, [128, D], BF16) for i in range(4)]

            with ExitStack() as sB:
                TB = lambda name, shape, dt: sB.enter_context(nc.sbuf_tensor("sb_" + name, list(shape), dt))
                wo = TB("wo", [128, 4, D], BF16)
                woA = TB("woA", [64, 8, D], BF16)
                ps = [sB.enter_context(nc.psum_tensor("psB%d" % i, [128, 512], F32)) for i in range(4)]
                P.dma("pool", wo[:], w_out_v[:, 0:4, :], writes=["wo"], key="wo")
                P.dma("pool", woA[:], w_out[512:1024, :].rearrange("(h d) n -> d h n", d=64), writes=["wo"], key="wk")
                for i in range(NT):
                    P.dma("sp", x1[:, i, :], xh[2048 + 128 * i:2048 + 128 * (i + 1), :], writes=[("x1", i)],
                          key=("x1", i % 4))
                pi = 0
                for i in range(NT):
                    for half in range(2):
                        pp = ps[pi % 4]
                        pres = ("psB", pi % 4)
                        pi += 1
                        for kc in range(4):
                            P.op("pe", lambda E, pp=pp, kc=kc, i=i, half=half: E.matmul(
                                pp[:], lhsT=actT[:, kc, 128 * i:128 * (i + 1)], rhs=wo[:, kc, 512 * half:512 * (half + 1)],
                                start=(kc == 0), stop=False), reads=["wo", "mixT"], writes=[pres])
                        for h in range(8):
                            P.op("pe", lambda E, pp=pp, h=h, i=i, half=half: E.matmul(
                                pp[:], lhsT=mixA[:, h, 128 * i:128 * (i + 1)], rhs=woA[:, h, 512 * half:512 * (half + 1)],
                                start=False, stop=(h == 7)), reads=["wo", "mixT"], writes=[pres])
                        P.op("dve", lambda E, pp=pp, i=i, half=half: E.tensor_tensor(
                            out=x1[:, i, 512 * half:512 * (half + 1)], in0=x1[:, i, 512 * half:512 * (half + 1)],
                            in1=pp[:], op=ALU.add), reads=[pres, ("x1", i)], writes=[("x1", i)])
                P.emit(defer_cv=True)
            if debug:
                P.dma("sp", dbg["x1"].rearrange("p (c t) -> p c t", c=NT), x1[:], key="dbg")
                P.emit()

            with ExitStack() as sC1:
                TC1 = lambda name, shape, dt: sC1.enter_context(nc.sbuf_tensor("sb_" + name, list(shape), dt))
                hn2T = TC1("hn2T", [128, 8, 2048], BF16)
                gates = TC1("gates", [128, NT, NE], F32)
                gates_bf = TC1("gates_bf", [128, NT, NE], BF16)
                gT = TC1("gT", [NE, NT, 128], BF16)
                bdn = TC1("bdn", [NE, D], BF16)
                wr = TC1("wr", [128, 8, NE], BF16)
                brb = TC1("brb", [128, NE], F32)
                ltri = TC1("ltri", [128, 128], BF16)
                ones128 = TC1("ones128", [128, 128], BF16)
                iota4 = TC1("iota4", [128, 4, NE], F32)
                ebase = TC1("ebase", [128, NE], F32)
                carry = TC1("carry", [128, NE], F32)
                lg = TC1("lg", [128, NE], F32)
                m8 = TC1("m8", [128, 8], F32)
                idx8 = TC1("idx8", [128, 8], mybir.dt.uint32)
                ef = TC1("ef", [128, 4], F32)
                negm = TC1("negm", [128, 1], F32)
                ex = TC1("ex", [128, NE], F32)
                msk = TC1("msk", [128, NE], F32)
                mskb = TC1("mskb", [128, NE], BF16)
                ssum = TC1("ssum", [128, 1], F32)
                posf = TC1("posf", [128, NE], F32)
                ovf = TC1("ovf", [128, NE], F32)
                oh4 = TC1("oh4", [128, 4, NE], F32)
                pr4 = TC1("pr4", [128, 4, NE], F32)
                slotf = TC1("slotf", [128, 4], F32)
                g4 = TC1("g4", [128, 4], F32)
                ps_tr = [sC1.enter_context(nc.psum_tensor("pstrC%d" % i, [128, 8, 128], BF16)) for i in range(2)]
                ps_l = sC1.enter_context(nc.psum_tensor("psl", [128, NE], F32))
                ps_pos = sC1.enter_context(nc.psum_tensor("pspos", [128, NE], F32))
                ps_cnt = sC1.enter_context(nc.psum_tensor("pscnt", [128, NE], F32))
                ps_g = sC1.enter_context(nc.psum_tensor("psgT", [NE, 128], BF16))
                ps_b = [sC1.enter_context(nc.psum_tensor("psbd%d" % i, [128, 512], F32)) for i in range(2)]
                P.dma("sp", g_b[:], g_ffn_in[:, :], writes=["gains"], key=P.ckey())
                P.dma("pool", wr[:], w_router_v, writes=["wr"], key="wr")
                P.dma("sp", brb[:], brouter_in[:, :], writes=["brb"], key=P.ckey())
                P.dma("pool", bdn[:], b_down[:, :], writes=["bdn"], key=P.ckey())
                P.dma("pool", ltri[:], ltri_in[:, :], writes=["ltri"], key=P.ckey())
                P.dma("sp", iota4[:], iota4_in.rearrange("p (k e) -> p k e", k=4), writes=["iota4"], key=P.ckey())
                P.dma("sp", ebase[:], ebase_in[:, :], writes=["ebase"], key=P.ckey())
                P.op("pool", lambda E: E.memset(ones128[:], 1.0), writes=["ones128"])
                P.op("pool", lambda E: E.memset(carry[:], 0.0), writes=["carry"])

                def route(i, hb, hres):
                    for kc in range(8):
                        P.op("pe", lambda E, kc=kc, i=i: E.matmul(
                            ps_l[:], lhsT=hn2T[:, kc, 128 * i:128 * (i + 1)], rhs=wr[:, kc, :],
                            start=(kc == 0), stop=(kc == 7)), reads=["wr", ("T", i)], writes=["psl"])
                    P.op("dve", lambda E: E.tensor_tensor(out=lg[:], in0=ps_l[:], in1=brb[:], op=ALU.add),
                         reads=["psl", "brb"], writes=["lg"])
                    P.op("dve", lambda E: E.max(out=m8[:], in_=lg[:]), reads=["lg"], writes=["m8"])
                    P.op("dve", lambda E: E.max_index(out=idx8[:], in_max=m8[:], in_values=lg[:]),
                         reads=["lg", "m8"], writes=["idx8"])
                    P.op("dve", lambda E: E.tensor_copy(out=ef[:], in_=idx8[:, 0:4]), reads=["idx8"], writes=["ef"])
                    P.op("dve", lambda E: E.tensor_scalar(out=negm[:], in0=m8[:, 0:1], scalar1=-1.0, scalar2=None,
                                                          op0=ALU.mult), reads=["m8"], writes=["negm"])
                    P.op("dve", lambda E: E.tensor_scalar(out=msk[:], in0=lg[:], scalar1=m8[:, 3:4], scalar2=None,
                                                          op0=ALU.is_ge), reads=["lg", "m8"], writes=["msk"])
                    P.op("dve", lambda E: E.tensor_copy(out=mskb[:], in_=msk[:]), reads=["msk"], writes=["mskb"])
                    P.op("act", lambda E: E.activation(out=ex[:], in_=lg[:], func=AF.Exp, bias=negm[:], scale=1.0),
                         reads=["lg", "negm"], writes=["ex"])
                    P.op("dve", lambda E: E.tensor_tensor(out=ex[:], in0=ex[:], in1=msk[:], op=ALU.mult),
                         reads=["ex", "msk"], writes=["ex"])
                    P.op("dve", lambda E: E.reduce_sum(out=ssum[:], in_=ex[:], axis=mybir.AxisListType.X),
                         reads=["ex"], writes=["ssum"])
                    P.op("dve", lambda E: E.reciprocal(out=ssum[:], in_=ssum[:]), reads=["ssum"], writes=["ssum"])
                    P.op("dve", lambda E, i=i: E.tensor_scalar(out=gates[:, i, :], in0=ex[:], scalar1=ssum[:, 0:1],
                                                               scalar2=None, op0=ALU.mult),
                         reads=["ex", "ssum"], writes=[("gates", i)])
                    P.op("dve", lambda E, i=i: E.tensor_copy(out=gates_bf[:, i, :], in_=gates[:, i, :]),
                         reads=[("gates", i)], writes=[("gates_bf", i)])
                    P.op("pe", lambda E, i=i: E.transpose(ps_g[:], gates_bf[:, i, :], ident[:]),
                         reads=[("gates_bf", i), "ident"], writes=["psgT"])
                    P.op("act", lambda E, i=i: E.copy(out=gT[:, i, :], in_=ps_g[:]), reads=["psgT"],
                         writes=[("gT", i)])
                    P.op("pe", lambda E: E.matmul(ps_pos[:], lhsT=ltri[:], rhs=mskb[:], start=True, stop=True),
                         reads=["ltri", "mskb"], writes=["pspos"])
                    P.op("pe", lambda E: E.matmul(ps_cnt[:], lhsT=ones128[:], rhs=mskb[:], start=True, stop=True),
                         reads=["ones128", "mskb"], writes=["pscnt"])
                    P.op("dve", lambda E: E.tensor_tensor(out=posf[:], in0=ps_pos[:], in1=carry[:], op=ALU.add),
                         reads=["pspos", "carry"], writes=["posf"])
                    P.op("dve", lambda E: E.tensor_tensor(out=carry[:], in0=ps_cnt[:], in1=carry[:], op=ALU.add),
                         reads=["pscnt", "carry", "posf"], writes=["carry"])
                    P.op("dve", lambda E: E.tensor_scalar(out=ovf[:], in0=posf[:], scalar1=float(CAP), scalar2=BIGSLOT,
                                                          op0=ALU.is_ge, op1=ALU.mult), reads=["posf"], writes=["ovf"])
                    P.op("dve", lambda E: E.tensor_tensor(out=posf[:], in0=posf[:], in1=ebase[:], op=ALU.add),
                         reads=["posf", "ebase", "ovf"], writes=["posf"])
                    P.op("dve", lambda E: E.tensor_tensor(out=posf[:], in0=posf[:], in1=ovf[:], op=ALU.add),
                         reads=["posf", "ovf"], writes=["posf"])
                    P.op("dve", lambda E: E.tensor_tensor(
                        out=oh4[:], in0=iota4[:], in1=ef[:].unsqueeze(2).to_broadcast([128, 4, NE]), op=ALU.is_equal),
                         reads=["iota4", "ef"], writes=["oh4"])
                    P.op("dve", lambda E: E.tensor_tensor(
                        out=pr4[:], in0=oh4[:], in1=posf[:].unsqueeze(1).to_broadcast([128, 4, NE]), op=ALU.mult),
                         reads=["oh4", "posf"], writes=["pr4"])
                    P.op("dve", lambda E: E.reduce_sum(out=slotf[:], in_=pr4[:], axis=mybir.AxisListType.X),
                         reads=["pr4"], writes=["slotf"])
                    P.op("dve", lambda E, i=i: E.tensor_copy(out=slot_i32[:, i, :], in_=slotf[:]),
                         reads=["slotf"], writes=[("slot", i)])
                    P.op("dve", lambda E, i=i: E.tensor_tensor(
                        out=pr4[:], in0=oh4[:], in1=gates[:, i, :].unsqueeze(1).to_broadcast([128, 4, NE]), op=ALU.mult),
                         reads=["oh4", ("gates", i), "pr4"], writes=["pr4"])
                    P.op("dve", lambda E: E.reduce_sum(out=g4[:], in_=pr4[:], axis=mybir.AxisListType.X),
                         reads=["pr4"], writes=["g4"])
                    P.op("dve", lambda E, i=i: E.tensor_scalar(out=g4n[:, i, :], in0=g4[:], scalar1=-1.0, scalar2=None,
                                                               op0=ALU.mult), reads=["g4"], writes=[("g4n", i)])
                    for k in range(4):
                        P.op("pool", lambda E, i=i, k=k, hb=hb: E.indirect_dma_start(
                            out=xs[:, :], out_offset=bass.IndirectOffsetOnAxis(ap=slot_i32[:, i, k:k + 1], axis=0),
                            in_=hb[:], in_offset=None, bounds_check=P.bc_reg(E, NE * CAP - 1), oob_is_err=False),
                             reads=[hres, ("slot", i)], writes=[], dma_key=("scat", i % 2, k))

                rms_tiles(lambda i: (x1[:, i, :], ("x1", i)), NT, g_b, hn2T, 0, "C1", None,
                          (junk, ssq, rstd), hns, ps_tr, post=route)
                for i in range(NT):
                    for half in range(2):
                        pp = ps_b[(2 * i + half) % 2]
                        pres = ("psbd", (2 * i + half) % 2)
                        P.op("pe", lambda E, pp=pp, i=i, half=half: E.matmul(
                            pp[:], lhsT=gT[:, i, :], rhs=bdn[:, 512 * half:512 * (half + 1)], start=True, stop=True),
                             reads=[("gT", i), "bdn"], writes=[pres])
                        P.op("dve", lambda E, pp=pp, i=i, half=half: E.tensor_tensor(
                            out=x1[:, i, 512 * half:512 * (half + 1)], in0=x1[:, i, 512 * half:512 * (half + 1)],
                            in1=pp[:], op=ALU.add), reads=[pres, ("x1", i)], writes=[("x1", i)])
                    P.dma("sp", x1_spill[:, D * i:D * (i + 1)], x1[:, i, :], reads=[("x1", i)], key=("spill", i % 4))
                P.emit()
                if debug:
                    P.dma("sp", dbg["gates"].rearrange("p (c t) -> p c t", c=NT), gates[:], key="dbg")
                    P.emit()

        sAB.close()
        with ExitStack() as sC2:
            TC2 = lambda name, shape, dt: sC2.enter_context(nc.sbuf_tensor("sb_" + name, list(shape), dt))
            NRING = 8
            ring = [TC2("ring%d" % i, [128, 8, 512], BF16) for i in range(NRING)]
            bgu = TC2("bgu", [128, NE, 16], F32)
            xes = [TC2("xe%d" % i, [128, CAP // 128, D], BF16) for i in range(2)]
            xeTs = [TC2("xeT%d" % i, [128, 8, CAP], BF16) for i in range(2)]
            act_es = [TC2("act_e%d" % i, [128, 8, CAP], BF16) for i in range(2)]
            rs = [TC2("r%d" % i, [128, CAP], F32) for i in range(2)]
            sgs = [TC2("sg%d" % i, [128, CAP], F32) for i in range(2)]
            ucs = [TC2("uc%d" % i, [128, CAP], F32) for i in range(2)]
            yts = [TC2("yt%d" % i, [128, D], F32) for i in range(3)]
            ps_gu = [sC2.enter_context(nc.psum_tensor("psgu%d" % i, [128, 512], F32)) for i in range(4)]
            ps_d = [sC2.enter_context(nc.psum_tensor("psd%d" % i, [128, 512], F32)) for i in range(2)]
            ps_tr = [sC2.enter_context(nc.psum_tensor("pstrE%d" % i, [128, 8, 128], BF16)) for i in range(2)]
            P.dma("sp", bgu[:], bgu_in.rearrange("p (e c) -> p e c", e=NE), writes=["bgu"], key=P.ckey())
            P.op("dve", lambda E: E.tensor_scalar(out=bgu[:, :, 0:8], in0=bgu[:, :, 0:8], scalar1=-1.0, scalar2=7.0,
                                                  op0=ALU.mult, op1=ALU.add), reads=["bgu"], writes=["bgu"])
            P.op("dve", lambda E: E.tensor_scalar(out=bgu[:, :, 8:16], in0=bgu[:, :, 8:16], scalar1=1.0, scalar2=None,
                                                  op0=ALU.add), reads=["bgu"], writes=["bgu"])
            pieces = []
            for e in range(NE):
                for j in range(2):
                    pieces.append((e, "g", j))
                    pieces.append((e, "u", j))
                for half in range(2):
                    pieces.append((e, "dn", half))

            def piece_dma(n):
                e, kind, j = pieces[n]
                slot = n % NRING
                wg_e = wgu_bf[e] if e < NCONV else w_gu[e]
                wd_e = wdn_bf[e] if e < NCONV else w_down[e]
                for part in range(2):
                    if kind in ("g", "u"):
                        c0 = (0 if kind == "g" else 1024) + 512 * j
                        src = wg_e.rearrange("(kc p) f -> p kc f", p=128)[:, 4 * part:4 * (part + 1), c0:c0 + 512]
                    else:
                        src = wd_e.rearrange("(kc p) n -> p kc n", p=128)[
                            :, 4 * part:4 * (part + 1), 512 * j:512 * (j + 1)]
                    dst = ring[slot][:, 4 * part:4 * (part + 1), :]
                    P.dma("pool", dst, src, writes=[("ring", slot, part)], key=("ring", slot, part))

            def xe_load(e):
                b = e % 2
                P.dma("sp", xes[b][:], xs[e * CAP:(e + 1) * CAP, :].rearrange("(j p) d -> p j d", p=128),
                      writes=[("xe", b)], key=("xe", b))

            LOOK = NRING - 2
            for n in range(min(LOOK, len(pieces))):
                piece_dma(n)
            xe_load(0)
            gi = 0
            di = 0
            ei = 0
            ti = 0
            yi = 0
            for n, (e, kind, j) in enumerate(pieces):
                if n + LOOK < len(pieces):
                    piece_dma(n + LOOK)
                slot = n % NRING
                rg = ring[slot]
                b = e % 2
                xeT, act_e = xeTs[b], act_es[b]
                if kind == "g" and j == 0:
                    if e + 1 < NE:
                        xe_load(e + 1)
                    for jj in range(CAP // 128):
                        pt, ptres = ps_tr[ti % 2], ("pstrE", ti % 2)
                        ti += 1
                        for kc in range(8):
                            P.op("pe", lambda E, pt=pt, kc=kc, jj=jj, b=b: E.transpose(
                                pt[:, kc, :], xes[b][:, jj, 128 * kc:128 * (kc + 1)], ident[:]),
                                 reads=[("xe", b), "ident"], writes=[ptres])
                        P.op("act", lambda E, pt=pt, jj=jj, xeT=xeT: E.copy(out=xeT[:, :, 128 * jj:128 * (jj + 1)], in_=pt[:]),
                             reads=[ptres], writes=[("xeT", b)])
                if kind == "g":
                    continue
                if kind == "u":
                    slot_g = (n - 1) % NRING
                    rgg = ring[slot_g]
                    for c in range(4):
                        fc = 4 * j + c
                        pg, pgres = ps_gu[gi % 4], ("psgu", gi % 4)
                        gi += 1
                        pu, pures = ps_gu[gi % 4], ("psgu", gi % 4)
                        gi += 1
                        for (pp, pres, wt, wslot) in ((pg, pgres, rgg, slot_g), (pu, pures, rg, slot)):
                            for kc in range(8):
                                P.op("pe", lambda E, pp=pp, wt=wt, kc=kc, c=c, xeT=xeT: E.matmul(
                                    pp[:, 0:CAP], lhsT=wt[:, kc, 128 * c:128 * (c + 1)],
                                    rhs=xeT[:, kc, :], start=(kc == 0), stop=(kc == 7)),
                                     reads=[("ring", wslot, kc // 4), ("xeT", b)], writes=[pres])
                        k2 = ei % 2
                        ei += 1
                        r, sg, uc = rs[k2], sgs[k2], ucs[k2]
                        P.op("act", lambda E, pg=pg, r=r, e=e, fc=fc: E.activation(
                            out=r[:], in_=pg[:, 0:CAP], func=AF.Relu, bias=bgu[:, e, fc:fc + 1], scale=-1.0),
                             reads=[pgres, "bgu"], writes=[("r", k2)])
                        P.op("act", lambda E, r=r, sg=sg: E.activation(out=sg[:], in_=r[:], func=AF.Sigmoid, bias=c119[:],
                                                                       scale=-1.702),
                             reads=[("r", k2), "c119"], writes=[("sg", k2)])
                        P.op("dve", lambda E, r=r, sg=sg: E.scalar_tensor_tensor(
                            out=r[:], in0=r[:], scalar=7.0, in1=sg[:], op0=ALU.subtract, op1=ALU.mult),
                             reads=[("r", k2), ("sg", k2)], writes=[("r", k2)])
                        P.op("dve", lambda E, pu=pu, uc=uc, e=e, fc=fc: E.tensor_scalar(
                            out=uc[:], in0=pu[:, 0:CAP], scalar1=bgu[:, e, 8 + fc:9 + fc], scalar2=8.0, op0=ALU.add,
                            op1=ALU.min), reads=[pures, "bgu"], writes=[("uc", k2)])
                        P.op("dve", lambda E, r=r, uc=uc, fc=fc, act_e=act_e: E.scalar_tensor_tensor(
                            out=act_e[:, fc, :], in0=uc[:], scalar=-6.0, in1=r[:], op0=ALU.max, op1=ALU.mult),
                             reads=[("r", k2), ("uc", k2)], writes=[("act_e", b, fc)])
                else:
                    half = j
                    for jj in range(CAP // 128):
                        pp, pres = ps_d[di % 2], ("psd", di % 2)
                        di += 1
                        for fc in range(8):
                            P.op("pe", lambda E, pp=pp, rg=rg, fc=fc, jj=jj, act_e=act_e: E.matmul(
                                pp[:], lhsT=act_e[:, fc, 128 * jj:128 * (jj + 1)], rhs=rg[:, fc, :],
                                start=(fc == 0), stop=(fc == 7)),
                                 reads=[("ring", slot, fc // 4), ("act_e", b, fc)], writes=[pres])
                        yt = yts[jj]
                        P.op("act", lambda E, pp=pp, yt=yt, half=half: E.copy(out=yt[:, 512 * half:512 * (half + 1)],
                                                                              in_=pp[:]),
                             reads=[pres], writes=[("yt", jj, half)])
                        if half == 1:
                            P.dma("sp", ys[e * CAP + 128 * jj:e * CAP + 128 * (jj + 1), :], yt[:],
                                  reads=[("yt", jj, 0), ("yt", jj, 1)], key=("ys", jj))
            P.emit()

        with ExitStack() as sR:
            TR = lambda name, shape, dt: sR.enter_context(nc.sbuf_tensor("sb_" + name, list(shape), dt))
            x1 = TR("x1b", [128, NT, D], F32)
            g_b = TR("g_b3", [128, D], F32)
            junk = TR("junk3", [128, D], F32)
            ssq = TR("ssq3", [128, NT], F32)
            rstd = TR("rstd3", [128, NT], F32)
            hns = [TR("hnc%d" % i, [128, D], BF16) for i in range(4)]
            with ExitStack() as sD:
                TD = lambda name, shape, dt: sD.enter_context(nc.sbuf_tensor("sb_" + name, list(shape), dt))
                hn3T = TD("hn3T", [128, 8, 2048], BF16)
                wpg = TD("wpg", [128, 8, D], BF16)
                wpp = TD("wpp", [128, 2, D], BF16)
                gfin = TD("gfin", [128, D], F32)
                pts = [TD("pt%d" % i, [128, 256], BF16) for i in range(NT)]
                pTs = [TD("pT%d" % i, [128, 2, 128], BF16) for i in range(2)]
                sgs = [TD("sgD%d" % i, [128, 512], F32) for i in range(2)]
                outs = [TD("outD%d" % i, [128, D], F32) for i in range(2)]
                ssq2 = TD("ssqD", [128, NT], F32)
                rstd2 = TD("rstdD", [128, NT], F32)
                ps_tr = [sD.enter_context(nc.psum_tensor("pstrD%d" % i, [128, 8, 128], BF16)) for i in range(3)]
                ps_pt = sD.enter_context(nc.psum_tensor("pspt", [128, 2, 128], BF16))
                ps_g = [sD.enter_context(nc.psum_tensor("psDg%d" % i, [128, 512], F32)) for i in range(2)]
                ps_p = [sD.enter_context(nc.psum_tensor("psDp%d" % i, [128, 512], F32)) for i in range(2)]
                P.dma("sp", g_b[:], g_ple_in[:, :], writes=["gains"], key=P.ckey())
                P.dma("sp", gfin[:], g_fin_in[:, :], writes=["gfin"], key=P.ckey())
                P.dma("pool", wpg[:], w_pg_v, writes=["wpg"], key="wpg")
                P.dma("pool", wpp[:], w_pp_v, writes=["wpp"], key="wpp")
                for i in range(NT):
                    P.dma("pool", pts[i][:], p_in[128 * i:128 * (i + 1), :], writes=[("pt", i)], key=("pt", i % 2))
                yks = [TD("yk%d" % i, [128, D], F32) for i in range(8)]
                for i in range(8):
                    P.op("pool", lambda E, i=i: E.memset(yks[i][:], 0.0), writes=[("yk", i)])
                for i in range(NT):
                    P.dma("sp", x1[:, i, :], x1_spill[:, D * i:D * (i + 1)], writes=[("x1", i)], key=("x1", i % 4))

                def combine_tile(i):
                    for k in range(4):
                        q = (4 * i + k) % 8
                        P.op("pool", lambda E, i=i, k=k, q=q: E.indirect_dma_start(
                            out=yks[q][:], out_offset=None, in_=ys[:, :],
                            in_offset=bass.IndirectOffsetOnAxis(ap=slot_i32[:, i, k:k + 1], axis=0),
                            bounds_check=P.bc_reg(E, NE * CAP - 1), oob_is_err=False),
                             reads=[], writes=[("yk", q)], dma_key=("yk", q))
                        P.op("dve", lambda E, i=i, k=k, q=q: E.scalar_tensor_tensor(
                            out=x1[:, i, :], in0=yks[q][:], scalar=g4n[:, i, k:k + 1], in1=x1[:, i, :],
                            op0=ALU.mult, op1=ALU.add), reads=[("yk", q), ("x1", i)], writes=[("x1", i)])

                def pre_combine(i):
                    if debug:
                        return
                    if i == 0:
                        combine_tile(0)
                    if i + 1 < NT:
                        combine_tile(i + 1)

                if debug:
                    for i in range(NT):
                        combine_tile(i)
                if debug:
                    P.emit()
                if debug:
                    P.dma("sp", dbg["x2"].rearrange("p (c t) -> p c t", c=NT), x1[:], key="dbg")
                    P.emit()

                pi_box = [0]

                def ple_tile(i, hb_unused, hres_unused):
                    b = i % 2
                    for c in range(2):
                        P.op("pe", lambda E, i=i, c=c: E.transpose(ps_pt[:, c, :], pts[i][:, 128 * c:128 * (c + 1)], ident[:]),
                             reads=[("pt", i), "ident"], writes=["pspt"])
                    P.op("act", lambda E, b=b: E.copy(out=pTs[b][:], in_=ps_pt[:]), reads=["pspt"], writes=[("pT", b)])
                    for half in range(2):
                        k2 = pi_box[0] % 2
                        pi_box[0] += 1
                        pg, pgres = ps_g[k2], ("psDg", k2)
                        pq, pqres = ps_p[k2], ("psDp", k2)
                        for kc in range(8):
                            P.op("pe", lambda E, pg=pg, kc=kc, i=i, half=half: E.matmul(
                                pg[:], lhsT=hn3T[:, kc, 128 * i:128 * (i + 1)], rhs=wpg[:, kc, 512 * half:512 * (half + 1)],
                                start=(kc == 0), stop=(kc == 7)), reads=["wpg", ("T", i)], writes=[pgres])
                        for c in range(2):
                            P.op("pe", lambda E, pq=pq, c=c, b=b, half=half: E.matmul(
                                pq[:], lhsT=pTs[b][:, c, :], rhs=wpp[:, c, 512 * half:512 * (half + 1)],
                                start=(c == 0), stop=(c == 1)), reads=["wpp", ("pT", b)], writes=[pqres])
                        sg = sgs[k2]
                        P.op("act", lambda E, pg=pg, sg=sg: E.activation(out=sg[:], in_=pg[:], func=AF.Sigmoid),
                             reads=[pgres], writes=[("sgD", k2)])
                        P.op("dve", lambda E, pq=pq, sg=sg: E.tensor_tensor(out=sg[:], in0=sg[:], in1=pq[:], op=ALU.mult),
                             reads=[pqres, ("sgD", k2)], writes=[("sgD", k2)])
                        P.op("dve", lambda E, sg=sg, i=i, half=half: E.tensor_tensor(
                            out=x1[:, i, 512 * half:512 * (half + 1)], in0=x1[:, i, 512 * half:512 * (half + 1)],
                            in1=sg[:], op=ALU.add), reads=[("sgD", k2), ("x1", i)], writes=[("x1", i)])
                    ob = outs[b]
                    P.op("act", lambda E, i=i: E.activation(out=junk[:], in_=x1[:, i, :], func=AF.Square,
                                                            accum_out=ssq2[:, i:i + 1]),
                         reads=[("x1", i)], writes=["Djunk2", ("ssqD", i)])
                    P.op("act", lambda E, i=i: E.activation(out=rstd2[:, i:i + 1], in_=ssq2[:, i:i + 1], func=AF.Sqrt,
                                                            bias=epsb[:], scale=1.0 / D),
                         reads=[("ssqD", i), "epsb"], writes=[("rstdD", i)])
                    P.op("dve", lambda E, i=i: E.reciprocal(out=rstd2[:, i:i + 1], in_=rstd2[:, i:i + 1]),
                         reads=[("rstdD", i)], writes=[("rstdD", i)])
                    P.op("dve", lambda E, i=i, ob=ob: E.scalar_tensor_tensor(
                        out=ob[:], in0=x1[:, i, :], scalar=rstd2[:, i:i + 1], in1=gfin[:], op0=ALU.mult, op1=ALU.mult),
                         reads=[("x1", i), ("rstdD", i), "gfin"], writes=[("outD", b)])
                    P.dma("sp", y[128 * i:128 * (i + 1), :], ob[:], reads=[("outD", b)], key=("outD", b))
                rms_tiles(lambda i: (x1[:, i, :], ("x1", i)), NT, g_b, hn3T, 0, "D", None,
                          (junk, ssq, rstd), hns, ps_tr, pre=pre_combine, post=ple_tile)
                P.emit()
    return nc


def _t5_bucket_np(dist):
    n = np.maximum(dist, 1).astype(np.float32)
    large = 16 + (np.log(n / 16) / math.log(2048 / 16) * 16).astype(np.int32)
    large = np.minimum(large, 31)
    return np.where(dist < 16, dist, large)


def _bias_tables(rel_bias, first_half):
    ki = np.arange(128)[:, None]
    qi = np.arange(128)[None, :]
    btab = np.full((128, 3, 8, 256), NEG, np.float32)
    for br, (window, dil) in enumerate(BRANCHES):
        relp = 128 + qi - ki
        idxp = _t5_bucket_np(np.maximum(relp, 0) * dil)
        relc = qi - ki
        idxc = _t5_bucket_np(np.maximum(relc, 0) * dil)
        for h in range(8):
            bp = rel_bias[idxp, h]
            bc = rel_bias[idxc, h]
            btab[:, br, h, 128:256] = np.where((relp >= 0) & (relp <= 128), bp, NEG)
            btab[:, br, h, 0:128] = np.where((relc >= 0) & (relc <= 128), bc, NEG)
    bhalo = btab[:, :, :, 128:256].copy()
    if first_half:
        bhalo[:] = NEG
    return btab.reshape(128, -1), np.ascontiguousarray(bhalo).reshape(128, -1)


_NC_CACHE = {}


def _prepare_in_maps(inputs):
    f = lambda a: np.ascontiguousarray(np.asarray(a, dtype=np.float32))
    x = f(inputs["x"])
    p = f(inputs["p"])[0]
    rb = f(inputs["rel_bias"])
    bc = lambda v: np.ascontiguousarray(np.broadcast_to(f(v).reshape(1, -1), (128, f(v).size)))
    shared = {
        "ident": np.eye(128, dtype=np.float32),
        "ltri": np.triu(np.ones((128, 128), np.float32), 1),
        "iota4": np.ascontiguousarray(np.broadcast_to(np.tile(np.arange(NE, dtype=np.float32), 4)[None, :], (128, 4 * NE))),
        "ebase": np.ascontiguousarray(np.broadcast_to((np.arange(NE, dtype=np.float32) * CAP)[None, :], (128, NE))),
        "g_mix_b": bc(inputs["g_mix"][0]),
        "g_ffn_b": bc(inputs["g_ffn"][0]),
        "g_ple_b": bc(inputs["g_ple"][0]),
        "g_fin_b": bc(inputs["g_final"]),
        "w_in": f(inputs["w_in"])[0],
        "w_pool": f(inputs["w_pool"])[0],
        "pscale": np.ascontiguousarray(f(inputs["pool_scale"])[0].reshape(4, 128).T),
        "w_out": f(inputs["w_out"])[0],
        "w_router": f(inputs["w_router"])[0],
        "b_router_b": bc(inputs["b_router"][0]),
        "w_gate_up": f(inputs["w_gate_up"])[0],
        "bgu": np.ascontiguousarray(f(inputs["b_gate_up"])[0].reshape(NE, 16, 128).transpose(2, 0, 1)).reshape(128, -1),
        "w_down": f(inputs["w_down"])[0],
        "b_down": f(inputs["b_down"])[0],
        "w_ple_gate": f(inputs["w_ple_gate"])[0],
        "w_ple_proj": f(inputs["w_ple_proj"])[0],
    }
    tabs = {fh: _bias_tables(rb, fh) for fh in (True, False)}
    in_maps = []
    for c in range(NCORES):
        b, half = c // 2, c % 2
        base = half * S_OWN
        xh = np.zeros((4096, D), np.float32)
        if half == 1:
            xh[0:2048] = x[b, 0:2048]
        xh[2048:4096] = x[b, base:base + S_OWN]
        pos = base + np.arange(16)
        invcnt = np.stack([1.0 / np.minimum(pos + 1, w) for w in (2, 4, 8, 16)]).astype(np.float32)
        m = dict(shared)
        m["xh"] = xh
        m["p"] = np.ascontiguousarray(p[b, base:base + S_OWN])
        m["btab"], m["bhalo"] = tabs[half == 0]
        m["invcnt"] = np.ascontiguousarray(np.broadcast_to(invcnt.reshape(1, -1), (128, 64)))
        in_maps.append(m)
    return in_maps


def kernel(**inputs):
    in_maps = _prepare_in_maps(inputs)
    if "nc" not in _NC_CACHE:
        _NC_CACHE["nc"] = build_program(debug=False)
    nc = _NC_CACHE["nc"]
    res = run_bass_kernel_spmd(nc, in_maps, core_ids=list(range(NCORES)))
    out = np.zeros((4, 4096, D), np.float32)
    for c in range(NCORES):
        b, half = c // 2, c % 2
        out[b, half * S_OWN:(half + 1) * S_OWN] = np.asarray(res.results[c]["y"], dtype=np.float32)
    return out
```

```python
import math
from contextlib import ExitStack

import numpy as np
import concourse.bass as bass
import concourse.mybir as mybir
from concourse.bass_utils import run_bass_kernel_spmd

F32 = mybir.dt.float32
BF16 = mybir.dt.bfloat16
AF = mybir.ActivationFunctionType
ALU = mybir.AluOpType

NCORES = 8
D = 1024
S_OWN = 2048
NT = 16
NE = 32
NEG = -1e30
CAP = 384
BIGSLOT = 1.0e6
NCONV = 9
OPT_ACT_RECIP = True
OPT_ATT_NEW = False
EPS = 1e-6
BRANCHES = ((128, 1), (512, 4), (2048, 16))


class Op:
    __slots__ = ("eng", "fn", "deps", "signal", "count", "dma_sem", "is_dma")

    def __init__(self, eng, fn, dma_sem=None):
        self.eng = eng
        self.fn = fn
        self.deps = []
        self.signal = False
        self.count = 0
        self.dma_sem = dma_sem
        self.is_dma = dma_sem is not None


class Prog:
    ENGS = ("pe", "act", "dve", "pool", "sp")

    def __init__(self, nc, stack):
        self.nc = nc
        self.stack = stack
        self.esem = {e: stack.enter_context(nc.semaphore("sem_" + e)) for e in self.ENGS}
        self.ecount = {e: 0 for e in self.ENGS}
        self.dsem = {}
        self.dcount = {}
        self.waited = {e: {} for e in self.ENGS}
        self.begin()

    def bc_reg(self, E, val):
        if self._bc is None:
            self._bc = E.to_reg(val)
        return self._bc

    def begin(self):
        self._bc = None
        self.ops = []
        self.res_w = {}
        self.res_r = {}

    def _dma_sem(self, key):
        if key not in self.dsem:
            self.dsem[key] = self.stack.enter_context(self.nc.semaphore("dsem_%d" % len(self.dsem)))
            self.dcount[key] = 0
        return key

    def op(self, eng, fn, reads=(), writes=(), dma_key=None):
        o = Op(eng, fn, self._dma_sem(dma_key) if dma_key is not None else None)
        if dma_key is not None:
            writes = list(writes) + [("__key", dma_key)]
        deps = {}
        for r in reads:
            w = self.res_w.get(r)
            if w is not None:
                deps[id(w)] = w
        for r in writes:
            w = self.res_w.get(r)
            if w is not None:
                deps[id(w)] = w
            for rd in self.res_r.get(r, ()):
                deps[id(rd)] = rd
        for d in deps.values():
            if d is o:
                continue
            if d.eng == "pe" and eng == "pe" and not d.is_dma and not o.is_dma:
                continue
            o.deps.append(d)
            d.signal = True
        for r in reads:
            lst = self.res_r.setdefault(r, [])
            if not o.is_dma:
                lst[:] = [x for x in lst if x.is_dma or x.eng != eng]
            lst.append(o)
        for r in writes:
            self.res_w[r] = o
            self.res_r[r] = []
        self.ops.append(o)
        return o

    def dma(self, eng, out, in_, reads=(), writes=(), key=None):
        assert key is not None
        return self.op(eng, lambda E: E.dma_start(out=out, in_=in_), reads, writes, dma_key=key)

    def ckey(self):
        self._ck = (getattr(self, "_ck", 0) + 1) % 6
        return ("const", self._ck)

    def emit(self, defer_cv=False):
        nc = self.nc
        last = {}
        for o in self.ops:
            if not o.is_dma:
                last[o.eng] = o
        for o in last.values():
            o.signal = True
        for o in self.ops:
            if o.is_dma:
                o.signal = True
                self.dcount[o.dma_sem] += 16
                o.count = self.dcount[o.dma_sem]
            elif o.signal:
                self.ecount[o.eng] += 1
                o.count = self.ecount[o.eng]
        by_eng = {e: [o for o in self.ops if o.eng == e] for e in self.ENGS}
        final_e = dict(self.ecount)
        final_d = dict(self.dcount)

        def run(ename, E):
            waited = self.waited[ename]
            for o in by_eng[ename]:
                need = {}
                for d in o.deps:
                    if d.is_dma:
                        k = ("d", d.dma_sem)
                    else:
                        k = ("e", d.eng)
                    if d.count > need.get(k, 0):
                        need[k] = d.count
                for k, v in need.items():
                    if waited.get(k, 0) < v:
                        sem = self.dsem[k[1]] if k[0] == "d" else self.esem[k[1]]
                        E.wait_ge(sem, v)
                        waited[k] = v
                ins = o.fn(E)
                if o.signal:
                    if o.is_dma:
                        ins.then_inc(self.dsem[o.dma_sem], 16)
                    else:
                        ins.then_inc(self.esem[o.eng], 1)
            for e2, v in final_e.items():
                if v > 0 and waited.get(("e", e2), 0) < v:
                    E.wait_ge(self.esem[e2], v)
                    waited[("e", e2)] = v
            for k2, v in final_d.items():
                if defer_cv and isinstance(k2, tuple) and k2[0] == "cv":
                    continue
                if v > 0 and waited.get(("d", k2), 0) < v:
                    E.wait_ge(self.dsem[k2], v)
                    waited[("d", k2)] = v

        with nc.Block() as block:
            @block.tensor
            def _(E):
                run("pe", E)

            @block.scalar
            def _(E):
                run("act", E)

            @block.vector
            def _(E):
                run("dve", E)

            @block.gpsimd
            def _(E):
                run("pool", E)

            @block.sync
            def _(E):
                run("sp", E)
        self.begin()


def build_program(debug=False):
    nc = bass.Bass("TRN2", target_bir_lowering=False)

    def din(name, shape, dt=F32):
        return nc.dram_tensor(name, list(shape), dt, kind="ExternalInput").ap()

    xh = din("xh", [4096, D])
    p_in = din("p", [S_OWN, 256])
    ident_in = din("ident", [128, 128])
    btab_in = din("btab", [128, 3 * 8 * 256])
    bhalo_in = din("bhalo", [128, 3 * 8 * 128])
    invcnt_in = din("invcnt", [128, 4 * 16])
    g_mix_in = din("g_mix_b", [128, D])
    g_ffn_in = din("g_ffn_b", [128, D])
    g_ple_in = din("g_ple_b", [128, D])
    g_fin_in = din("g_fin_b", [128, D])
    w_in = din("w_in", [D, 2048])
    w_pool = din("w_pool", [4, 128, 128])
    pscale_in = din("pscale", [128, 4])
    w_out = din("w_out", [D, D])
    w_router = din("w_router", [D, NE])
    brouter_in = din("b_router_b", [128, NE])
    w_gu = din("w_gate_up", [NE, D, 2 * D])
    bgu_in = din("bgu", [128, NE * 16])
    w_down = din("w_down", [NE, D, D])
    b_down = din("b_down", [NE, D])
    w_pg = din("w_ple_gate", [D, D])
    w_pp = din("w_ple_proj", [256, D])
    ltri_in = din("ltri", [128, 128])
    iota4_in = din("iota4", [128, 4 * NE])
    ebase_in = din("ebase", [128, NE])
    y = nc.dram_tensor("y", [S_OWN, D], F32, kind="ExternalOutput").ap()
    xs = nc.dram_tensor("xs_scratch", [NE * CAP, D], BF16).ap()
    ys = nc.dram_tensor("ys_scratch", [NE * CAP, D], F32).ap()
    x1_spill = nc.dram_tensor("x1_spill", [128, NT * D], F32).ap()
    wgu_bf = nc.dram_tensor("wgu_bf16", [NCONV, D, 2 * D], BF16).ap()
    wdn_bf = nc.dram_tensor("wdn_bf16", [NCONV, D, D], BF16).ap()
    dbg = {}
    if debug:
        dbg["mixT"] = nc.dram_tensor("dbg_mixT", [128, 8 * 2048], BF16, kind="ExternalOutput").ap()
        dbg["mixA"] = nc.dram_tensor("dbg_mixA", [64, 8 * 2048], BF16, kind="ExternalOutput").ap()
        dbg["x1"] = nc.dram_tensor("dbg_x1", [128, NT * D], F32, kind="ExternalOutput").ap()
        dbg["gates"] = nc.dram_tensor("dbg_gates", [128, NT * NE], F32, kind="ExternalOutput").ap()
        dbg["x2"] = nc.dram_tensor("dbg_x2", [128, NT * D], F32, kind="ExternalOutput").ap()

    w_in_v = w_in.rearrange("(kc p) n -> p kc n", p=128)
    w_out_v = w_out.rearrange("(kc p) n -> p kc n", p=128)
    w_pg_v = w_pg.rearrange("(kc p) n -> p kc n", p=128)
    w_pp_v = w_pp.rearrange("(kc p) n -> p kc n", p=128)
    w_router_v = w_router.rearrange("(kc p) n -> p kc n", p=128)

    with ExitStack() as stack:
        P = Prog(nc, stack)
        T = lambda name, shape, dt: stack.enter_context(nc.sbuf_tensor("sb_" + name, list(shape), dt))
        ident = T("ident_sb", [128, 128], BF16)
        ones_bf = T("ones_bf", [128, 64], BF16)
        sAB = ExitStack()
        TAB = lambda name, shape, dt: sAB.enter_context(nc.sbuf_tensor("sb_" + name, list(shape), dt))
        epsb = T("epsb", [128, 1], F32)
        c119 = T("c119", [128, 1], F32)
        slot_i32 = T("slot_i32", [128, NT, 4], mybir.dt.int32)
        g4n = T("g4n", [128, NT, 4], F32)
        actT = TAB("actT", [128, 4, 2048], BF16)
        mixA = TAB("mixA", [64, 8, 2048], BF16)

        def rms_tiles(x_ap_of, ntiles, g_b, dstT, dst_col0, pfx, xt_res, stats, hn_bufs, ps_tr,
                      pre=None, post=None):
            junk, ssq, rstd = stats
            for i in range(ntiles):
                if pre is not None:
                    pre(i)
                xa, xres = x_ap_of(i)
                P.op("act", lambda E, xa=xa, i=i: E.activation(out=junk[:], in_=xa, func=AF.Square,
                                                                accum_out=ssq[:, i:i + 1]),
                     reads=[xres], writes=[pfx + "junk", (pfx + "ssq", i)])
                P.op("act", lambda E, i=i: E.activation(out=rstd[:, i:i + 1], in_=ssq[:, i:i + 1], func=AF.Sqrt,
                                                        bias=epsb[:], scale=1.0 / D),
                     reads=[(pfx + "ssq", i), "epsb"], writes=[(pfx + "rstd", i)])
                P.op("dve", lambda E, i=i: E.reciprocal(out=rstd[:, i:i + 1], in_=rstd[:, i:i + 1]),
                     reads=[(pfx + "rstd", i)], writes=[(pfx + "rstd", i)])
                hb = hn_bufs[i % len(hn_bufs)]
                hres = (pfx + "hn", i % len(hn_bufs))
                P.op("dve", lambda E, xa=xa, i=i, hb=hb: E.scalar_tensor_tensor(
                    out=hb[:], in0=xa, scalar=rstd[:, i:i + 1], in1=g_b[:], op0=ALU.mult, op1=ALU.mult),
                     reads=[xres, (pfx + "rstd", i), "gains"], writes=[hres])
                pt = ps_tr[i % len(ps_tr)]
                pres = (pfx + "pstr", i % len(ps_tr))
                for kc in range(8):
                    P.op("pe", lambda E, kc=kc, hb=hb, pt=pt: E.transpose(pt[:, kc, :], hb[:, kc * 128:(kc + 1) * 128],
                                                                          ident[:]),
                         reads=[hres, "ident"], writes=[pres])
                c0 = dst_col0 + 128 * i
                P.op("act", lambda E, pt=pt, c0=c0: E.copy(out=dstT[:, :, c0:c0 + 128], in_=pt[:]),
                     reads=[pres], writes=[("T", c0 // 128)])
                if post is not None:
                    post(i, hb, hres)

        P.dma("pool", ident[:], ident_in[:, :], writes=["ident"], key=P.ckey())
        P.op("pool", lambda E: E.memset(ones_bf[:], 1.0), writes=["ones"])
        P.op("pool", lambda E: E.memset(epsb[:], EPS), writes=["epsb"])
        P.op("pool", lambda E: E.memset(c119[:], 7.0 * 1.702), writes=["c119"])
        zero_jobs = list(range(NE * CAP // 1024))

        with ExitStack() as sA:
            TA = lambda name, shape, dt: sA.enter_context(nc.sbuf_tensor("sb_" + name, list(shape), dt))
            hnT = TA("hnT", [128, 8, 4096], BF16)

            with ExitStack() as s1:
                T1 = lambda name, shape, dt: s1.enter_context(nc.sbuf_tensor("sb_" + name, list(shape), dt))
                g_b = T1("g_b", [128, D], F32)
                NXT = 4
                xts = [T1("xt%d" % i, [128, D], F32) for i in range(NXT)]
                hns = [T1("hn%d" % i, [128, D], BF16) for i in range(4)]
                junk = T1("junk", [128, D], F32)
                ssq = T1("ssq", [128, 32], F32)
                rstd = T1("rstd", [128, 32], F32)
                ps_tr = [s1.enter_context(nc.psum_tensor("pstrA%d" % i, [128, 8, 128], BF16)) for i in range(4)]
                P.dma("sp", g_b[:], g_mix_in[:, :], writes=["gains"], key=P.ckey())
                def ld(i):
                    P.dma("sp", xts[i % NXT][:], xh[128 * i:128 * (i + 1), :], writes=[("xt", i % NXT)],
                          key=("xt", i % NXT))

                zt = T1("zeros", [128, 8192], BF16)
                P.op("pool", lambda E: E.memset(zt[:], 0.0), writes=["zt"])

                def pre(i):
                    if i == 0:
                        ld(0)
                        ld(1)
                        ld(2)
                    if i + 3 < 32:
                        ld(i + 3)
                    if i >= 2 and zero_jobs:
                        c = zero_jobs.pop()
                        P.dma("sp", xs[1024 * c:1024 * (c + 1), :].rearrange("(p r) d -> p (r d)", p=128), zt[:],
                              reads=["zt"], key=("zx", c % 4))
                rms_tiles(lambda i: (xts[i % NXT][:], ("xt", i % NXT)), 32, g_b, hnT, 0, "A1", None,
                          (junk, ssq, rstd), hns, ps_tr, pre=pre)
                P.emit()

            with ExitStack() as s2:
                T2 = lambda name, shape, dt: s2.enter_context(nc.sbuf_tensor("sb_" + name, list(shape), dt))
                wpc = T2("wpc", [128, 8, 512], BF16)
                wpl = T2("wpl", [128, 4, 128], BF16)
                psc = T2("psc", [128, 4], F32)
                icn = T2("icn", [128, 4, 16], F32)
                u = T2("u", [128, 2064], F32)
                sa = T2("sa", [128, 2064], F32)
                sb = T2("sb", [128, 2064], F32)
                pooled = T2("pooled", [128, 2048], BF16)
                t16 = T2("t16", [128, 16], F32)
                ps = [s2.enter_context(nc.psum_tensor("psA2_%d" % i, [128, 512], F32)) for i in range(2)]
                P.dma("pool", wpc[:], w_in_v[:, :, 0:512], writes=["wpc"], key="wpc")
                P.dma("pool", wpl[:], w_pool.rearrange("g c d -> c g d"), writes=["wpl"], key="wpl")
                P.dma("sp", psc[:], pscale_in[:, :], writes=["psc"], key=P.ckey())
                P.dma("sp", icn[:], invcnt_in.rearrange("p (g t) -> p g t", g=4), writes=["icn"], key=P.ckey())
                pi = 0
                for g in range(4):
                    w = (2, 4, 8, 16)[g]
                    for blk in range(5):
                        pp = ps[pi % 2]
                        pres = ("psA2", pi % 2)
                        pi += 1
                        if blk == 0:
                            c0, n, o0 = 2032, 16, 0
                        else:
                            c0, n, o0 = 2048 + 512 * (blk - 1), 512, 16 + 512 * (blk - 1)
                        for kc in range(8):
                            P.op("pe", lambda E, pp=pp, kc=kc, g=g, c0=c0, n=n: E.matmul(
                                pp[:, 0:n], lhsT=wpc[:, kc, g * 128:(g + 1) * 128], rhs=hnT[:, kc, c0:c0 + n],
                                start=(kc == 0), stop=(kc == 7)), reads=["wpc", "hnT"], writes=[pres])
                        P.op("act", lambda E, pp=pp, n=n, o0=o0: E.copy(out=u[:, o0:o0 + n], in_=pp[:, 0:n]),
                             reads=[pres], writes=["u"])
                    src, srcres = u, "u"
                    sh = 1
                    bufs = [(sa, "sa"), (sb, "sb")]
                    bi = 0
                    while sh < w:
                        dst, dres = bufs[bi % 2]
                        bi += 1
                        P.op("dve", lambda E, dst=dst, src=src, sh=sh: E.tensor_tensor(
                            out=dst[:, sh:2064], in0=src[:, sh:2064], in1=src[:, 0:2064 - sh], op=ALU.add),
                             reads=[srcres], writes=[dres])
                        src, srcres = dst, dres
                        sh *= 2
                    P.op("dve", lambda E, src=src, w=w: E.scalar_tensor_tensor(
                        out=pooled[:], in0=src[:, 16:2064], scalar=1.0 / w, in1=u[:, 16:2064],
                        op0=ALU.mult, op1=ALU.subtract), reads=[srcres, "u"], writes=["pooled"])
                    P.op("dve", lambda E, src=src, g=g: E.tensor_tensor(
                        out=t16[:], in0=src[:, 16:32], in1=icn[:, g, :], op=ALU.mult),
                         reads=[srcres, "icn"], writes=["t16"])
                    P.op("dve", lambda E: E.tensor_tensor(out=pooled[:, 0:16], in0=t16[:], in1=u[:, 16:32],
                                                          op=ALU.subtract),
                         reads=["t16", "u", "pooled"], writes=["pooled"])
                    for blk in range(4):
                        pp = ps[pi % 2]
                        pres = ("psA2", pi % 2)
                        pi += 1
                        P.op("pe", lambda E, pp=pp, g=g, blk=blk: E.matmul(
                            pp[:], lhsT=wpl[:, g, :], rhs=pooled[:, 512 * blk:512 * (blk + 1)], start=True, stop=True),
                             reads=["wpl", "pooled"], writes=[pres])
                        P.op("dve", lambda E, pp=pp, g=g, blk=blk: E.tensor_scalar(
                            out=actT[:, g, 512 * blk:512 * (blk + 1)], in0=pp[:], scalar1=psc[:, g:g + 1],
                            scalar2=None, op0=ALU.mult), reads=[pres, "psc"], writes=[("mixT", g, blk)])
                P.emit()

            with ExitStack() as s3:
                T3 = lambda name, shape, dt: s3.enter_context(nc.sbuf_tensor("sb_" + name, list(shape), dt))
                btabs = [T3("btab%d" % i, [128, 3, 2, 256], BF16) for i in range(2)]
                bhalos = [T3("bhalo%d" % i, [128, 3, 2, 128], BF16) for i in range(2)]
                wqs = [T3("wq%d" % i, [128, 8, 128], BF16) for i in range(2)]
                wks = [T3("wk%d" % i, [128, 8, 128], BF16) for i in range(2)]
                wvs = [T3("wv%d" % i, [128, 8, 128], BF16) for i in range(2)]
                QT = T3("QTz", [128, 2, 2048], BF16)
                KT = T3("KT", [128, 4096], BF16)
                VT = T3("VT", [128, 4096], BF16)
                V = T3("V", [128, 69, 2, 65], BF16)
                acc = T3("acc", [65, 2, 2048], F32)
                onesf = T3("onesf", [65, 64], F32)
                NSB, NOB = 4, 3
                Pb = [T3("Pb%d" % i, [128, 2, 256], BF16) for i in range(NSB)]
                ps_tr = s3.enter_context(nc.psum_tensor("pstrV", [128, 8, 128], BF16))
                ps_s = [s3.enter_context(nc.psum_tensor("pss%d" % i, [128, 2, 256], F32)) for i in range(NSB)]
                ps_o = [s3.enter_context(nc.psum_tensor("pso%d" % i, [128, 2, 256], F32)) for i in range(NOB)]
                ps_pr = [t[:].rearrange("p h q -> p (h q)") for t in ps_o]
                btab_v = btab_in.rearrange("p (b h q) -> p b h q", b=3, h=8)
                bhalo_v = bhalo_in.rearrange("p (b h q) -> p b h q", b=3, h=8)
                P.op("dve", lambda E: E.memset(V[:], 1.0), writes=["V"])
                P.op("dve", lambda E: E.memset(QT[:], 0.0), writes=["QT"])
                P.op("dve", lambda E: E.memset(onesf[:], 1.0), writes=["onesf"])

                def tok_slice(start, dil, n=128):
                    return slice(start, start + (n - 1) * dil + 1, dil)

                ktiles = []
                for j in range(15, 32):
                    ks = tok_slice(128 * j, 1)
                    if j == 15:
                        ktiles.append((0, ks, tok_slice(0, 1), 128, "halo", 0))
                    elif j == 31:
                        ktiles.append((0, ks, tok_slice(128 * 15, 1), 128, "tab", 0))
                    else:
                        ktiles.append((0, ks, tok_slice(128 * (j - 16), 1, 256), 256, "tab", 0))
                for n in range(3, 8):
                    for r in range(4):
                        ks = tok_slice(512 * n + r, 4)
                        if n == 3:
                            ktiles.append((1, ks, tok_slice(r, 4), 128, "halo", 0))
                        elif n == 7:
                            ktiles.append((1, ks, tok_slice(512 * 3 + r, 4), 128, "tab", 0))
                        else:
                            ktiles.append((1, ks, tok_slice(512 * (n - 4) + r, 4, 256), 256, "tab", 0))
                for n in range(2):
                    for r in range(16):
                        ks = tok_slice(2048 * n + r, 16)
                        ktiles.append((2, ks, tok_slice(r, 16), 128, "halo" if n == 0 else "tab", 0))
                assert len(ktiles) == 69

                def load_hp(hp):
                    cq = 512 + 128 * hp
                    w = hp % 2
                    P.dma("pool", wqs[w][:], w_in_v[:, :, cq:cq + 128], writes=[("wq", w)], key=("wq", w))
                    P.dma("pool", wks[w][:], w_in_v[:, :, 512 + cq:512 + cq + 128], writes=[("wk", w)], key=("wk", w))
                    P.dma("pool", wvs[w][:], w_in_v[:, :, 1024 + cq:1024 + cq + 128], writes=[("wv", w)], key=("wv", w))
                    P.dma("pool", btabs[w][:], btab_v[:, :, 2 * hp:2 * hp + 2, :], writes=[("btab", w)], key=("btab", w))
                    P.dma("pool", bhalos[w][:], bhalo_v[:, :, 2 * hp:2 * hp + 2, :], writes=[("bhalo", w)],
                          key=("bhalo", w))

                conv_jobs = []
                for e in range(NCONV):
                    for r in range(8):
                        conv_jobs.append((wgu_bf[e][128 * r:128 * (r + 1), :], w_gu[e][128 * r:128 * (r + 1), :]))
                    for r in range(8):
                        conv_jobs.append((wdn_bf[e][128 * r:128 * (r + 1), :], w_down[e][128 * r:128 * (r + 1), :]))
                conv_jobs.reverse()
                conv_n = [0]

                def conv_issue():
                    if conv_jobs:
                        dst, src = conv_jobs.pop()
                        P.dma("pool", dst, src, key=("cv", conv_n[0] % 10))
                        conv_n[0] += 1

                ppi = 0
                load_hp(0)
                for hp in range(4):
                    if hp + 1 < 4:
                        load_hp(hp + 1)
                    w = hp % 2
                    wq, wk, wv, btab, bhalo = wqs[w], wks[w], wvs[w], btabs[w], bhalos[w]
                    P.op("dve", lambda E: E.memset(acc[:], 0.0), writes=["acc"])
                    for (wt, wres, dst, dres, t0, nblk, scale) in ((wq, ("wq", w), QT, "QT", 2048, 4, 0.125),
                                                                    (wk, ("wk", w), KT, "KT", 0, 8, None),
                                                                    (wv, ("wv", w), VT, "VT", 0, 8, None)):
                        for blk in range(nblk):
                            pp = ps_pr[ppi % NOB]
                            pres = ("pso", ppi % NOB)
                            ppi += 1
                            for kc in range(8):
                                P.op("pe", lambda E, pp=pp, wt=wt, kc=kc, c0=t0 + 512 * blk: E.matmul(
                                    pp[:], lhsT=wt[:, kc, :], rhs=hnT[:, kc, c0:c0 + 512], start=(kc == 0),
                                    stop=(kc == 7)), reads=[wres, "hnT"], writes=[pres])
                            if scale is not None:
                                for h2 in range(2):
                                    P.op("act", lambda E, pp=pp, dst=dst, blk=blk, scale=scale, h2=h2: E.mul(
                                        out=dst[64 * h2:64 * (h2 + 1), h2, 512 * blk:512 * (blk + 1)],
                                        in_=pp[64 * h2:64 * (h2 + 1), :], mul=scale),
                                         reads=[pres], writes=[dres])
                            else:
                                P.op("act", lambda E, pp=pp, dst=dst, blk=blk: E.copy(
                                    out=dst[:, 512 * blk:512 * (blk + 1)], in_=pp[:]), reads=[pres], writes=[dres])
                    for t0 in range(0, 69, 8):
                        nt = min(8, 69 - t0)
                        for k in range(nt):
                            P.op("pe", lambda E, k=k, sl=ktiles[t0 + k][1]: E.transpose(ps_tr[:, k, :], VT[:, sl], ident[:]),
                                 reads=["VT", "ident"], writes=["pstrV"])
                        P.op("dve", lambda E, t0=t0, nt=nt: E.tensor_copy(
                            out=V[:, t0:t0 + nt, :, 0:64],
                            in_=ps_tr[:, 0:nt, :].rearrange("p t (h d) -> p t h d", h=2)),
                             reads=["pstrV"], writes=["V"])

                    def emit_bias(ti):
                        br, ks, qs, nq, kind, c0 = ktiles[ti]
                        b = ti % 2
                        for h2 in range(2):
                            if kind == "halo":
                                bt = bhalo[:, br, h2, :]
                            else:
                                bt = btab[:, br, h2, c0:c0 + nq]
                            P.op("pe", lambda E, b=b, bt=bt, nq=nq, h2=h2: E.matmul(
                                ps_s[b][:, h2, 0:nq], lhsT=ident[:], rhs=bt, start=(h2 == 0), stop=False,
                                skip_group_check=True),
                                 reads=["ident", "btab", "bhalo"], writes=[("pss", b)])

                    def emit_QK(ti):
                        br, ks, qs, nq, kind, c0 = ktiles[ti]
                        b = ti % 2
                        for h2 in range(2):
                            r0 = 64 * h2
                            P.op("pe", lambda E, b=b, h2=h2, nq=nq, ks=ks, qs=qs, r0=r0: E.matmul(
                                ps_s[b][:, h2, 0:nq], lhsT=KT[r0:r0 + 64, ks], rhs=QT[r0:r0 + 64, qs], start=False,
                                stop=(h2 == 1), tile_position=(r0, 0), skip_group_check=True),
                                 reads=["KT", "QT"], writes=[("pss", b)])
                        P.op("act", lambda E, b=b, nq=nq: E.activation(out=Pb[b][:, :, 0:nq], in_=ps_s[b][:, :, 0:nq],
                                                                      func=AF.Exp),
                             reads=[("pss", b)], writes=[("Pb", b)])

                    def emit_PV(ti):
                        br, ks, qs, nq, kind, c0 = ktiles[ti]
                        b = ti % NSB
                        ob = ti % NOB
                        for h2 in range(2):
                            P.op("pe", lambda E, h2=h2, b=b, ob=ob, nq=nq, ti=ti: E.matmul(
                                ps_o[ob][0:65, h2, 0:nq], lhsT=V[:, ti, h2, :], rhs=Pb[b][:, h2, 0:nq], start=True,
                                stop=True), reads=["V", ("Pb", b)], writes=[("pso", ob)])
                        P.op("dve", lambda E, ob=ob, qs=qs, nq=nq: E.tensor_tensor(
                            out=acc[:, :, qs], in0=acc[:, :, qs], in1=ps_o[ob][0:65, :, 0:nq], op=ALU.add),
                             reads=[("pso", ob), "acc"], writes=["acc"])

                    def emit_S_old(ti):
                        br, ks, qs, nq, kind, c0 = ktiles[ti]
                        b = ti % NSB
                        out = ps_s[b][:, :, 0:nq]
                        P.op("pe", lambda E, out=out, ks=ks, qs=qs: E.matmul(
                            out, lhsT=KT[:, ks], rhs=QT[:, :, qs], start=True, stop=False),
                             reads=["KT", "QT"], writes=[("pss", b)])
                        if kind == "halo":
                            bt = bhalo[:, br, :, :]
                        else:
                            bt = btab[:, br, :, c0:c0 + nq]
                        P.op("pe", lambda E, out=out, bt=bt: E.matmul(
                            out, lhsT=ident[:], rhs=bt, start=False, stop=True),
                             reads=["ident", ("btab", w), ("bhalo", w)], writes=[("pss", b)])
                        P.op("act", lambda E, b=b, nq=nq: E.activation(out=Pb[b][:, :, 0:nq], in_=ps_s[b][:, :, 0:nq],
                                                                      func=AF.Exp),
                             reads=[("pss", b)], writes=[("Pb", b)])

                    if OPT_ATT_NEW:
                        emit_bias(0)
                        emit_QK(0)
                        for ti in range(len(ktiles)):
                            if ti + 1 < len(ktiles):
                                emit_bias(ti + 1)
                            emit_PV(ti)
                            if ti + 1 < len(ktiles):
                                emit_QK(ti + 1)
                    else:
                        LA = NSB - 1
                        for t in range(LA):
                            emit_S_old(t)
                        for ti in range(len(ktiles)):
                            if ti + LA < len(ktiles):
                                emit_S_old(ti + LA)
                            emit_PV(ti)
                            if ti % 2 == 0:
                                conv_issue()
                    for h2 in range(2):
                        h = 2 * hp + h2
                        if OPT_ACT_RECIP:
                            P.op("act", lambda E, h2=h2: E.activation(out=acc[64:65, h2, :], in_=acc[64:65, h2, :], func=AF.Ln),
                                 reads=["acc"], writes=["acc"])
                            P.op("act", lambda E, h2=h2: E.activation(out=acc[64:65, h2, :], in_=acc[64:65, h2, :],
                                                                      func=AF.Exp, scale=-1.0),
                                 reads=["acc"], writes=["acc"])
                        else:
                            P.op("dve", lambda E, h2=h2: E.reciprocal(out=acc[64:65, h2, :], in_=acc[64:65, h2, :]),
                                 reads=["acc"], writes=["acc"])
                        for blk in range(4):
                            pp = ps_pr[ppi % NOB]
                            pres = ("pso", ppi % NOB)
                            ppi += 1
                            P.op("pe", lambda E, pp=pp, blk=blk, h2=h2: E.matmul(
                                pp[0:64, :], lhsT=onesf[64:65, :], rhs=acc[64:65, h2, 512 * blk:512 * (blk + 1)],
                                start=True, stop=True, tile_position=(64, 0)), reads=["onesf", "acc"], writes=[pres])
                            P.op("dve", lambda E, pp=pp, blk=blk, h=h, h2=h2: E.tensor_tensor(
                                out=mixA[:, h, 512 * blk:512 * (blk + 1)], in0=acc[0:64, h2, 512 * blk:512 * (blk + 1)],
                                in1=pp[0:64, :], op=ALU.mult), reads=[pres, "acc"], writes=[("mixA", h, blk)])
                while conv_jobs:
                    conv_issue()
                P.emit(defer_cv=True)
        if debug:
            P.dma("sp", dbg["mixT"].rearrange("p (c t) -> p c t", c=8)[:, 0:4, :], actT[:], key="dbg")
            P.emit()
            P.dma("sp", dbg["mixA"].rearrange("p (c t) -> p c t", c=8), mixA[:], key="dbg")
            P.emit()

        with ExitStack() as sR:
            TR = lambda name, shape, dt: sR.enter_context(nc.sbuf_tensor("sb_" + name, list(shape), dt))
            x1 = TR("x1", [128, NT, D], F32)
            g_b = TR("g_b2", [128, D], F32)
            junk = TR("junk2", [128, D], F32)
            ssq = TR("ssq2", [128, NT], F32)
            rstd = TR("rstd2", [128, NT], F32)
            hns = [TR("hnb%d" % i, [128, D], BF16) for i in range(4)]

            with ExitStack() as sB:
                TB = lambda name, shape, dt: sB.enter_context(nc.sbuf_tensor("sb_" + name, list(shape), dt))
                wo = TB("wo", [128, 4, D], BF16)
                woA = TB("woA", [64, 8, D], BF16)
                ps = [sB.enter_context(nc.psum_tensor("psB%d" % i, [128, 512], F32)) for i in range(4)]
                P.dma("pool", wo[:], w_out_v[:, 0:4, :], writes=["wo"], key="wo")
                P.dma("pool", woA[:], w_out[512:1024, :].rearrange("(h d) n -> d h n", d=64), writes=["wo"], key="wk")
                for i in range(NT):
                    P.dma("sp", x1[:, i, :], xh[2048 + 128 * i:2048 + 128 * (i + 1), :], writes=[("x1", i)],
                          key=("x1", i % 4))
                pi = 0
                for i in range(NT):
                    for half in range(2):
                        pp = ps[pi % 4]
                        pres = ("psB", pi % 4)
                        pi += 1
                        for kc in range(4):
                            P.op("pe", lambda E, pp=pp, kc=kc, i=i, half=half: E.matmul(
                                pp[:], lhsT=actT[:, kc, 128 * i:128 * (i + 1)], rhs=wo[:, kc, 512 * half:512 * (half + 1)],
                                start=(kc == 0), stop=False), reads=["wo", "mixT"], writes=[pres])
                        for h in range(8):
                            P.op("pe", lambda E, pp=pp, h=h, i=i, half=half: E.matmul(
                                pp[:], lhsT=mixA[:, h, 128 * i:128 * (i + 1)], rhs=woA[:, h, 512 * half:512 * (half + 1)],
                                start=False, stop=(h == 7)), reads=["wo", "mixT"], writes=[pres])
                        P.op("dve", lambda E, pp=pp, i=i, half=half: E.tensor_tensor(
                            out=x1[:, i, 512 * half:512 * (half + 1)], in0=x1[:, i, 512 * half:512 * (half + 1)],
                            in1=pp[:], op=ALU.add), reads=[pres, ("x1", i)], writes=[("x1", i)])
                P.emit(defer_cv=True)
            if debug:
                P.dma("sp", dbg["x1"].rearrange("p (c t) -> p c t", c=NT), x1[:], key="dbg")
                P.emit()

            with ExitStack() as sC1:
                TC1 = lambda name, shape, dt: sC1.enter_context(nc.sbuf_tensor("sb_" + name, list(shape), dt))
                hn2T = TC1("hn2T", [128, 8, 2048], BF16)
                gates = TC1("gates", [128, NT, NE], F32)
                gates_bf = TC1("gates_bf", [128, NT, NE], BF16)
                gT = TC1("gT", [NE, NT, 128], BF16)
                bdn = TC1("bdn", [NE, D], BF16)
                wr = TC1("wr", [128, 8, NE], BF16)
                brb = TC1("brb", [128, NE], F32)
                ltri = TC1("ltri", [128, 128], BF16)
                ones128 = TC1("ones128", [128, 128], BF16)
                iota4 = TC1("iota4", [128, 4, NE], F32)
                ebase = TC1("ebase", [128, NE], F32)
                carry = TC1("carry", [128, NE], F32)
                lg = TC1("lg", [128, NE], F32)
                m8 = TC1("m8", [128, 8], F32)
                idx8 = TC1("idx8", [128, 8], mybir.dt.uint32)
                ef = TC1("ef", [128, 4], F32)
                negm = TC1("negm", [128, 1], F32)
                ex = TC1("ex", [128, NE], F32)
                msk = TC1("msk", [128, NE], F32)
                mskb = TC1("mskb", [128, NE], BF16)
                ssum = TC1("ssum", [128, 1], F32)
                posf = TC1("posf", [128, NE], F32)
                ovf = TC1("ovf", [128, NE], F32)
                oh4 = TC1("oh4", [128, 4, NE], F32)
                pr4 = TC1("pr4", [128, 4, NE], F32)
                slotf = TC1("slotf", [128, 4], F32)
                g4 = TC1("g4", [128, 4], F32)
                ps_tr = [sC1.enter_context(nc.psum_tensor("pstrC%d" % i, [128, 8, 128], BF16)) for i in range(2)]
                ps_l = sC1.enter_context(nc.psum_tensor("psl", [128, NE], F32))
                ps_pos = sC1.enter_context(nc.psum_tensor("pspos", [128, NE], F32))
                ps_cnt = sC1.enter_context(nc.psum_tensor("pscnt", [128, NE], F32))
                ps_g = sC1.enter_context(nc.psum_tensor("psgT", [NE, 128], BF16))
                ps_b = [sC1.enter_context(nc.psum_tensor("psbd%d" % i, [128, 512], F32)) for i in range(2)]
                P.dma("sp", g_b[:], g_ffn_in[:, :], writes=["gains"], key=P.ckey())
                P.dma("pool", wr[:], w_router_v, writes=["wr"], key="wr")
                P.dma("sp", brb[:], brouter_in[:, :], writes=["brb"], key=P.ckey())
                P.dma("pool", bdn[:], b_down[:, :], writes=["bdn"], key=P.ckey())
                P.dma("pool", ltri[:], ltri_in[:, :], writes=["ltri"], key=P.ckey())
                P.dma("sp", iota4[:], iota4_in.rearrange("p (k e) -> p k e", k=4), writes=["iota4"], key=P.ckey())
                P.dma("sp", ebase[:], ebase_in[:, :], writes=["ebase"], key=P.ckey())
                P.op("pool", lambda E: E.memset(ones128[:], 1.0), writes=["ones128"])
                P.op("pool", lambda E: E.memset(carry[:], 0.0), writes=["carry"])

                def route(i, hb, hres):
                    for kc in range(8):
                        P.op("pe", lambda E, kc=kc, i=i: E.matmul(
                            ps_l[:], lhsT=hn2T[:, kc, 128 * i:128 * (i + 1)], rhs=wr[:, kc, :],
                            start=(kc == 0), stop=(kc == 7)), reads=["wr", ("T", i)], writes=["psl"])
                    P.op("dve", lambda E: E.tensor_tensor(out=lg[:], in0=ps_l[:], in1=brb[:], op=ALU.add),
                         reads=["psl", "brb"], writes=["lg"])
                    P.op("dve", lambda E: E.max(out=m8[:], in_=lg[:]), reads=["lg"], writes=["m8"])
                    P.op("dve", lambda E: E.max_index(out=idx8[:], in_max=m8[:], in_values=lg[:]),
                         reads=["lg", "m8"], writes=["idx8"])
                    P.op("dve", lambda E: E.tensor_copy(out=ef[:], in_=idx8[:, 0:4]), reads=["idx8"], writes=["ef"])
                    P.op("dve", lambda E: E.tensor_scalar(out=negm[:], in0=m8[:, 0:1], scalar1=-1.0, scalar2=None,
                                                          op0=ALU.mult), reads=["m8"], writes=["negm"])
                    P.op("dve", lambda E: E.tensor_scalar(out=msk[:], in0=lg[:], scalar1=m8[:, 3:4], scalar2=None,
                                                          op0=ALU.is_ge), reads=["lg", "m8"], writes=["msk"])
                    P.op("dve", lambda E: E.tensor_copy(out=mskb[:], in_=msk[:]), reads=["msk"], writes=["mskb"])
                    P.op("act", lambda E: E.activation(out=ex[:], in_=lg[:], func=AF.Exp, bias=negm[:], scale=1.0),
                         reads=["lg", "negm"], writes=["ex"])
                    P.op("dve", lambda E: E.tensor_tensor(out=ex[:], in0=ex[:], in1=msk[:], op=ALU.mult),
                         reads=["ex", "msk"], writes=["ex"])
                    P.op("dve", lambda E: E.reduce_sum(out=ssum[:], in_=ex[:], axis=mybir.AxisListType.X),
                         reads=["ex"], writes=["ssum"])
                    P.op("dve", lambda E: E.reciprocal(out=ssum[:], in_=ssum[:]), reads=["ssum"], writes=["ssum"])
                    P.op("dve", lambda E, i=i: E.tensor_scalar(out=gates[:, i, :], in0=ex[:], scalar1=ssum[:, 0:1],
                                                               scalar2=None, op0=ALU.mult),
                         reads=["ex", "ssum"], writes=[("gates", i)])
                    P.op("dve", lambda E, i=i: E.tensor_copy(out=gates_bf[:, i, :], in_=gates[:, i, :]),
                         reads=[("gates", i)], writes=[("gates_bf", i)])
                    P.op("pe", lambda E, i=i: E.transpose(ps_g[:], gates_bf[:, i, :], ident[:]),
                         reads=[("gates_bf", i), "ident"], writes=["psgT"])
                    P.op("act", lambda E, i=i: E.copy(out=gT[:, i, :], in_=ps_g[:]), reads=["psgT"],
                         writes=[("gT", i)])
                    P.op("pe", lambda E: E.matmul(ps_pos[:], lhsT=ltri[:], rhs=mskb[:], start=True, stop=True),
                         reads=["ltri", "mskb"], writes=["pspos"])
                    P.op("pe", lambda E: E.matmul(ps_cnt[:], lhsT=ones128[:], rhs=mskb[:], start=True, stop=True),
                         reads=["ones128", "mskb"], writes=["pscnt"])
                    P.op("dve", lambda E: E.tensor_tensor(out=posf[:], in0=ps_pos[:], in1=carry[:], op=ALU.add),
                         reads=["pspos", "carry"], writes=["posf"])
                    P.op("dve", lambda E: E.tensor_tensor(out=carry[:], in0=ps_cnt[:], in1=carry[:], op=ALU.add),
                         reads=["pscnt", "carry", "posf"], writes=["carry"])
                    P.op("dve", lambda E: E.tensor_scalar(out=ovf[:], in0=posf[:], scalar1=float(CAP), scalar2=BIGSLOT,
                                                          op0=ALU.is_ge, op1=ALU.mult), reads=["posf"], writes=["ovf"])
                    P.op("dve", lambda E: E.tensor_tensor(out=posf[:], in0=posf[:], in1=ebase[:], op=ALU.add),
                         reads=["posf", "ebase", "ovf"], writes=["posf"])
                    P.op("dve", lambda E: E.tensor_tensor(out=posf[:], in0=posf[:], in1=ovf[:], op=ALU.add),
                         reads=["posf", "ovf"], writes=["posf"])
                    P.op("dve", lambda E: E.tensor_tensor(
                        out=oh4[:], in0=iota4[:], in1=ef[:].unsqueeze(2).to_broadcast([128, 4, NE]), op=ALU.is_equal),
                         reads=["iota4", "ef"], writes=["oh4"])
                    P.op("dve", lambda E: E.tensor_tensor(
                        out=pr4[:], in0=oh4[:], in1=posf[:].unsqueeze(1).to_broadcast([128, 4, NE]), op=ALU.mult),
                         reads=["oh4", "posf"], writes=["pr4"])
                    P.op("dve", lambda E: E.reduce_sum(out=slotf[:], in_=pr4[:], axis=mybir.AxisListType.X),
                         reads=["pr4"], writes=["slotf"])
                    P.op("dve", lambda E, i=i: E.tensor_copy(out=slot_i32[:, i, :], in_=slotf[:]),
                         reads=["slotf"], writes=[("slot", i)])
                    P.op("dve", lambda E, i=i: E.tensor_tensor(
                        out=pr4[:], in0=oh4[:], in1=gates[:, i, :].unsqueeze(1).to_broadcast([128, 4, NE]), op=ALU.mult),
                         reads=["oh4", ("gates", i), "pr4"], writes=["pr4"])
                    P.op("dve", lambda E: E.reduce_sum(out=g4[:], in_=pr4[:], axis=mybir.AxisListType.X),
                         reads=["pr4"], writes=["g4"])
                    P.op("dve", lambda E, i=i: E.tensor_scalar(out=g4n[:, i, :], in0=g4[:], scalar1=-1.0, scalar2=None,
                                                               op0=ALU.mult), reads=["g4"], writes=[("g4n", i)])
                    for k in range(4):
                        P.op("pool", lambda E, i=i, k=k, hb=hb: E.indirect_dma_start(
                            out=xs[:, :], out_offset=bass.IndirectOffsetOnAxis(ap=slot_i32[:, i, k:k + 1], axis=0),
                            in_=hb[:], in_offset=None, bounds_check=P.bc_reg(E, NE * CAP - 1), oob_is_err=False),
                             reads=[hres, ("slot", i)], writes=[], dma_key=("scat", i % 2, k))

                rms_tiles(lambda i: (x1[:, i, :], ("x1", i)), NT, g_b, hn2T, 0, "C1", None,
                          (junk, ssq, rstd), hns, ps_tr, post=route)
                for i in range(NT):
                    for half in range(2):
                        pp = ps_b[(2 * i + half) % 2]
                        pres = ("psbd", (2 * i + half) % 2)
                        P.op("pe", lambda E, pp=pp, i=i, half=half: E.matmul(
                            pp[:], lhsT=gT[:, i, :], rhs=bdn[:, 512 * half:512 * (half + 1)], start=True, stop=True),
                             reads=[("gT", i), "bdn"], writes=[pres])
                        P.op("dve", lambda E, pp=pp, i=i, half=half: E.tensor_tensor(
                            out=x1[:, i, 512 * half:512 * (half + 1)], in0=x1[:, i, 512 * half:512 * (half + 1)],
                            in1=pp[:], op=ALU.add), reads=[pres, ("x1", i)], writes=[("x1", i)])
                    P.dma("sp", x1_spill[:, D * i:D * (i + 1)], x1[:, i, :], reads=[("x1", i)], key=("spill", i % 4))
                P.emit()
                if debug:
                    P.dma("sp", dbg["gates"].rearrange("p (c t) -> p c t", c=NT), gates[:], key="dbg")
                    P.emit()

        sAB.close()
        with ExitStack() as sC2:
            TC2 = lambda name, shape, dt: sC2.enter_context(nc.sbuf_tensor("sb_" + name, list(shape), dt))
            NRING = 8
            ring = [TC2("ring%d" % i, [128, 8, 512], BF16) for i in range(NRING)]
            bgu = TC2("bgu", [128, NE, 16], F32)
            xes = [TC2("xe%d" % i, [128, CAP // 128, D], BF16) for i in range(2)]
            xeTs = [TC2("xeT%d" % i, [128, 8, CAP], BF16) for i in range(2)]
            act_es = [TC2("act_e%d" % i, [128, 8, CAP], BF16) for i in range(2)]
            rs = [TC2("r%d" % i, [128, CAP], F32) for i in range(2)]
            sgs = [TC2("sg%d" % i, [128, CAP], F32) for i in range(2)]
            ucs = [TC2("uc%d" % i, [128, CAP], F32) for i in range(2)]
            yts = [TC2("yt%d" % i, [128, D], F32) for i in range(3)]
            ps_gu = [sC2.enter_context(nc.psum_tensor("psgu%d" % i, [128, 512], F32)) for i in range(4)]
            ps_d = [sC2.enter_context(nc.psum_tensor("psd%d" % i, [128, 512], F32)) for i in range(2)]
            ps_tr = [sC2.enter_context(nc.psum_tensor("pstrE%d" % i, [128, 8, 128], BF16)) for i in range(2)]
            P.dma("sp", bgu[:], bgu_in.rearrange("p (e c) -> p e c", e=NE), writes=["bgu"], key=P.ckey())
            P.op("dve", lambda E: E.tensor_scalar(out=bgu[:, :, 0:8], in0=bgu[:, :, 0:8], scalar1=-1.0, scalar2=7.0,
                                                  op0=ALU.mult, op1=ALU.add), reads=["bgu"], writes=["bgu"])
            P.op("dve", lambda E: E.tensor_scalar(out=bgu[:, :, 8:16], in0=bgu[:, :, 8:16], scalar1=1.0, scalar2=None,
                                                  op0=ALU.add), reads=["bgu"], writes=["bgu"])
            pieces = []
            for e in range(NE):
                for j in range(2):
                    pieces.append((e, "g", j))
                    pieces.append((e, "u", j))
                for half in range(2):
                    pieces.append((e, "dn", half))

            def piece_dma(n):
                e, kind, j = pieces[n]
                slot = n % NRING
                wg_e = wgu_bf[e] if e < NCONV else w_gu[e]
                wd_e = wdn_bf[e] if e < NCONV else w_down[e]
                for part in range(2):
                    if kind in ("g", "u"):
                        c0 = (0 if kind == "g" else 1024) + 512 * j
                        src = wg_e.rearrange("(kc p) f -> p kc f", p=128)[:, 4 * part:4 * (part + 1), c0:c0 + 512]
                    else:
                        src = wd_e.rearrange("(kc p) n -> p kc n", p=128)[
                            :, 4 * part:4 * (part + 1), 512 * j:512 * (j + 1)]
                    dst = ring[slot][:, 4 * part:4 * (part + 1), :]
                    P.dma("pool", dst, src, writes=[("ring", slot, part)], key=("ring", slot, part))

            def xe_load(e):
                b = e % 2
                P.dma("sp", xes[b][:], xs[e * CAP:(e + 1) * CAP, :].rearrange("(j p) d -> p j d", p=128),
                      writes=[("xe", b)], key=("xe", b))

            LOOK = NRING - 2
            for n in range(min(LOOK, len(pieces))):
                piece_dma(n)
            xe_load(0)
            gi = 0
            di = 0
            ei = 0
            ti = 0
            yi = 0
            for n, (e, kind, j) in enumerate(pieces):
                if n + LOOK < len(pieces):
                    piece_dma(n + LOOK)
                slot = n % NRING
                rg = ring[slot]
                b = e % 2
                xeT, act_e = xeTs[b], act_es[b]
                if kind == "g" and j == 0:
                    if e + 1 < NE:
                        xe_load(e + 1)
                    for jj in range(CAP // 128):
                        pt, ptres = ps_tr[ti % 2], ("pstrE", ti % 2)
                        ti += 1
                        for kc in range(8):
                            P.op("pe", lambda E, pt=pt, kc=kc, jj=jj, b=b: E.transpose(
                                pt[:, kc, :], xes[b][:, jj, 128 * kc:128 * (kc + 1)], ident[:]),
                                 reads=[("xe", b), "ident"], writes=[ptres])
                        P.op("act", lambda E, pt=pt, jj=jj, xeT=xeT: E.copy(out=xeT[:, :, 128 * jj:128 * (jj + 1)], in_=pt[:]),
                             reads=[ptres], writes=[("xeT", b)])
                if kind == "g":
                    continue
                if kind == "u":
                    slot_g = (n - 1) % NRING
                    rgg = ring[slot_g]
                    for c in range(4):
                        fc = 4 * j + c
                        pg, pgres = ps_gu[gi % 4], ("psgu", gi % 4)
                        gi += 1
                        pu, pures = ps_gu[gi % 4], ("psgu", gi % 4)
                        gi += 1
                        for (pp, pres, wt, wslot) in ((pg, pgres, rgg, slot_g), (pu, pures, rg, slot)):
                            for kc in range(8):
                                P.op("pe", lambda E, pp=pp, wt=wt, kc=kc, c=c, xeT=xeT: E.matmul(
                                    pp[:, 0:CAP], lhsT=wt[:, kc, 128 * c:128 * (c + 1)],
                                    rhs=xeT[:, kc, :], start=(kc == 0), stop=(kc == 7)),
                                     reads=[("ring", wslot, kc // 4), ("xeT", b)], writes=[pres])
                        k2 = ei % 2
                        ei += 1
                        r, sg, uc = rs[k2], sgs[k2], ucs[k2]
                        P.op("act", lambda E, pg=pg, r=r, e=e, fc=fc: E.activation(
                            out=r[:], in_=pg[:, 0:CAP], func=AF.Relu, bias=bgu[:, e, fc:fc + 1], scale=-1.0),
                             reads=[pgres, "bgu"], writes=[("r", k2)])
                        P.op("act", lambda E, r=r, sg=sg: E.activation(out=sg[:], in_=r[:], func=AF.Sigmoid, bias=c119[:],
                                                                       scale=-1.702),
                             reads=[("r", k2), "c119"], writes=[("sg", k2)])
                        P.op("dve", lambda E, r=r, sg=sg: E.scalar_tensor_tensor(
                            out=r[:], in0=r[:], scalar=7.0, in1=sg[:], op0=ALU.subtract, op1=ALU.mult),
                             reads=[("r", k2), ("sg", k2)], writes=[("r", k2)])
                        P.op("dve", lambda E, pu=pu, uc=uc, e=e, fc=fc: E.tensor_scalar(
                            out=uc[:], in0=pu[:, 0:CAP], scalar1=bgu[:, e, 8 + fc:9 + fc], scalar2=8.0, op0=ALU.add,
                            op1=ALU.min), reads=[pures, "bgu"], writes=[("uc", k2)])
                        P.op("dve", lambda E, r=r, uc=uc, fc=fc, act_e=act_e: E.scalar_tensor_tensor(
                            out=act_e[:, fc, :], in0=uc[:], scalar=-6.0, in1=r[:], op0=ALU.max, op1=ALU.mult),
                             reads=[("r", k2), ("uc", k2)], writes=[("act_e", b, fc)])
                else:
                    half = j
                    for jj in range(CAP // 128):
                        pp, pres = ps_d[di % 2], ("psd", di % 2)
                        di += 1
                        for fc in range(8):
                            P.op("pe", lambda E, pp=pp, rg=rg, fc=fc, jj=jj, act_e=act_e: E.matmul(
                                pp[:], lhsT=act_e[:, fc, 128 * jj:128 * (jj + 1)], rhs=rg[:, fc, :],
                                start=(fc == 0), stop=(fc == 7)),
                                 reads=[("ring", slot, fc // 4), ("act_e", b, fc)], writes=[pres])
                        yt = yts[jj]
                        P.op("act", lambda E, pp=pp, yt=yt, half=half: E.copy(out=yt[:, 512 * half:512 * (half + 1)],
                                                                              in_=pp[:]),
                             reads=[pres], writes=[("yt", jj, half)])
                        if half == 1:
                            P.dma("sp", ys[e * CAP + 128 * jj:e * CAP + 128 * (jj + 1), :], yt[:],
                                  reads=[("yt", jj, 0), ("yt", jj, 1)], key=("ys", jj))
            P.emit()

        with ExitStack() as sR:
            TR = lambda name, shape, dt: sR.enter_context(nc.sbuf_tensor("sb_" + name, list(shape), dt))
            x1 = TR("x1b", [128, NT, D], F32)
            g_b = TR("g_b3", [128, D], F32)
            junk = TR("junk3", [128, D], F32)
            ssq = TR("ssq3", [128, NT], F32)
            rstd = TR("rstd3", [128, NT], F32)
            hns = [TR("hnc%d" % i, [128, D], BF16) for i in range(4)]
            with ExitStack() as sD:
                TD = lambda name, shape, dt: sD.enter_context(nc.sbuf_tensor("sb_" + name, list(shape), dt))
                hn3T = TD("hn3T", [128, 8, 2048], BF16)
                wpg = TD("wpg", [128, 8, D], BF16)
                wpp = TD("wpp", [128, 2, D], BF16)
                gfin = TD("gfin", [128, D], F32)
                pts = [TD("pt%d" % i, [128, 256], BF16) for i in range(NT)]
                pTs = [TD("pT%d" % i, [128, 2, 128], BF16) for i in range(2)]
                sgs = [TD("sgD%d" % i, [128, 512], F32) for i in range(2)]
                outs = [TD("outD%d" % i, [128, D], F32) for i in range(2)]
                ssq2 = TD("ssqD", [128, NT], F32)
                rstd2 = TD("rstdD", [128, NT], F32)
                ps_tr = [sD.enter_context(nc.psum_tensor("pstrD%d" % i, [128, 8, 128], BF16)) for i in range(3)]
                ps_pt = sD.enter_context(nc.psum_tensor("pspt", [128, 2, 128], BF16))
                ps_g = [sD.enter_context(nc.psum_tensor("psDg%d" % i, [128, 512], F32)) for i in range(2)]
                ps_p = [sD.enter_context(nc.psum_tensor("psDp%d" % i, [128, 512], F32)) for i in range(2)]
                P.dma("sp", g_b[:], g_ple_in[:, :], writes=["gains"], key=P.ckey())
                P.dma("sp", gfin[:], g_fin_in[:, :], writes=["gfin"], key=P.ckey())
                P.dma("pool", wpg[:], w_pg_v, writes=["wpg"], key="wpg")
                P.dma("pool", wpp[:], w_pp_v, writes=["wpp"], key="wpp")
                for i in range(NT):
                    P.dma("pool", pts[i][:], p_in[128 * i:128 * (i + 1), :], writes=[("pt", i)], key=("pt", i % 2))
                yks = [TD("yk%d" % i, [128, D], F32) for i in range(8)]
                for i in range(8):
                    P.op("pool", lambda E, i=i: E.memset(yks[i][:], 0.0), writes=[("yk", i)])
                for i in range(NT):
                    P.dma("sp", x1[:, i, :], x1_spill[:, D * i:D * (i + 1)], writes=[("x1", i)], key=("x1", i % 4))

                def combine_tile(i):
                    for k in range(4):
                        q = (4 * i + k) % 8
                        P.op("pool", lambda E, i=i, k=k, q=q: E.indirect_dma_start(
                            out=yks[q][:], out_offset=None, in_=ys[:, :],
                            in_offset=bass.IndirectOffsetOnAxis(ap=slot_i32[:, i, k:k + 1], axis=0),
                            bounds_check=P.bc_reg(E, NE * CAP - 1), oob_is_err=False),
                             reads=[], writes=[("yk", q)], dma_key=("yk", q))
                        P.op("dve", lambda E, i=i, k=k, q=q: E.scalar_tensor_tensor(
                            out=x1[:, i, :], in0=yks[q][:], scalar=g4n[:, i, k:k + 1], in1=x1[:, i, :],
                            op0=ALU.mult, op1=ALU.add), reads=[("yk", q), ("x1", i)], writes=[("x1", i)])

                def pre_combine(i):
                    if debug:
                        return
                    if i == 0:
                        combine_tile(0)
                    if i + 1 < NT:
                        combine_tile(i + 1)

                if debug:
                    for i in range(NT):
                        combine_tile(i)
                if debug:
                    P.emit()
                if debug:
                    P.dma("sp", dbg["x2"].rearrange("p (c t) -> p c t", c=NT), x1[:], key="dbg")
                    P.emit()

                pi_box = [0]

                def ple_tile(i, hb_unused, hres_unused):
                    b = i % 2
                    for c in range(2):
                        P.op("pe", lambda E, i=i, c=c: E.transpose(ps_pt[:, c, :], pts[i][:, 128 * c:128 * (c + 1)], ident[:]),
                             reads=[("pt", i), "ident"], writes=["pspt"])
                    P.op("act", lambda E, b=b: E.copy(out=pTs[b][:], in_=ps_pt[:]), reads=["pspt"], writes=[("pT", b)])
                    for half in range(2):
                        k2 = pi_box[0] % 2
                        pi_box[0] += 1
                        pg, pgres = ps_g[k2], ("psDg", k2)
                        pq, pqres = ps_p[k2], ("psDp", k2)
                        for kc in range(8):
                            P.op("pe", lambda E, pg=pg, kc=kc, i=i, half=half: E.matmul(
                                pg[:], lhsT=hn3T[:, kc, 128 * i:128 * (i + 1)], rhs=wpg[:, kc, 512 * half:512 * (half + 1)],
                                start=(kc == 0), stop=(kc == 7)), reads=["wpg", ("T", i)], writes=[pgres])
                        for c in range(2):
                            P.op("pe", lambda E, pq=pq, c=c, b=b, half=half: E.matmul(
                                pq[:], lhsT=pTs[b][:, c, :], rhs=wpp[:, c, 512 * half:512 * (half + 1)],
                                start=(c == 0), stop=(c == 1)), reads=["wpp", ("pT", b)], writes=[pqres])
                        sg = sgs[k2]
                        P.op("act", lambda E, pg=pg, sg=sg: E.activation(out=sg[:], in_=pg[:], func=AF.Sigmoid),
                             reads=[pgres], writes=[("sgD", k2)])
                        P.op("dve", lambda E, pq=pq, sg=sg: E.tensor_tensor(out=sg[:], in0=sg[:], in1=pq[:], op=ALU.mult),
                             reads=[pqres, ("sgD", k2)], writes=[("sgD", k2)])
                        P.op("dve", lambda E, sg=sg, i=i, half=half: E.tensor_tensor(
                            out=x1[:, i, 512 * half:512 * (half + 1)], in0=x1[:, i, 512 * half:512 * (half + 1)],
                            in1=sg[:], op=ALU.add), reads=[("sgD", k2), ("x1", i)], writes=[("x1", i)])
                    ob = outs[b]
                    P.op("act", lambda E, i=i: E.activation(out=junk[:], in_=x1[:, i, :], func=AF.Square,
                                                            accum_out=ssq2[:, i:i + 1]),
                         reads=[("x1", i)], writes=["Djunk2", ("ssqD", i)])
                    P.op("act", lambda E, i=i: E.activation(out=rstd2[:, i:i + 1], in_=ssq2[:, i:i + 1], func=AF.Sqrt,
                                                            bias=epsb[:], scale=1.0 / D),
                         reads=[("ssqD", i), "epsb"], writes=[("rstdD", i)])
                    P.op("dve", lambda E, i=i: E.reciprocal(out=rstd2[:, i:i + 1], in_=rstd2[:, i:i + 1]),
                         reads=[("rstdD", i)], writes=[("rstdD", i)])
                    P.op("dve", lambda E, i=i, ob=ob: E.scalar_tensor_tensor(
                        out=ob[:], in0=x1[:, i, :], scalar=rstd2[:, i:i + 1], in1=gfin[:], op0=ALU.mult, op1=ALU.mult),
                         reads=[("x1", i), ("rstdD", i), "gfin"], writes=[("outD", b)])
                    P.dma("sp", y[128 * i:128 * (i + 1), :], ob[:], reads=[("outD", b)], key=("outD", b))
                def post_skewed(i, hb, hres):
                    if i >= 1:
                        ple_tile(i - 1, None, None)

                rms_tiles(lambda i: (x1[:, i, :], ("x1", i)), NT, g_b, hn3T, 0, "D", None,
                          (junk, ssq, rstd), hns, ps_tr, pre=pre_combine, post=post_skewed)
                ple_tile(NT - 1, None, None)
                P.emit()
    return nc


def _t5_bucket_np(dist):
    n = np.maximum(dist, 1).astype(np.float32)
    large = 16 + (np.log(n / 16) / math.log(2048 / 16) * 16).astype(np.int32)
    large = np.minimum(large, 31)
    return np.where(dist < 16, dist, large)


def _bias_tables(rel_bias, first_half):
    ki = np.arange(128)[:, None]
    qi = np.arange(128)[None, :]
    btab = np.full((128, 3, 8, 256), NEG, np.float32)
    for br, (window, dil) in enumerate(BRANCHES):
        relp = 128 + qi - ki
        idxp = _t5_bucket_np(np.maximum(relp, 0) * dil)
        relc = qi - ki
        idxc = _t5_bucket_np(np.maximum(relc, 0) * dil)
        for h in range(8):
            bp = rel_bias[idxp, h]
            bc = rel_bias[idxc, h]
            btab[:, br, h, 128:256] = np.where((relp >= 0) & (relp <= 128), bp, NEG)
            btab[:, br, h, 0:128] = np.where((relc >= 0) & (relc <= 128), bc, NEG)
    bhalo = btab[:, :, :, 128:256].copy()
    if first_half:
        bhalo[:] = NEG
    return btab.reshape(128, -1), np.ascontiguousarray(bhalo).reshape(128, -1)


_NC_CACHE = {}


def _prepare_in_maps(inputs):
    f = lambda a: np.ascontiguousarray(np.asarray(a, dtype=np.float32))
    x = f(inputs["x"])
    p = f(inputs["p"])[0]
    rb = f(inputs["rel_bias"])
    bc = lambda v: np.ascontiguousarray(np.broadcast_to(f(v).reshape(1, -1), (128, f(v).size)))
    shared = {
        "ident": np.eye(128, dtype=np.float32),
        "ltri": np.triu(np.ones((128, 128), np.float32), 1),
        "iota4": np.ascontiguousarray(np.broadcast_to(np.tile(np.arange(NE, dtype=np.float32), 4)[None, :], (128, 4 * NE))),
        "ebase": np.ascontiguousarray(np.broadcast_to((np.arange(NE, dtype=np.float32) * CAP)[None, :], (128, NE))),
        "g_mix_b": bc(inputs["g_mix"][0]),
        "g_ffn_b": bc(inputs["g_ffn"][0]),
        "g_ple_b": bc(inputs["g_ple"][0]),
        "g_fin_b": bc(inputs["g_final"]),
        "w_in": f(inputs["w_in"])[0],
        "w_pool": f(inputs["w_pool"])[0],
        "pscale": np.ascontiguousarray(f(inputs["pool_scale"])[0].reshape(4, 128).T),
        "w_out": f(inputs["w_out"])[0],
        "w_router": f(inputs["w_router"])[0],
        "b_router_b": bc(inputs["b_router"][0]),
        "w_gate_up": f(inputs["w_gate_up"])[0],
        "bgu": np.ascontiguousarray(f(inputs["b_gate_up"])[0].reshape(NE, 16, 128).transpose(2, 0, 1)).reshape(128, -1),
        "w_down": f(inputs["w_down"])[0],
        "b_down": f(inputs["b_down"])[0],
        "w_ple_gate": f(inputs["w_ple_gate"])[0],
        "w_ple_proj": f(inputs["w_ple_proj"])[0],
    }
    tabs = {fh: _bias_tables(rb, fh) for fh in (True, False)}
    in_maps = []
    for c in range(NCORES):
        b, half = c // 2, c % 2
        base = half * S_OWN
        xh = np.zeros((4096, D), np.float32)
        if half == 1:
            xh[0:2048] = x[b, 0:2048]
        xh[2048:4096] = x[b, base:base + S_OWN]
        pos = base + np.arange(16)
        invcnt = np.stack([1.0 / np.minimum(pos + 1, w) for w in (2, 4, 8, 16)]).astype(np.float32)
        m = dict(shared)
        m["xh"] = xh
        m["p"] = np.ascontiguousarray(p[b, base:base + S_OWN])
        m["btab"], m["bhalo"] = tabs[half == 0]
        m["invcnt"] = np.ascontiguousarray(np.broadcast_to(invcnt.reshape(1, -1), (128, 64)))
        in_maps.append(m)
    return in_maps


def kernel(**inputs):
    in_maps = _prepare_in_maps(inputs)
    if "nc" not in _NC_CACHE:
        _NC_CACHE["nc"] = build_program(debug=False)
    nc = _NC_CACHE["nc"]
    res = run_bass_kernel_spmd(nc, in_maps, core_ids=list(range(NCORES)))
    out = np.zeros((4, 4096, D), np.float32)
    for c in range(NCORES):
        b, half = c // 2, c % 2
        out[b, half * S_OWN:(half + 1) * S_OWN] = np.asarray(res.results[c]["y"], dtype=np.float32)
    return out
```

```python
import math
from contextlib import ExitStack

import numpy as np
import concourse.bass as bass
import concourse.mybir as mybir
from concourse.bass_utils import run_bass_kernel_spmd

F32 = mybir.dt.float32
BF16 = mybir.dt.bfloat16
AF = mybir.ActivationFunctionType
ALU = mybir.AluOpType

NCORES = 8
D = 1024
S_OWN = 2048
NT = 16
NE = 32
NEG = -1e30
CAP = 384
BIGSLOT = 1.0e6
NCONV = 12
OPT_ACT_RECIP = True
OPT_ATT_NEW = False
EPS = 1e-6
BRANCHES = ((128, 1), (512, 4), (2048, 16))


class Op:
    __slots__ = ("eng", "fn", "deps", "signal", "count", "dma_sem", "is_dma")

    def __init__(self, eng, fn, dma_sem=None):
        self.eng = eng
        self.fn = fn
        self.deps = []
        self.signal = False
        self.count = 0
        self.dma_sem = dma_sem
        self.is_dma = dma_sem is not None


class Prog:
    ENGS = ("pe", "act", "dve", "pool", "sp")

    def __init__(self, nc, stack):
        self.nc = nc
        self.stack = stack
        self.esem = {e: stack.enter_context(nc.semaphore("sem_" + e)) for e in self.ENGS}
        self.ecount = {e: 0 for e in self.ENGS}
        self.dsem = {}
        self.dcount = {}
        self.waited = {e: {} for e in self.ENGS}
        self.begin()

    def bc_reg(self, E, val):
        if self._bc is None:
            self._bc = E.to_reg(val)
        return self._bc

    def begin(self):
        self._bc = None
        self.ops = []
        self.res_w = {}
        self.res_r = {}

    def _dma_sem(self, key):
        if key not in self.dsem:
            self.dsem[key] = self.stack.enter_context(self.nc.semaphore("dsem_%d" % len(self.dsem)))
            self.dcount[key] = 0
        return key

    def op(self, eng, fn, reads=(), writes=(), dma_key=None):
        o = Op(eng, fn, self._dma_sem(dma_key) if dma_key is not None else None)
        if dma_key is not None:
            writes = list(writes) + [("__key", dma_key)]
        deps = {}
        for r in reads:
            w = self.res_w.get(r)
            if w is not None:
                deps[id(w)] = w
        for r in writes:
            w = self.res_w.get(r)
            if w is not None:
                deps[id(w)] = w
            for rd in self.res_r.get(r, ()):
                deps[id(rd)] = rd
        for d in deps.values():
            if d is o:
                continue
            if d.eng == "pe" and eng == "pe" and not d.is_dma and not o.is_dma:
                continue
            o.deps.append(d)
            d.signal = True
        for r in reads:
            lst = self.res_r.setdefault(r, [])
            if not o.is_dma:
                lst[:] = [x for x in lst if x.is_dma or x.eng != eng]
            lst.append(o)
        for r in writes:
            self.res_w[r] = o
            self.res_r[r] = []
        self.ops.append(o)
        return o

    def dma(self, eng, out, in_, reads=(), writes=(), key=None):
        assert key is not None
        return self.op(eng, lambda E: E.dma_start(out=out, in_=in_), reads, writes, dma_key=key)

    def ckey(self):
        self._ck = (getattr(self, "_ck", 0) + 1) % 6
        return ("const", self._ck)

    def emit(self, defer_cv=False):
        nc = self.nc
        last = {}
        for o in self.ops:
            if not o.is_dma:
                last[o.eng] = o
        for o in last.values():
            o.signal = True
        for o in self.ops:
            if o.is_dma:
                o.signal = True
                self.dcount[o.dma_sem] += 16
                o.count = self.dcount[o.dma_sem]
            elif o.signal:
                self.ecount[o.eng] += 1
                o.count = self.ecount[o.eng]
        by_eng = {e: [o for o in self.ops if o.eng == e] for e in self.ENGS}
        final_e = dict(self.ecount)
        final_d = dict(self.dcount)

        def run(ename, E):
            waited = self.waited[ename]
            for o in by_eng[ename]:
                need = {}
                for d in o.deps:
                    if d.is_dma:
                        k = ("d", d.dma_sem)
                    else:
                        k = ("e", d.eng)
                    if d.count > need.get(k, 0):
                        need[k] = d.count
                for k, v in need.items():
                    if waited.get(k, 0) < v:
                        sem = self.dsem[k[1]] if k[0] == "d" else self.esem[k[1]]
                        E.wait_ge(sem, v)
                        waited[k] = v
                ins = o.fn(E)
                if o.signal:
                    if o.is_dma:
                        ins.then_inc(self.dsem[o.dma_sem], 16)
                    else:
                        ins.then_inc(self.esem[o.eng], 1)
            for e2, v in final_e.items():
                if v > 0 and waited.get(("e", e2), 0) < v:
                    E.wait_ge(self.esem[e2], v)
                    waited[("e", e2)] = v
            for k2, v in final_d.items():
                if defer_cv and isinstance(k2, tuple) and k2[0] == "cv":
                    continue
                if v > 0 and waited.get(("d", k2), 0) < v:
                    E.wait_ge(self.dsem[k2], v)
                    waited[("d", k2)] = v

        with nc.Block() as block:
            @block.tensor
            def _(E):
                run("pe", E)

            @block.scalar
            def _(E):
                run("act", E)

            @block.vector
            def _(E):
                run("dve", E)

            @block.gpsimd
            def _(E):
                run("pool", E)

            @block.sync
            def _(E):
                run("sp", E)
        self.begin()


def build_program(debug=False):
    nc = bass.Bass("TRN2", target_bir_lowering=False)

    def din(name, shape, dt=F32):
        return nc.dram_tensor(name, list(shape), dt, kind="ExternalInput").ap()

    xh = din("xh", [4096, D])
    p_in = din("p", [S_OWN, 256])
    ident_in = din("ident", [128, 128])
    btab_in = din("btab", [128, 3 * 8 * 256])
    bhalo_in = din("bhalo", [128, 3 * 8 * 128])
    invcnt_in = din("invcnt", [128, 4 * 16])
    g_mix_in = din("g_mix_b", [128, D])
    g_ffn_in = din("g_ffn_b", [128, D])
    g_ple_in = din("g_ple_b", [128, D])
    g_fin_in = din("g_fin_b", [128, D])
    w_in = din("w_in", [D, 2048])
    w_pool = din("w_pool", [4, 128, 128])
    pscale_in = din("pscale", [128, 4])
    w_out = din("w_out", [D, D])
    w_router = din("w_router", [D, NE])
    brouter_in = din("b_router_b", [128, NE])
    w_gu = din("w_gate_up", [NE, D, 2 * D])
    bgu_in = din("bgu", [128, NE * 16])
    w_down = din("w_down", [NE, D, D])
    b_down = din("b_down", [NE, D])
    w_pg = din("w_ple_gate", [D, D])
    w_pp = din("w_ple_proj", [256, D])
    ltri_in = din("ltri", [128, 128])
    iota4_in = din("iota4", [128, 4 * NE])
    ebase_in = din("ebase", [128, NE])
    y = nc.dram_tensor("y", [S_OWN, D], F32, kind="ExternalOutput").ap()
    xs = nc.dram_tensor("xs_scratch", [NE * CAP, D], BF16).ap()
    ys = nc.dram_tensor("ys_scratch", [NE * CAP, D], F32).ap()
    x1_spill = nc.dram_tensor("x1_spill", [128, NT * D], F32).ap()
    wgu_bf = nc.dram_tensor("wgu_bf16", [NCONV, D, 2 * D], BF16).ap()
    wdn_bf = nc.dram_tensor("wdn_bf16", [NCONV, D, D], BF16).ap()
    dbg = {}
    if debug:
        dbg["mixT"] = nc.dram_tensor("dbg_mixT", [128, 8 * 2048], BF16, kind="ExternalOutput").ap()
        dbg["mixA"] = nc.dram_tensor("dbg_mixA", [64, 8 * 2048], BF16, kind="ExternalOutput").ap()
        dbg["x1"] = nc.dram_tensor("dbg_x1", [128, NT * D], F32, kind="ExternalOutput").ap()
        dbg["gates"] = nc.dram_tensor("dbg_gates", [128, NT * NE], F32, kind="ExternalOutput").ap()
        dbg["x2"] = nc.dram_tensor("dbg_x2", [128, NT * D], F32, kind="ExternalOutput").ap()

    w_in_v = w_in.rearrange("(kc p) n -> p kc n", p=128)
    w_out_v = w_out.rearrange("(kc p) n -> p kc n", p=128)
    w_pg_v = w_pg.rearrange("(kc p) n -> p kc n", p=128)
    w_pp_v = w_pp.rearrange("(kc p) n -> p kc n", p=128)
    w_router_v = w_router.rearrange("(kc p) n -> p kc n", p=128)

    with ExitStack() as stack:
        P = Prog(nc, stack)
        T = lambda name, shape, dt: stack.enter_context(nc.sbuf_tensor("sb_" + name, list(shape), dt))
        ident = T("ident_sb", [128, 128], BF16)
        ones_bf = T("ones_bf", [128, 64], BF16)
        sAB = ExitStack()
        TAB = lambda name, shape, dt: sAB.enter_context(nc.sbuf_tensor("sb_" + name, list(shape), dt))
        epsb = T("epsb", [128, 1], F32)
        c119 = T("c119", [128, 1], F32)
        slot_i32 = T("slot_i32", [128, NT, 4], mybir.dt.int32)
        g4n = T("g4n", [128, NT, 4], F32)
        actT = TAB("actT", [128, 4, 2048], BF16)
        mixA = TAB("mixA", [64, 8, 2048], BF16)

        def rms_tiles(x_ap_of, ntiles, g_b, dstT, dst_col0, pfx, xt_res, stats, hn_bufs, ps_tr,
                      pre=None, post=None):
            junk, ssq, rstd = stats
            for i in range(ntiles):
                if pre is not None:
                    pre(i)
                xa, xres = x_ap_of(i)
                P.op("act", lambda E, xa=xa, i=i: E.activation(out=junk[:], in_=xa, func=AF.Square,
                                                                accum_out=ssq[:, i:i + 1]),
                     reads=[xres], writes=[pfx + "junk", (pfx + "ssq", i)])
                P.op("act", lambda E, i=i: E.activation(out=rstd[:, i:i + 1], in_=ssq[:, i:i + 1], func=AF.Sqrt,
                                                        bias=epsb[:], scale=1.0 / D),
                     reads=[(pfx + "ssq", i), "epsb"], writes=[(pfx + "rstd", i)])
                P.op("dve", lambda E, i=i: E.reciprocal(out=rstd[:, i:i + 1], in_=rstd[:, i:i + 1]),
                     reads=[(pfx + "rstd", i)], writes=[(pfx + "rstd", i)])
                hb = hn_bufs[i % len(hn_bufs)]
                hres = (pfx + "hn", i % len(hn_bufs))
                P.op("dve", lambda E, xa=xa, i=i, hb=hb: E.scalar_tensor_tensor(
                    out=hb[:], in0=xa, scalar=rstd[:, i:i + 1], in1=g_b[:], op0=ALU.mult, op1=ALU.mult),
                     reads=[xres, (pfx + "rstd", i), "gains"], writes=[hres])
                pt = ps_tr[i % len(ps_tr)]
                pres = (pfx + "pstr", i % len(ps_tr))
                for kc in range(8):
                    P.op("pe", lambda E, kc=kc, hb=hb, pt=pt: E.transpose(pt[:, kc, :], hb[:, kc * 128:(kc + 1) * 128],
                                                                          ident[:]),
                         reads=[hres, "ident"], writes=[pres])
                c0 = dst_col0 + 128 * i
                P.op("act", lambda E, pt=pt, c0=c0: E.copy(out=dstT[:, :, c0:c0 + 128], in_=pt[:]),
                     reads=[pres], writes=[("T", c0 // 128)])
                if post is not None:
                    post(i, hb, hres)

        P.dma("pool", ident[:], ident_in[:, :], writes=["ident"], key=P.ckey())
        P.op("pool", lambda E: E.memset(ones_bf[:], 1.0), writes=["ones"])
        P.op("pool", lambda E: E.memset(epsb[:], EPS), writes=["epsb"])
        P.op("pool", lambda E: E.memset(c119[:], 7.0 * 1.702), writes=["c119"])
        zero_jobs = list(range(NE * CAP // 1024))

        with ExitStack() as sA:
            TA = lambda name, shape, dt: sA.enter_context(nc.sbuf_tensor("sb_" + name, list(shape), dt))
            hnT = TA("hnT", [128, 8, 4096], BF16)

            with ExitStack() as s1:
                T1 = lambda name, shape, dt: s1.enter_context(nc.sbuf_tensor("sb_" + name, list(shape), dt))
                g_b = T1("g_b", [128, D], F32)
                NXT = 4
                xts = [T1("xt%d" % i, [128, D], F32) for i in range(NXT)]
                hns = [T1("hn%d" % i, [128, D], BF16) for i in range(4)]
                junk = T1("junk", [128, D], F32)
                ssq = T1("ssq", [128, 32], F32)
                rstd = T1("rstd", [128, 32], F32)
                ps_tr = [s1.enter_context(nc.psum_tensor("pstrA%d" % i, [128, 8, 128], BF16)) for i in range(4)]
                P.dma("sp", g_b[:], g_mix_in[:, :], writes=["gains"], key=P.ckey())
                def ld(i):
                    P.dma("sp", xts[i % NXT][:], xh[128 * i:128 * (i + 1), :], writes=[("xt", i % NXT)],
                          key=("xt", i % NXT))

                zt = T1("zeros", [128, 8192], BF16)
                P.op("pool", lambda E: E.memset(zt[:], 0.0), writes=["zt"])

                def pre(i):
                    if i == 0:
                        ld(0)
                        ld(1)
                        ld(2)
                    if i + 3 < 32:
                        ld(i + 3)
                    if i >= 2 and zero_jobs:
                        c = zero_jobs.pop()
                        P.dma("sp", xs[1024 * c:1024 * (c + 1), :].rearrange("(p r) d -> p (r d)", p=128), zt[:],
                              reads=["zt"], key=("zx", c % 4))
                rms_tiles(lambda i: (xts[i % NXT][:], ("xt", i % NXT)), 32, g_b, hnT, 0, "A1", None,
                          (junk, ssq, rstd), hns, ps_tr, pre=pre)
                P.emit()

            with ExitStack() as s2:
                T2 = lambda name, shape, dt: s2.enter_context(nc.sbuf_tensor("sb_" + name, list(shape), dt))
                wpc = T2("wpc", [128, 8, 512], BF16)
                wpl = T2("wpl", [128, 4, 128], BF16)
                psc = T2("psc", [128, 4], F32)
                icn = T2("icn", [128, 4, 16], F32)
                u = T2("u", [128, 2064], F32)
                sa = T2("sa", [128, 2064], F32)
                sb = T2("sb", [128, 2064], F32)
                pooled = T2("pooled", [128, 2048], BF16)
                t16 = T2("t16", [128, 16], F32)
                ps = [s2.enter_context(nc.psum_tensor("psA2_%d" % i, [128, 512], F32)) for i in range(2)]
                P.dma("pool", wpc[:], w_in_v[:, :, 0:512], writes=["wpc"], key="wpc")
                P.dma("pool", wpl[:], w_pool.rearrange("g c d -> c g d"), writes=["wpl"], key="wpl")
                P.dma("sp", psc[:], pscale_in[:, :], writes=["psc"], key=P.ckey())
                P.dma("sp", icn[:], invcnt_in.rearrange("p (g t) -> p g t", g=4), writes=["icn"], key=P.ckey())
                pi = 0
                for g in range(4):
                    w = (2, 4, 8, 16)[g]
                    for blk in range(5):
                        pp = ps[pi % 2]
                        pres = ("psA2", pi % 2)
                        pi += 1
                        if blk == 0:
                            c0, n, o0 = 2032, 16, 0
                        else:
                            c0, n, o0 = 2048 + 512 * (blk - 1), 512, 16 + 512 * (blk - 1)
                        for kc in range(8):
                            P.op("pe", lambda E, pp=pp, kc=kc, g=g, c0=c0, n=n: E.matmul(
                                pp[:, 0:n], lhsT=wpc[:, kc, g * 128:(g + 1) * 128], rhs=hnT[:, kc, c0:c0 + n],
                                start=(kc == 0), stop=(kc == 7)), reads=["wpc", "hnT"], writes=[pres])
                        P.op("act", lambda E, pp=pp, n=n, o0=o0: E.copy(out=u[:, o0:o0 + n], in_=pp[:, 0:n]),
                             reads=[pres], writes=["u"])
                    src, srcres = u, "u"
                    sh = 1
                    bufs = [(sa, "sa"), (sb, "sb")]
                    bi = 0
                    while sh < w:
                        dst, dres = bufs[bi % 2]
                        bi += 1
                        P.op("dve", lambda E, dst=dst, src=src, sh=sh: E.tensor_tensor(
                            out=dst[:, sh:2064], in0=src[:, sh:2064], in1=src[:, 0:2064 - sh], op=ALU.add),
                             reads=[srcres], writes=[dres])
                        src, srcres = dst, dres
                        sh *= 2
                    P.op("dve", lambda E, src=src, w=w: E.scalar_tensor_tensor(
                        out=pooled[:], in0=src[:, 16:2064], scalar=1.0 / w, in1=u[:, 16:2064],
                        op0=ALU.mult, op1=ALU.subtract), reads=[srcres, "u"], writes=["pooled"])
                    P.op("dve", lambda E, src=src, g=g: E.tensor_tensor(
                        out=t16[:], in0=src[:, 16:32], in1=icn[:, g, :], op=ALU.mult),
                         reads=[srcres, "icn"], writes=["t16"])
                    P.op("dve", lambda E: E.tensor_tensor(out=pooled[:, 0:16], in0=t16[:], in1=u[:, 16:32],
                                                          op=ALU.subtract),
                         reads=["t16", "u", "pooled"], writes=["pooled"])
                    for blk in range(4):
                        pp = ps[pi % 2]
                        pres = ("psA2", pi % 2)
                        pi += 1
                        P.op("pe", lambda E, pp=pp, g=g, blk=blk: E.matmul(
                            pp[:], lhsT=wpl[:, g, :], rhs=pooled[:, 512 * blk:512 * (blk + 1)], start=True, stop=True),
                             reads=["wpl", "pooled"], writes=[pres])
                        P.op("dve", lambda E, pp=pp, g=g, blk=blk: E.tensor_scalar(
                            out=actT[:, g, 512 * blk:512 * (blk + 1)], in0=pp[:], scalar1=psc[:, g:g + 1],
                            scalar2=None, op0=ALU.mult), reads=[pres, "psc"], writes=[("mixT", g, blk)])
                P.emit()

            with ExitStack() as s3:
                T3 = lambda name, shape, dt: s3.enter_context(nc.sbuf_tensor("sb_" + name, list(shape), dt))
                btabs = [T3("btab%d" % i, [128, 3, 2, 256], BF16) for i in range(2)]
                bhalos = [T3("bhalo%d" % i, [128, 3, 2, 128], BF16) for i in range(2)]
                wqs = [T3("wq%d" % i, [128, 8, 128], BF16) for i in range(2)]
                wks = [T3("wk%d" % i, [128, 8, 128], BF16) for i in range(2)]
                wvs = [T3("wv%d" % i, [128, 8, 128], BF16) for i in range(2)]
                QT = T3("QTz", [128, 2, 2048], BF16)
                KT = T3("KT", [128, 4096], BF16)
                VT = T3("VT", [128, 4096], BF16)
                V = T3("V", [128, 69, 2, 65], BF16)
                acc = T3("acc", [65, 2, 2048], F32)
                onesf = T3("onesf", [65, 64], F32)
                NSB, NOB = 4, 3
                Pb = [T3("Pb%d" % i, [128, 2, 256], BF16) for i in range(NSB)]
                ps_tr = s3.enter_context(nc.psum_tensor("pstrV", [128, 8, 128], BF16))
                ps_s = [s3.enter_context(nc.psum_tensor("pss%d" % i, [128, 2, 256], F32)) for i in range(NSB)]
                ps_o = [s3.enter_context(nc.psum_tensor("pso%d" % i, [128, 2, 256], F32)) for i in range(NOB)]
                ps_pr = [t[:].rearrange("p h q -> p (h q)") for t in ps_o]
                btab_v = btab_in.rearrange("p (b h q) -> p b h q", b=3, h=8)
                bhalo_v = bhalo_in.rearrange("p (b h q) -> p b h q", b=3, h=8)
                P.op("dve", lambda E: E.memset(V[:], 1.0), writes=["V"])
                P.op("dve", lambda E: E.memset(QT[:], 0.0), writes=["QT"])
                P.op("dve", lambda E: E.memset(onesf[:], 1.0), writes=["onesf"])

                def tok_slice(start, dil, n=128):
                    return slice(start, start + (n - 1) * dil + 1, dil)

                ktiles = []
                for j in range(15, 32):
                    ks = tok_slice(128 * j, 1)
                    if j == 15:
                        ktiles.append((0, ks, tok_slice(0, 1), 128, "halo", 0))
                    elif j == 31:
                        ktiles.append((0, ks, tok_slice(128 * 15, 1), 128, "tab", 0))
                    else:
                        ktiles.append((0, ks, tok_slice(128 * (j - 16), 1, 256), 256, "tab", 0))
                for n in range(3, 8):
                    for r in range(4):
                        ks = tok_slice(512 * n + r, 4)
                        if n == 3:
                            ktiles.append((1, ks, tok_slice(r, 4), 128, "halo", 0))
                        elif n == 7:
                            ktiles.append((1, ks, tok_slice(512 * 3 + r, 4), 128, "tab", 0))
                        else:
                            ktiles.append((1, ks, tok_slice(512 * (n - 4) + r, 4, 256), 256, "tab", 0))
                for n in range(2):
                    for r in range(16):
                        ks = tok_slice(2048 * n + r, 16)
                        ktiles.append((2, ks, tok_slice(r, 16), 128, "halo" if n == 0 else "tab", 0))
                assert len(ktiles) == 69

                def load_hp(hp):
                    cq = 512 + 128 * hp
                    w = hp % 2
                    P.dma("pool", wqs[w][:], w_in_v[:, :, cq:cq + 128], writes=[("wq", w)], key=("wq", w))
                    P.dma("pool", wks[w][:], w_in_v[:, :, 512 + cq:512 + cq + 128], writes=[("wk", w)], key=("wk", w))
                    P.dma("pool", wvs[w][:], w_in_v[:, :, 1024 + cq:1024 + cq + 128], writes=[("wv", w)], key=("wv", w))
                    P.dma("pool", btabs[w][:], btab_v[:, :, 2 * hp:2 * hp + 2, :], writes=[("btab", w)], key=("btab", w))
                    P.dma("pool", bhalos[w][:], bhalo_v[:, :, 2 * hp:2 * hp + 2, :], writes=[("bhalo", w)],
                          key=("bhalo", w))

                conv_jobs = []
                for e in range(NCONV):
                    for r in range(8):
                        conv_jobs.append((wgu_bf[e][128 * r:128 * (r + 1), :], w_gu[e][128 * r:128 * (r + 1), :]))
                    for r in range(8):
                        conv_jobs.append((wdn_bf[e][128 * r:128 * (r + 1), :], w_down[e][128 * r:128 * (r + 1), :]))
                conv_jobs.reverse()
                conv_n = [0]

                def conv_issue():
                    if conv_jobs:
                        dst, src = conv_jobs.pop()
                        P.dma("pool", dst, src, key=("cv", conv_n[0] % 10))
                        conv_n[0] += 1

                ppi = 0
                load_hp(0)
                for hp in range(4):
                    if hp + 1 < 4:
                        load_hp(hp + 1)
                    w = hp % 2
                    wq, wk, wv, btab, bhalo = wqs[w], wks[w], wvs[w], btabs[w], bhalos[w]
                    P.op("dve", lambda E: E.memset(acc[:], 0.0), writes=["acc"])
                    for (wt, wres, dst, dres, t0, nblk, scale) in ((wq, ("wq", w), QT, "QT", 2048, 4, 0.125),
                                                                    (wk, ("wk", w), KT, "KT", 0, 8, None),
                                                                    (wv, ("wv", w), VT, "VT", 0, 8, None)):
                        for blk in range(nblk):
                            pp = ps_pr[ppi % NOB]
                            pres = ("pso", ppi % NOB)
                            ppi += 1
                            for kc in range(8):
                                P.op("pe", lambda E, pp=pp, wt=wt, kc=kc, c0=t0 + 512 * blk: E.matmul(
                                    pp[:], lhsT=wt[:, kc, :], rhs=hnT[:, kc, c0:c0 + 512], start=(kc == 0),
                                    stop=(kc == 7)), reads=[wres, "hnT"], writes=[pres])
                            if scale is not None:
                                for h2 in range(2):
                                    P.op("act", lambda E, pp=pp, dst=dst, blk=blk, scale=scale, h2=h2: E.mul(
                                        out=dst[64 * h2:64 * (h2 + 1), h2, 512 * blk:512 * (blk + 1)],
                                        in_=pp[64 * h2:64 * (h2 + 1), :], mul=scale),
                                         reads=[pres], writes=[dres])
                            else:
                                P.op("act", lambda E, pp=pp, dst=dst, blk=blk: E.copy(
                                    out=dst[:, 512 * blk:512 * (blk + 1)], in_=pp[:]), reads=[pres], writes=[dres])
                    for t0 in range(0, 69, 8):
                        nt = min(8, 69 - t0)
                        for k in range(nt):
                            P.op("pe", lambda E, k=k, sl=ktiles[t0 + k][1]: E.transpose(ps_tr[:, k, :], VT[:, sl], ident[:]),
                                 reads=["VT", "ident"], writes=["pstrV"])
                        P.op("dve", lambda E, t0=t0, nt=nt: E.tensor_copy(
                            out=V[:, t0:t0 + nt, :, 0:64],
                            in_=ps_tr[:, 0:nt, :].rearrange("p t (h d) -> p t h d", h=2)),
                             reads=["pstrV"], writes=["V"])

                    def emit_bias(ti):
                        br, ks, qs, nq, kind, c0 = ktiles[ti]
                        b = ti % 2
                        for h2 in range(2):
                            if kind == "halo":
                                bt = bhalo[:, br, h2, :]
                            else:
                                bt = btab[:, br, h2, c0:c0 + nq]
                            P.op("pe", lambda E, b=b, bt=bt, nq=nq, h2=h2: E.matmul(
                                ps_s[b][:, h2, 0:nq], lhsT=ident[:], rhs=bt, start=(h2 == 0), stop=False,
                                skip_group_check=True),
                                 reads=["ident", "btab", "bhalo"], writes=[("pss", b)])

                    def emit_QK(ti):
                        br, ks, qs, nq, kind, c0 = ktiles[ti]
                        b = ti % 2
                        for h2 in range(2):
                            r0 = 64 * h2
                            P.op("pe", lambda E, b=b, h2=h2, nq=nq, ks=ks, qs=qs, r0=r0: E.matmul(
                                ps_s[b][:, h2, 0:nq], lhsT=KT[r0:r0 + 64, ks], rhs=QT[r0:r0 + 64, qs], start=False,
                                stop=(h2 == 1), tile_position=(r0, 0), skip_group_check=True),
                                 reads=["KT", "QT"], writes=[("pss", b)])
                        P.op("act", lambda E, b=b, nq=nq: E.activation(out=Pb[b][:, :, 0:nq], in_=ps_s[b][:, :, 0:nq],
                                                                      func=AF.Exp),
                             reads=[("pss", b)], writes=[("Pb", b)])

                    def emit_PV(ti):
                        br, ks, qs, nq, kind, c0 = ktiles[ti]
                        b = ti % NSB
                        ob = ti % NOB
                        for h2 in range(2):
                            P.op("pe", lambda E, h2=h2, b=b, ob=ob, nq=nq, ti=ti: E.matmul(
                                ps_o[ob][0:65, h2, 0:nq], lhsT=V[:, ti, h2, :], rhs=Pb[b][:, h2, 0:nq], start=True,
                                stop=True), reads=["V", ("Pb", b)], writes=[("pso", ob)])
                        P.op("dve", lambda E, ob=ob, qs=qs, nq=nq: E.tensor_tensor(
                            out=acc[:, :, qs], in0=acc[:, :, qs], in1=ps_o[ob][0:65, :, 0:nq], op=ALU.add),
                             reads=[("pso", ob), "acc"], writes=["acc"])

                    def emit_S_old(ti):
                        br, ks, qs, nq, kind, c0 = ktiles[ti]
                        b = ti % NSB
                        out = ps_s[b][:, :, 0:nq]
                        P.op("pe", lambda E, out=out, ks=ks, qs=qs: E.matmul(
                            out, lhsT=KT[:, ks], rhs=QT[:, :, qs], start=True, stop=False),
                             reads=["KT", "QT"], writes=[("pss", b)])
                        if kind == "halo":
                            bt = bhalo[:, br, :, :]
                        else:
                            bt = btab[:, br, :, c0:c0 + nq]
                        P.op("pe", lambda E, out=out, bt=bt: E.matmul(
                            out, lhsT=ident[:], rhs=bt, start=False, stop=True),
                             reads=["ident", ("btab", w), ("bhalo", w)], writes=[("pss", b)])
                        P.op("act", lambda E, b=b, nq=nq: E.activation(out=Pb[b][:, :, 0:nq], in_=ps_s[b][:, :, 0:nq],
                                                                      func=AF.Exp),
                             reads=[("pss", b)], writes=[("Pb", b)])

                    if OPT_ATT_NEW:
                        emit_bias(0)
                        emit_QK(0)
                        for ti in range(len(ktiles)):
                            if ti + 1 < len(ktiles):
                                emit_bias(ti + 1)
                            emit_PV(ti)
                            if ti + 1 < len(ktiles):
                                emit_QK(ti + 1)
                    else:
                        LA = NSB - 1
                        for t in range(LA):
                            emit_S_old(t)
                        for ti in range(len(ktiles)):
                            if ti + LA < len(ktiles):
                                emit_S_old(ti + LA)
                            emit_PV(ti)
                            if ti % 3 != 2:
                                conv_issue()
                    for h2 in range(2):
                        h = 2 * hp + h2
                        if OPT_ACT_RECIP:
                            P.op("act", lambda E, h2=h2: E.activation(out=acc[64:65, h2, :], in_=acc[64:65, h2, :], func=AF.Ln),
                                 reads=["acc"], writes=["acc"])
                            P.op("act", lambda E, h2=h2: E.activation(out=acc[64:65, h2, :], in_=acc[64:65, h2, :],
                                                                      func=AF.Exp, scale=-1.0),
                                 reads=["acc"], writes=["acc"])
                        else:
                            P.op("dve", lambda E, h2=h2: E.reciprocal(out=acc[64:65, h2, :], in_=acc[64:65, h2, :]),
                                 reads=["acc"], writes=["acc"])
                        for blk in range(4):
                            pp = ps_pr[ppi % NOB]
                            pres = ("pso", ppi % NOB)
                            ppi += 1
                            P.op("pe", lambda E, pp=pp, blk=blk, h2=h2: E.matmul(
                                pp[0:64, :], lhsT=onesf[64:65, :], rhs=acc[64:65, h2, 512 * blk:512 * (blk + 1)],
                                start=True, stop=True, tile_position=(64, 0)), reads=["onesf", "acc"], writes=[pres])
                            P.op("dve", lambda E, pp=pp, blk=blk, h=h, h2=h2: E.tensor_tensor(
                                out=mixA[:, h, 512 * blk:512 * (blk + 1)], in0=acc[0:64, h2, 512 * blk:512 * (blk + 1)],
                                in1=pp[0:64, :], op=ALU.mult), reads=[pres, "acc"], writes=[("mixA", h, blk)])
                while conv_jobs:
                    conv_issue()
                P.emit(defer_cv=True)
        if debug:
            P.dma("sp", dbg["mixT"].rearrange("p (c t) -> p c t", c=8)[:, 0:4, :], actT[:], key="dbg")
            P.emit()
            P.dma("sp", dbg["mixA"].rearrange("p (c t) -> p c t", c=8), mixA[:], key="dbg")
            P.emit()

        with ExitStack() as sR:
            TR = lambda name, shape, dt: sR.enter_context(nc.sbuf_tensor("sb_" + name, list(shape), dt))
            x1 = TR("x1", [128, NT, D], F32)
            g_b = TR("g_b2", [128, D], F32)
            junk = TR("junk2", [128, D], F32)
            ssq = TR("ssq2", [128, NT], F32)
            rstd = TR("rstd2", [128, NT], F32)
            hns = [TR("hnb%d" % i, [128, D], BF16) for i in range(4)]

            with ExitStack() as sB:
                TB = lambda name, shape, dt: sB.enter_context(nc.sbuf_tensor("sb_" + name, list(shape), dt))
                wo = TB("wo", [128, 4, D], BF16)
                woA = TB("woA", [64, 8, D], BF16)
                ps = [sB.enter_context(nc.psum_tensor("psB%d" % i, [128, 512], F32)) for i in range(4)]
                P.dma("pool", wo[:], w_out_v[:, 0:4, :], writes=["wo"], key="wo")
                P.dma("pool", woA[:], w_out[512:1024, :].rearrange("(h d) n -> d h n", d=64), writes=["wo"], key="wk")
                for i in range(NT):
                    P.dma("sp", x1[:, i, :], xh[2048 + 128 * i:2048 + 128 * (i + 1), :], writes=[("x1", i)],
                          key=("x1", i % 4))
                pi = 0
                for i in range(NT):
                    for half in range(2):
                        pp = ps[pi % 4]
                        pres = ("psB", pi % 4)
                        pi += 1
                        for kc in range(4):
                            P.op("pe", lambda E, pp=pp, kc=kc, i=i, half=half: E.matmul(
                                pp[:], lhsT=actT[:, kc, 128 * i:128 * (i + 1)], rhs=wo[:, kc, 512 * half:512 * (half + 1)],
                                start=(kc == 0), stop=False), reads=["wo", "mixT"], writes=[pres])
                        for h in range(8):
                            P.op("pe", lambda E, pp=pp, h=h, i=i, half=half: E.matmul(
                                pp[:], lhsT=mixA[:, h, 128 * i:128 * (i + 1)], rhs=woA[:, h, 512 * half:512 * (half + 1)],
                                start=False, stop=(h == 7)), reads=["wo", "mixT"], writes=[pres])
                        P.op("dve", lambda E, pp=pp, i=i, half=half: E.tensor_tensor(
                            out=x1[:, i, 512 * half:512 * (half + 1)], in0=x1[:, i, 512 * half:512 * (half + 1)],
                            in1=pp[:], op=ALU.add), reads=[pres, ("x1", i)], writes=[("x1", i)])
                P.emit(defer_cv=True)
            if debug:
                P.dma("sp", dbg["x1"].rearrange("p (c t) -> p c t", c=NT), x1[:], key="dbg")
                P.emit()

            with ExitStack() as sC1:
                TC1 = lambda name, shape, dt: sC1.enter_context(nc.sbuf_tensor("sb_" + name, list(shape), dt))
                hn2T = TC1("hn2T", [128, 8, 2048], BF16)
                gates = TC1("gates", [128, NT, NE], F32)
                gates_bf = TC1("gates_bf", [128, NT, NE], BF16)
                gT = TC1("gT", [NE, NT, 128], BF16)
                bdn = TC1("bdn", [NE, D], BF16)
                wr = TC1("wr", [128, 8, NE], BF16)
                brb = TC1("brb", [128, NE], F32)
                ltri = TC1("ltri", [128, 128], BF16)
                ones128 = TC1("ones128", [128, 128], BF16)
                iota4 = TC1("iota4", [128, 4, NE], F32)
                ebase = TC1("ebase", [128, NE], F32)
                carry = TC1("carry", [128, NE], F32)
                lg = TC1("lg", [128, NE], F32)
                m8 = TC1("m8", [128, 8], F32)
                idx8 = TC1("idx8", [128, 8], mybir.dt.uint32)
                ef = TC1("ef", [128, 4], F32)
                negm = TC1("negm", [128, 1], F32)
                ex = TC1("ex", [128, NE], F32)
                msk = TC1("msk", [128, NE], F32)
                mskb = TC1("mskb", [128, NE], BF16)
                ssum = TC1("ssum", [128, 1], F32)
                posf = TC1("posf", [128, NE], F32)
                ovf = TC1("ovf", [128, NE], F32)
                oh4 = TC1("oh4", [128, 4, NE], F32)
                pr4 = TC1("pr4", [128, 4, NE], F32)
                slotf = TC1("slotf", [128, 4], F32)
                g4 = TC1("g4", [128, 4], F32)
                ps_tr = [sC1.enter_context(nc.psum_tensor("pstrC%d" % i, [128, 8, 128], BF16)) for i in range(2)]
                ps_l = sC1.enter_context(nc.psum_tensor("psl", [128, NE], F32))
                ps_pos = sC1.enter_context(nc.psum_tensor("pspos", [128, NE], F32))
                ps_cnt = sC1.enter_context(nc.psum_tensor("pscnt", [128, NE], F32))
                ps_g = sC1.enter_context(nc.psum_tensor("psgT", [NE, 128], BF16))
                ps_b = [sC1.enter_context(nc.psum_tensor("psbd%d" % i, [128, 512], F32)) for i in range(2)]
                P.dma("sp", g_b[:], g_ffn_in[:, :], writes=["gains"], key=P.ckey())
                P.dma("pool", wr[:], w_router_v, writes=["wr"], key="wr")
                P.dma("sp", brb[:], brouter_in[:, :], writes=["brb"], key=P.ckey())
                P.dma("pool", bdn[:], b_down[:, :], writes=["bdn"], key=P.ckey())
                P.dma("pool", ltri[:], ltri_in[:, :], writes=["ltri"], key=P.ckey())
                P.dma("sp", iota4[:], iota4_in.rearrange("p (k e) -> p k e", k=4), writes=["iota4"], key=P.ckey())
                P.dma("sp", ebase[:], ebase_in[:, :], writes=["ebase"], key=P.ckey())
                P.op("pool", lambda E: E.memset(ones128[:], 1.0), writes=["ones128"])
                P.op("pool", lambda E: E.memset(carry[:], 0.0), writes=["carry"])

                def route(i, hb, hres):
                    for kc in range(8):
                        P.op("pe", lambda E, kc=kc, i=i: E.matmul(
                            ps_l[:], lhsT=hn2T[:, kc, 128 * i:128 * (i + 1)], rhs=wr[:, kc, :],
                            start=(kc == 0), stop=(kc == 7)), reads=["wr", ("T", i)], writes=["psl"])
                    P.op("dve", lambda E: E.tensor_tensor(out=lg[:], in0=ps_l[:], in1=brb[:], op=ALU.add),
                         reads=["psl", "brb"], writes=["lg"])
                    P.op("dve", lambda E: E.max(out=m8[:], in_=lg[:]), reads=["lg"], writes=["m8"])
                    P.op("dve", lambda E: E.max_index(out=idx8[:], in_max=m8[:], in_values=lg[:]),
                         reads=["lg", "m8"], writes=["idx8"])
                    P.op("dve", lambda E: E.tensor_copy(out=ef[:], in_=idx8[:, 0:4]), reads=["idx8"], writes=["ef"])
                    P.op("dve", lambda E: E.tensor_scalar(out=negm[:], in0=m8[:, 0:1], scalar1=-1.0, scalar2=None,
                                                          op0=ALU.mult), reads=["m8"], writes=["negm"])
                    P.op("dve", lambda E: E.tensor_scalar(out=msk[:], in0=lg[:], scalar1=m8[:, 3:4], scalar2=None,
                                                          op0=ALU.is_ge), reads=["lg", "m8"], writes=["msk"])
                    P.op("dve", lambda E: E.tensor_copy(out=mskb[:], in_=msk[:]), reads=["msk"], writes=["mskb"])
                    P.op("act", lambda E: E.activation(out=ex[:], in_=lg[:], func=AF.Exp, bias=negm[:], scale=1.0),
                         reads=["lg", "negm"], writes=["ex"])
                    P.op("dve", lambda E: E.tensor_tensor(out=ex[:], in0=ex[:], in1=msk[:], op=ALU.mult),
                         reads=["ex", "msk"], writes=["ex"])
                    P.op("dve", lambda E: E.reduce_sum(out=ssum[:], in_=ex[:], axis=mybir.AxisListType.X),
                         reads=["ex"], writes=["ssum"])
                    P.op("dve", lambda E: E.reciprocal(out=ssum[:], in_=ssum[:]), reads=["ssum"], writes=["ssum"])
                    P.op("dve", lambda E, i=i: E.tensor_scalar(out=gates[:, i, :], in0=ex[:], scalar1=ssum[:, 0:1],
                                                               scalar2=None, op0=ALU.mult),
                         reads=["ex", "ssum"], writes=[("gates", i)])
                    P.op("dve", lambda E, i=i: E.tensor_copy(out=gates_bf[:, i, :], in_=gates[:, i, :]),
                         reads=[("gates", i)], writes=[("gates_bf", i)])
                    P.op("pe", lambda E, i=i: E.transpose(ps_g[:], gates_bf[:, i, :], ident[:]),
                         reads=[("gates_bf", i), "ident"], writes=["psgT"])
                    P.op("act", lambda E, i=i: E.copy(out=gT[:, i, :], in_=ps_g[:]), reads=["psgT"],
                         writes=[("gT", i)])
                    P.op("pe", lambda E: E.matmul(ps_pos[:], lhsT=ltri[:], rhs=mskb[:], start=True, stop=True),
                         reads=["ltri", "mskb"], writes=["pspos"])
                    P.op("pe", lambda E: E.matmul(ps_cnt[:], lhsT=ones128[:], rhs=mskb[:], start=True, stop=True),
                         reads=["ones128", "mskb"], writes=["pscnt"])
                    P.op("dve", lambda E: E.tensor_tensor(out=posf[:], in0=ps_pos[:], in1=carry[:], op=ALU.add),
                         reads=["pspos", "carry"], writes=["posf"])
                    P.op("dve", lambda E: E.tensor_tensor(out=carry[:], in0=ps_cnt[:], in1=carry[:], op=ALU.add),
                         reads=["pscnt", "carry", "posf"], writes=["carry"])
                    P.op("dve", lambda E: E.tensor_scalar(out=ovf[:], in0=posf[:], scalar1=float(CAP), scalar2=BIGSLOT,
                                                          op0=ALU.is_ge, op1=ALU.mult), reads=["posf"], writes=["ovf"])
                    P.op("dve", lambda E: E.tensor_tensor(out=posf[:], in0=posf[:], in1=ebase[:], op=ALU.add),
                         reads=["posf", "ebase", "ovf"], writes=["posf"])
                    P.op("dve", lambda E: E.tensor_tensor(out=posf[:], in0=posf[:], in1=ovf[:], op=ALU.add),
                         reads=["posf", "ovf"], writes=["posf"])
                    P.op("dve", lambda E: E.tensor_tensor(
                        out=oh4[:], in0=iota4[:], in1=ef[:].unsqueeze(2).to_broadcast([128, 4, NE]), op=ALU.is_equal),
                         reads=["iota4", "ef"], writes=["oh4"])
                    P.op("dve", lambda E: E.tensor_tensor(
                        out=pr4[:], in0=oh4[:], in1=posf[:].unsqueeze(1).to_broadcast([128, 4, NE]), op=ALU.mult),
                         reads=["oh4", "posf"], writes=["pr4"])
                    P.op("dve", lambda E: E.reduce_sum(out=slotf[:], in_=pr4[:], axis=mybir.AxisListType.X),
                         reads=["pr4"], writes=["slotf"])
                    P.op("dve", lambda E, i=i: E.tensor_copy(out=slot_i32[:, i, :], in_=slotf[:]),
                         reads=["slotf"], writes=[("slot", i)])
                    P.op("dve", lambda E, i=i: E.tensor_tensor(
                        out=pr4[:], in0=oh4[:], in1=gates[:, i, :].unsqueeze(1).to_broadcast([128, 4, NE]), op=ALU.mult),
                         reads=["oh4", ("gates", i), "pr4"], writes=["pr4"])
                    P.op("dve", lambda E: E.reduce_sum(out=g4[:], in_=pr4[:], axis=mybir.AxisListType.X),
                         reads=["pr4"], writes=["g4"])
                    P.op("dve", lambda E, i=i: E.tensor_scalar(out=g4n[:, i, :], in0=g4[:], scalar1=-1.0, scalar2=None,
                                                               op0=ALU.mult), reads=["g4"], writes=[("g4n", i)])
                    for k in range(4):
                        P.op("pool", lambda E, i=i, k=k, hb=hb: E.indirect_dma_start(
                            out=xs[:, :], out_offset=bass.IndirectOffsetOnAxis(ap=slot_i32[:, i, k:k + 1], axis=0),
                            in_=hb[:], in_offset=None, bounds_check=P.bc_reg(E, NE * CAP - 1), oob_is_err=False),
                             reads=[hres, ("slot", i)], writes=[], dma_key=("scat", i % 2, k))

                rms_tiles(lambda i: (x1[:, i, :], ("x1", i)), NT, g_b, hn2T, 0, "C1", None,
                          (junk, ssq, rstd), hns, ps_tr, post=route)
                for i in range(NT):
                    for half in range(2):
                        pp = ps_b[(2 * i + half) % 2]
                        pres = ("psbd", (2 * i + half) % 2)
                        P.op("pe", lambda E, pp=pp, i=i, half=half: E.matmul(
                            pp[:], lhsT=gT[:, i, :], rhs=bdn[:, 512 * half:512 * (half + 1)], start=True, stop=True),
                             reads=[("gT", i), "bdn"], writes=[pres])
                        P.op("dve", lambda E, pp=pp, i=i, half=half: E.tensor_tensor(
                            out=x1[:, i, 512 * half:512 * (half + 1)], in0=x1[:, i, 512 * half:512 * (half + 1)],
                            in1=pp[:], op=ALU.add), reads=[pres, ("x1", i)], writes=[("x1", i)])
                    P.dma("sp", x1_spill[:, D * i:D * (i + 1)], x1[:, i, :], reads=[("x1", i)], key=("spill", i % 4))
                P.emit()
                if debug:
                    P.dma("sp", dbg["gates"].rearrange("p (c t) -> p c t", c=NT), gates[:], key="dbg")
                    P.emit()

        sAB.close()
        with ExitStack() as sC2:
            TC2 = lambda name, shape, dt: sC2.enter_context(nc.sbuf_tensor("sb_" + name, list(shape), dt))
            NRING = 8
            ring = [TC2("ring%d" % i, [128, 8, 512], BF16) for i in range(NRING)]
            bgu = TC2("bgu", [128, NE, 16], F32)
            xes = [TC2("xe%d" % i, [128, CAP // 128, D], BF16) for i in range(2)]
            xeTs = [TC2("xeT%d" % i, [128, 8, CAP], BF16) for i in range(2)]
            act_es = [TC2("act_e%d" % i, [128, 8, CAP], BF16) for i in range(2)]
            rs = [TC2("r%d" % i, [128, CAP], F32) for i in range(2)]
            sgs = [TC2("sg%d" % i, [128, CAP], F32) for i in range(2)]
            ucs = [TC2("uc%d" % i, [128, CAP], F32) for i in range(2)]
            yts = [TC2("yt%d" % i, [128, D], F32) for i in range(3)]
            ps_gu = [sC2.enter_context(nc.psum_tensor("psgu%d" % i, [128, 512], F32)) for i in range(4)]
            ps_d = [sC2.enter_context(nc.psum_tensor("psd%d" % i, [128, 512], F32)) for i in range(2)]
            ps_tr = [sC2.enter_context(nc.psum_tensor("pstrE%d" % i, [128, 8, 128], BF16)) for i in range(2)]
            P.dma("sp", bgu[:], bgu_in.rearrange("p (e c) -> p e c", e=NE), writes=["bgu"], key=P.ckey())
            P.op("dve", lambda E: E.tensor_scalar(out=bgu[:, :, 0:8], in0=bgu[:, :, 0:8], scalar1=-1.0, scalar2=7.0,
                                                  op0=ALU.mult, op1=ALU.add), reads=["bgu"], writes=["bgu"])
            P.op("dve", lambda E: E.tensor_scalar(out=bgu[:, :, 8:16], in0=bgu[:, :, 8:16], scalar1=1.0, scalar2=None,
                                                  op0=ALU.add), reads=["bgu"], writes=["bgu"])
            pieces = []
            for e in range(NE):
                for j in range(2):
                    pieces.append((e, "g", j))
                    pieces.append((e, "u", j))
                for half in range(2):
                    pieces.append((e, "dn", half))

            def piece_dma(n):
                e, kind, j = pieces[n]
                slot = n % NRING
                wg_e = wgu_bf[e] if e < NCONV else w_gu[e]
                wd_e = wdn_bf[e] if e < NCONV else w_down[e]
                for part in range(2):
                    if kind in ("g", "u"):
                        c0 = (0 if kind == "g" else 1024) + 512 * j
                        src = wg_e.rearrange("(kc p) f -> p kc f", p=128)[:, 4 * part:4 * (part + 1), c0:c0 + 512]
                    else:
                        src = wd_e.rearrange("(kc p) n -> p kc n", p=128)[
                            :, 4 * part:4 * (part + 1), 512 * j:512 * (j + 1)]
                    dst = ring[slot][:, 4 * part:4 * (part + 1), :]
                    P.dma("pool", dst, src, writes=[("ring", slot, part)], key=("ring", slot, part))

            def xe_load(e):
                b = e % 2
                P.dma("sp", xes[b][:], xs[e * CAP:(e + 1) * CAP, :].rearrange("(j p) d -> p j d", p=128),
                      writes=[("xe", b)], key=("xe", b))

            LOOK = NRING - 2
            for n in range(min(LOOK, len(pieces))):
                piece_dma(n)
            xe_load(0)
            gi = 0
            di = 0
            ei = 0
            ti = 0
            yi = 0
            for n, (e, kind, j) in enumerate(pieces):
                if n + LOOK < len(pieces):
                    piece_dma(n + LOOK)
                slot = n % NRING
                rg = ring[slot]
                b = e % 2
                xeT, act_e = xeTs[b], act_es[b]
                if kind == "g" and j == 0:
                    if e + 1 < NE:
                        xe_load(e + 1)
                    for jj in range(CAP // 128):
                        pt, ptres = ps_tr[ti % 2], ("pstrE", ti % 2)
                        ti += 1
                        for kc in range(8):
                            P.op("pe", lambda E, pt=pt, kc=kc, jj=jj, b=b: E.transpose(
                                pt[:, kc, :], xes[b][:, jj, 128 * kc:128 * (kc + 1)], ident[:]),
                                 reads=[("xe", b), "ident"], writes=[ptres])
                        P.op("act", lambda E, pt=pt, jj=jj, xeT=xeT: E.copy(out=xeT[:, :, 128 * jj:128 * (jj + 1)], in_=pt[:]),
                             reads=[ptres], writes=[("xeT", b)])
                if kind == "g":
                    continue
                if kind == "u":
                    slot_g = (n - 1) % NRING
                    rgg = ring[slot_g]
                    for c in range(4):
                        fc = 4 * j + c
                        pg, pgres = ps_gu[gi % 4], ("psgu", gi % 4)
                        gi += 1
                        pu, pures = ps_gu[gi % 4], ("psgu", gi % 4)
                        gi += 1
                        for (pp, pres, wt, wslot) in ((pg, pgres, rgg, slot_g), (pu, pures, rg, slot)):
                            for kc in range(8):
                                P.op("pe", lambda E, pp=pp, wt=wt, kc=kc, c=c, xeT=xeT: E.matmul(
                                    pp[:, 0:CAP], lhsT=wt[:, kc, 128 * c:128 * (c + 1)],
                                    rhs=xeT[:, kc, :], start=(kc == 0), stop=(kc == 7)),
                                     reads=[("ring", wslot, kc // 4), ("xeT", b)], writes=[pres])
                        k2 = ei % 2
                        ei += 1
                        r, sg, uc = rs[k2], sgs[k2], ucs[k2]
                        P.op("act", lambda E, pg=pg, r=r, e=e, fc=fc: E.activation(
                            out=r[:], in_=pg[:, 0:CAP], func=AF.Relu, bias=bgu[:, e, fc:fc + 1], scale=-1.0),
                             reads=[pgres, "bgu"], writes=[("r", k2)])
                        P.op("act", lambda E, r=r, sg=sg: E.activation(out=sg[:], in_=r[:], func=AF.Sigmoid, bias=c119[:],
                                                                       scale=-1.702),
                             reads=[("r", k2), "c119"], writes=[("sg", k2)])
                        P.op("dve", lambda E, r=r, sg=sg: E.scalar_tensor_tensor(
                            out=r[:], in0=r[:], scalar=7.0, in1=sg[:], op0=ALU.subtract, op1=ALU.mult),
                             reads=[("r", k2), ("sg", k2)], writes=[("r", k2)])
                        P.op("dve", lambda E, pu=pu, uc=uc, e=e, fc=fc: E.tensor_scalar(
                            out=uc[:], in0=pu[:, 0:CAP], scalar1=bgu[:, e, 8 + fc:9 + fc], scalar2=8.0, op0=ALU.add,
                            op1=ALU.min), reads=[pures, "bgu"], writes=[("uc", k2)])
                        P.op("dve", lambda E, r=r, uc=uc, fc=fc, act_e=act_e: E.scalar_tensor_tensor(
                            out=act_e[:, fc, :], in0=uc[:], scalar=-6.0, in1=r[:], op0=ALU.max, op1=ALU.mult),
                             reads=[("r", k2), ("uc", k2)], writes=[("act_e", b, fc)])
                else:
                    half = j
                    for jj in range(CAP // 128):
                        pp, pres = ps_d[di % 2], ("psd", di % 2)
                        di += 1
                        for fc in range(8):
                            P.op("pe", lambda E, pp=pp, rg=rg, fc=fc, jj=jj, act_e=act_e: E.matmul(
                                pp[:], lhsT=act_e[:, fc, 128 * jj:128 * (jj + 1)], rhs=rg[:, fc, :],
                                start=(fc == 0), stop=(fc == 7)),
                                 reads=[("ring", slot, fc // 4), ("act_e", b, fc)], writes=[pres])
                        yt = yts[jj]
                        P.op("act", lambda E, pp=pp, yt=yt, half=half: E.copy(out=yt[:, 512 * half:512 * (half + 1)],
                                                                              in_=pp[:]),
                             reads=[pres], writes=[("yt", jj, half)])
                        if half == 1:
                            P.dma("sp", ys[e * CAP + 128 * jj:e * CAP + 128 * (jj + 1), :], yt[:],
                                  reads=[("yt", jj, 0), ("yt", jj, 1)], key=("ys", jj))
            P.emit()

        with ExitStack() as sR:
            TR = lambda name, shape, dt: sR.enter_context(nc.sbuf_tensor("sb_" + name, list(shape), dt))
            x1 = TR("x1b", [128, NT, D], F32)
            g_b = TR("g_b3", [128, D], F32)
            junk = TR("junk3", [128, D], F32)
            ssq = TR("ssq3", [128, NT], F32)
            rstd = TR("rstd3", [128, NT], F32)
            hns = [TR("hnc%d" % i, [128, D], BF16) for i in range(4)]
            with ExitStack() as sD:
                TD = lambda name, shape, dt: sD.enter_context(nc.sbuf_tensor("sb_" + name, list(shape), dt))
                hn3T = TD("hn3T", [128, 8, 2048], BF16)
                wpg = TD("wpg", [128, 8, D], BF16)
                wpp = TD("wpp", [128, 2, D], BF16)
                gfin = TD("gfin", [128, D], F32)
                pts = [TD("pt%d" % i, [128, 256], BF16) for i in range(NT)]
                pTs = [TD("pT%d" % i, [128, 2, 128], BF16) for i in range(2)]
                sgs = [TD("sgD%d" % i, [128, 512], F32) for i in range(2)]
                outs = [TD("outD%d" % i, [128, D], F32) for i in range(2)]
                ssq2 = TD("ssqD", [128, NT], F32)
                rstd2 = TD("rstdD", [128, NT], F32)
                ps_tr = [sD.enter_context(nc.psum_tensor("pstrD%d" % i, [128, 8, 128], BF16)) for i in range(3)]
                ps_pt = sD.enter_context(nc.psum_tensor("pspt", [128, 2, 128], BF16))
                ps_g = [sD.enter_context(nc.psum_tensor("psDg%d" % i, [128, 512], F32)) for i in range(2)]
                ps_p = [sD.enter_context(nc.psum_tensor("psDp%d" % i, [128, 512], F32)) for i in range(2)]
                P.dma("sp", g_b[:], g_ple_in[:, :], writes=["gains"], key=P.ckey())
                P.dma("sp", gfin[:], g_fin_in[:, :], writes=["gfin"], key=P.ckey())
                P.dma("pool", wpg[:], w_pg_v, writes=["wpg"], key="wpg")
                P.dma("pool", wpp[:], w_pp_v, writes=["wpp"], key="wpp")
                for i in range(NT):
                    P.dma("pool", pts[i][:], p_in[128 * i:128 * (i + 1), :], writes=[("pt", i)], key=("pt", i % 2))
                yks = [TD("yk%d" % i, [128, D], F32) for i in range(8)]
                for i in range(8):
                    P.op("pool", lambda E, i=i: E.memset(yks[i][:], 0.0), writes=[("yk", i)])
                for i in range(NT):
                    P.dma("sp", x1[:, i, :], x1_spill[:, D * i:D * (i + 1)], writes=[("x1", i)], key=("x1", i % 4))

                def combine_tile(i):
                    for k in range(4):
                        q = (4 * i + k) % 8
                        P.op("pool", lambda E, i=i, k=k, q=q: E.indirect_dma_start(
                            out=yks[q][:], out_offset=None, in_=ys[:, :],
                            in_offset=bass.IndirectOffsetOnAxis(ap=slot_i32[:, i, k:k + 1], axis=0),
                            bounds_check=P.bc_reg(E, NE * CAP - 1), oob_is_err=False),
                             reads=[], writes=[("yk", q)], dma_key=("yk", q))
                        P.op("dve", lambda E, i=i, k=k, q=q: E.scalar_tensor_tensor(
                            out=x1[:, i, :], in0=yks[q][:], scalar=g4n[:, i, k:k + 1], in1=x1[:, i, :],
                            op0=ALU.mult, op1=ALU.add), reads=[("yk", q), ("x1", i)], writes=[("x1", i)])

                def pre_combine(i):
                    if debug:
                        return
                    if i == 0:
                        combine_tile(0)
                    if i + 1 < NT:
                        combine_tile(i + 1)

                if debug:
                    for i in range(NT):
                        combine_tile(i)
                if debug:
                    P.emit()
                if debug:
                    P.dma("sp", dbg["x2"].rearrange("p (c t) -> p c t", c=NT), x1[:], key="dbg")
                    P.emit()

                pi_box = [0]

                def ple_tile(i, hb_unused, hres_unused):
                    b = i % 2
                    for c in range(2):
                        P.op("pe", lambda E, i=i, c=c: E.transpose(ps_pt[:, c, :], pts[i][:, 128 * c:128 * (c + 1)], ident[:]),
                             reads=[("pt", i), "ident"], writes=["pspt"])
                    P.op("act", lambda E, b=b: E.copy(out=pTs[b][:], in_=ps_pt[:]), reads=["pspt"], writes=[("pT", b)])
                    for half in range(2):
                        k2 = pi_box[0] % 2
                        pi_box[0] += 1
                        pg, pgres = ps_g[k2], ("psDg", k2)
                        pq, pqres = ps_p[k2], ("psDp", k2)
                        for kc in range(8):
                            P.op("pe", lambda E, pg=pg, kc=kc, i=i, half=half: E.matmul(
                                pg[:], lhsT=hn3T[:, kc, 128 * i:128 * (i + 1)], rhs=wpg[:, kc, 512 * half:512 * (half + 1)],
                                start=(kc == 0), stop=(kc == 7)), reads=["wpg", ("T", i)], writes=[pgres])
                        for c in range(2):
                            P.op("pe", lambda E, pq=pq, c=c, b=b, half=half: E.matmul(
                                pq[:], lhsT=pTs[b][:, c, :], rhs=wpp[:, c, 512 * half:512 * (half + 1)],
                                start=(c == 0), stop=(c == 1)), reads=["wpp", ("pT", b)], writes=[pqres])
                        sg = sgs[k2]
                        P.op("act", lambda E, pg=pg, sg=sg: E.activation(out=sg[:], in_=pg[:], func=AF.Sigmoid),
                             reads=[pgres], writes=[("sgD", k2)])
                        P.op("dve", lambda E, pq=pq, sg=sg: E.tensor_tensor(out=sg[:], in0=sg[:], in1=pq[:], op=ALU.mult),
                             reads=[pqres, ("sgD", k2)], writes=[("sgD", k2)])
                        P.op("dve", lambda E, sg=sg, i=i, half=half: E.tensor_tensor(
                            out=x1[:, i, 512 * half:512 * (half + 1)], in0=x1[:, i, 512 * half:512 * (half + 1)],
                            in1=sg[:], op=ALU.add), reads=[("sgD", k2), ("x1", i)], writes=[("x1", i)])
                    ob = outs[b]
                    P.op("act", lambda E, i=i: E.activation(out=junk[:], in_=x1[:, i, :], func=AF.Square,
                                                            accum_out=ssq2[:, i:i + 1]),
                         reads=[("x1", i)], writes=["Djunk2", ("ssqD", i)])
                    P.op("act", lambda E, i=i: E.activation(out=rstd2[:, i:i + 1], in_=ssq2[:, i:i + 1], func=AF.Sqrt,
                                                            bias=epsb[:], scale=1.0 / D),
                         reads=[("ssqD", i), "epsb"], writes=[("rstdD", i)])
                    P.op("dve", lambda E, i=i: E.reciprocal(out=rstd2[:, i:i + 1], in_=rstd2[:, i:i + 1]),
                         reads=[("rstdD", i)], writes=[("rstdD", i)])
                    P.op("dve", lambda E, i=i, ob=ob: E.scalar_tensor_tensor(
                        out=ob[:], in0=x1[:, i, :], scalar=rstd2[:, i:i + 1], in1=gfin[:], op0=ALU.mult, op1=ALU.mult),
                         reads=[("x1", i), ("rstdD", i), "gfin"], writes=[("outD", b)])
                    P.dma("sp", y[128 * i:128 * (i + 1), :], ob[:], reads=[("outD", b)], key=("outD", b))
                def post_skewed(i, hb, hres):
                    if i >= 1:
                        ple_tile(i - 1, None, None)

                rms_tiles(lambda i: (x1[:, i, :], ("x1", i)), NT, g_b, hn3T, 0, "D", None,
                          (junk, ssq, rstd), hns, ps_tr, pre=pre_combine, post=post_skewed)
                ple_tile(NT - 1, None, None)
                P.emit()
    return nc


def _t5_bucket_np(dist):
    n = np.maximum(dist, 1).astype(np.float32)
    large = 16 + (np.log(n / 16) / math.log(2048 / 16) * 16).astype(np.int32)
    large = np.minimum(large, 31)
    return np.where(dist < 16, dist, large)


def _bias_tables(rel_bias, first_half):
    ki = np.arange(128)[:, None]
    qi = np.arange(128)[None, :]
    btab = np.full((128, 3, 8, 256), NEG, np.float32)
    for br, (window, dil) in enumerate(BRANCHES):
        relp = 128 + qi - ki
        idxp = _t5_bucket_np(np.maximum(relp, 0) * dil)
        relc = qi - ki
        idxc = _t5_bucket_np(np.maximum(relc, 0) * dil)
        for h in range(8):
            bp = rel_bias[idxp, h]
            bc = rel_bias[idxc, h]
            btab[:, br, h, 128:256] = np.where((relp >= 0) & (relp <= 128), bp, NEG)
            btab[:, br, h, 0:128] = np.where((relc >= 0) & (relc <= 128), bc, NEG)
    bhalo = btab[:, :, :, 128:256].copy()
    if first_half:
        bhalo[:] = NEG
    return btab.reshape(128, -1), np.ascontiguousarray(bhalo).reshape(128, -1)


_NC_CACHE = {}


def _prepare_in_maps(inputs):
    f = lambda a: np.ascontiguousarray(np.asarray(a, dtype=np.float32))
    x = f(inputs["x"])
    p = f(inputs["p"])[0]
    rb = f(inputs["rel_bias"])
    bc = lambda v: np.ascontiguousarray(np.broadcast_to(f(v).reshape(1, -1), (128, f(v).size)))
    shared = {
        "ident": np.eye(128, dtype=np.float32),
        "ltri": np.triu(np.ones((128, 128), np.float32), 1),
        "iota4": np.ascontiguousarray(np.broadcast_to(np.tile(np.arange(NE, dtype=np.float32), 4)[None, :], (128, 4 * NE))),
        "ebase": np.ascontiguousarray(np.broadcast_to((np.arange(NE, dtype=np.float32) * CAP)[None, :], (128, NE))),
        "g_mix_b": bc(inputs["g_mix"][0]),
        "g_ffn_b": bc(inputs["g_ffn"][0]),
        "g_ple_b": bc(inputs["g_ple"][0]),
        "g_fin_b": bc(inputs["g_final"]),
        "w_in": f(inputs["w_in"])[0],
        "w_pool": f(inputs["w_pool"])[0],
        "pscale": np.ascontiguousarray(f(inputs["pool_scale"])[0].reshape(4, 128).T),
        "w_out": f(inputs["w_out"])[0],
        "w_router": f(inputs["w_router"])[0],
        "b_router_b": bc(inputs["b_router"][0]),
        "w_gate_up": f(inputs["w_gate_up"])[0],
        "bgu": np.ascontiguousarray(f(inputs["b_gate_up"])[0].reshape(NE, 16, 128).transpose(2, 0, 1)).reshape(128, -1),
        "w_down": f(inputs["w_down"])[0],
        "b_down": f(inputs["b_down"])[0],
        "w_ple_gate": f(inputs["w_ple_gate"])[0],
        "w_ple_proj": f(inputs["w_ple_proj"])[0],
    }
    tabs = {fh: _bias_tables(rb, fh) for fh in (True, False)}
    in_maps = []
    for c in range(NCORES):
        b, half = c // 2, c % 2
        base = half * S_OWN
        xh = np.zeros((4096, D), np.float32)
        if half == 1:
            xh[0:2048] = x[b, 0:2048]
        xh[2048:4096] = x[b, base:base + S_OWN]
        pos = base + np.arange(16)
        invcnt = np.stack([1.0 / np.minimum(pos + 1, w) for w in (2, 4, 8, 16)]).astype(np.float32)
        m = dict(shared)
        m["xh"] = xh
        m["p"] = np.ascontiguousarray(p[b, base:base + S_OWN])
        m["btab"], m["bhalo"] = tabs[half == 0]
        m["invcnt"] = np.ascontiguousarray(np.broadcast_to(invcnt.reshape(1, -1), (128, 64)))
        in_maps.append(m)
    return in_maps


def kernel(**inputs):
    in_maps = _prepare_in_maps(inputs)
    if "nc" not in _NC_CACHE:
        _NC_CACHE["nc"] = build_program(debug=False)
    nc = _NC_CACHE["nc"]
    res = run_bass_kernel_spmd(nc, in_maps, core_ids=list(range(NCORES)))
    out = np.zeros((4, 4096, D), np.float32)
    for c in range(NCORES):
        b, half = c // 2, c % 2
        out[b, half * S_OWN:(half + 1) * S_OWN] = np.asarray(res.results[c]["y"], dtype=np.float32)
    return out
```

```python
import math
from contextlib import ExitStack

import numpy as np
import concourse.bass as bass
import concourse.mybir as mybir
from concourse.bass_utils import run_bass_kernel_spmd

F32 = mybir.dt.float32
BF16 = mybir.dt.bfloat16
AF = mybir.ActivationFunctionType
ALU = mybir.AluOpType

NCORES = 8
D = 1024
S_OWN = 2048
NT = 16
NE = 32
NEG = -1e30
CAP = 384
BIGSLOT = 1.0e6
NCONV = 10
OPT_ACT_RECIP = True
OPT_ATT_NEW = False
EPS = 1e-6
BRANCHES = ((128, 1), (512, 4), (2048, 16))


class Op:
    __slots__ = ("eng", "fn", "deps", "signal", "count", "dma_sem", "is_dma")

    def __init__(self, eng, fn, dma_sem=None):
        self.eng = eng
        self.fn = fn
        self.deps = []
        self.signal = False
        self.count = 0
        self.dma_sem = dma_sem
        self.is_dma = dma_sem is not None


class Prog:
    ENGS = ("pe", "act", "dve", "pool", "sp")

    def __init__(self, nc, stack):
        self.nc = nc
        self.stack = stack
        self.esem = {e: stack.enter_context(nc.semaphore("sem_" + e)) for e in self.ENGS}
        self.ecount = {e: 0 for e in self.ENGS}
        self.dsem = {}
        self.dcount = {}
        self.waited = {e: {} for e in self.ENGS}
        self.begin()

    def bc_reg(self, E, val):
        if self._bc is None:
            self._bc = E.to_reg(val)
        return self._bc

    def begin(self):
        self._bc = None
        self.ops = []
        self.res_w = {}
        self.res_r = {}

    def _dma_sem(self, key):
        if key not in self.dsem:
            self.dsem[key] = self.stack.enter_context(self.nc.semaphore("dsem_%d" % len(self.dsem)))
            self.dcount[key] = 0
        return key

    def op(self, eng, fn, reads=(), writes=(), dma_key=None):
        o = Op(eng, fn, self._dma_sem(dma_key) if dma_key is not None else None)
        if dma_key is not None:
            writes = list(writes) + [("__key", dma_key)]
        deps = {}
        for r in reads:
            w = self.res_w.get(r)
            if w is not None:
                deps[id(w)] = w
        for r in writes:
            w = self.res_w.get(r)
            if w is not None:
                deps[id(w)] = w
            for rd in self.res_r.get(r, ()):
                deps[id(rd)] = rd
        for d in deps.values():
            if d is o:
                continue
            if d.eng == "pe" and eng == "pe" and not d.is_dma and not o.is_dma:
                continue
            o.deps.append(d)
            d.signal = True
        for r in reads:
            lst = self.res_r.setdefault(r, [])
            if not o.is_dma:
                lst[:] = [x for x in lst if x.is_dma or x.eng != eng]
            lst.append(o)
        for r in writes:
            self.res_w[r] = o
            self.res_r[r] = []
        self.ops.append(o)
        return o

    def dma(self, eng, out, in_, reads=(), writes=(), key=None):
        assert key is not None
        return self.op(eng, lambda E: E.dma_start(out=out, in_=in_), reads, writes, dma_key=key)

    def ckey(self):
        self._ck = (getattr(self, "_ck", 0) + 1) % 6
        return ("const", self._ck)

    def emit(self, defer_cv=False):
        nc = self.nc
        last = {}
        for o in self.ops:
            if not o.is_dma:
                last[o.eng] = o
        for o in last.values():
            o.signal = True
        for o in self.ops:
            if o.is_dma:
                o.signal = True
                self.dcount[o.dma_sem] += 16
                o.count = self.dcount[o.dma_sem]
            elif o.signal:
                self.ecount[o.eng] += 1
                o.count = self.ecount[o.eng]
        by_eng = {e: [o for o in self.ops if o.eng == e] for e in self.ENGS}
        final_e = dict(self.ecount)
        final_d = dict(self.dcount)

        def run(ename, E):
            waited = self.waited[ename]
            for o in by_eng[ename]:
                need = {}
                for d in o.deps:
                    if d.is_dma:
                        k = ("d", d.dma_sem)
                    else:
                        k = ("e", d.eng)
                    if d.count > need.get(k, 0):
                        need[k] = d.count
                for k, v in need.items():
                    if waited.get(k, 0) < v:
                        sem = self.dsem[k[1]] if k[0] == "d" else self.esem[k[1]]
                        E.wait_ge(sem, v)
                        waited[k] = v
                ins = o.fn(E)
                if o.signal:
                    if o.is_dma:
                        ins.then_inc(self.dsem[o.dma_sem], 16)
                    else:
                        ins.then_inc(self.esem[o.eng], 1)
            for e2, v in final_e.items():
                if v > 0 and waited.get(("e", e2), 0) < v:
                    E.wait_ge(self.esem[e2], v)
                    waited[("e", e2)] = v
            for k2, v in final_d.items():
                if defer_cv and isinstance(k2, tuple) and k2[0] == "cv":
                    continue
                if v > 0 and waited.get(("d", k2), 0) < v:
                    E.wait_ge(self.dsem[k2], v)
                    waited[("d", k2)] = v

        with nc.Block() as block:
            @block.tensor
            def _(E):
                run("pe", E)

            @block.scalar
            def _(E):
                run("act", E)

            @block.vector
            def _(E):
                run("dve", E)

            @block.gpsimd
            def _(E):
                run("pool", E)

            @block.sync
            def _(E):
                run("sp", E)
        self.begin()


def build_program(debug=False):
    nc = bass.Bass("TRN2", target_bir_lowering=False)

    def din(name, shape, dt=F32):
        return nc.dram_tensor(name, list(shape), dt, kind="ExternalInput").ap()

    xh = din("xh", [4096, D])
    p_in = din("p", [S_OWN, 256])
    ident_in = din("ident", [128, 128])
    btab_in = din("btab", [128, 3 * 8 * 256])
    bhalo_in = din("bhalo", [128, 3 * 8 * 128])
    invcnt_in = din("invcnt", [128, 4 * 16])
    g_mix_in = din("g_mix_b", [128, D])
    g_ffn_in = din("g_ffn_b", [128, D])
    g_ple_in = din("g_ple_b", [128, D])
    g_fin_in = din("g_fin_b", [128, D])
    w_in = din("w_in", [D, 2048])
    w_pool = din("w_pool", [4, 128, 128])
    pscale_in = din("pscale", [128, 4])
    w_out = din("w_out", [D, D])
    w_router = din("w_router", [D, NE])
    brouter_in = din("b_router_b", [128, NE])
    w_gu = din("w_gate_up", [NE, D, 2 * D])
    bgu_in = din("bgu", [128, NE * 16])
    w_down = din("w_down", [NE, D, D])
    b_down = din("b_down", [NE, D])
    w_pg = din("w_ple_gate", [D, D])
    w_pp = din("w_ple_proj", [256, D])
    ltri_in = din("ltri", [128, 128])
    iota4_in = din("iota4", [128, 4 * NE])
    ebase_in = din("ebase", [128, NE])
    y = nc.dram_tensor("y", [S_OWN, D], F32, kind="ExternalOutput").ap()
    xs = nc.dram_tensor("xs_scratch", [NE * CAP, D], BF16).ap()
    ys = nc.dram_tensor("ys_scratch", [NE * CAP, D], F32).ap()
    x1_spill = nc.dram_tensor("x1_spill", [128, NT * D], F32).ap()
    wgu_bf = nc.dram_tensor("wgu_bf16", [NCONV, D, 2 * D], BF16).ap()
    wdn_bf = nc.dram_tensor("wdn_bf16", [NCONV, D, D], BF16).ap()
    dbg = {}
    if debug:
        dbg["mixT"] = nc.dram_tensor("dbg_mixT", [128, 8 * 2048], BF16, kind="ExternalOutput").ap()
        dbg["mixA"] = nc.dram_tensor("dbg_mixA", [64, 8 * 2048], BF16, kind="ExternalOutput").ap()
        dbg["x1"] = nc.dram_tensor("dbg_x1", [128, NT * D], F32, kind="ExternalOutput").ap()
        dbg["gates"] = nc.dram_tensor("dbg_gates", [128, NT * NE], F32, kind="ExternalOutput").ap()
        dbg["x2"] = nc.dram_tensor("dbg_x2", [128, NT * D], F32, kind="ExternalOutput").ap()

    w_in_v = w_in.rearrange("(kc p) n -> p kc n", p=128)
    w_out_v = w_out.rearrange("(kc p) n -> p kc n", p=128)
    w_pg_v = w_pg.rearrange("(kc p) n -> p kc n", p=128)
    w_pp_v = w_pp.rearrange("(kc p) n -> p kc n", p=128)
    w_router_v = w_router.rearrange("(kc p) n -> p kc n", p=128)

    with ExitStack() as stack:
        P = Prog(nc, stack)
        T = lambda name, shape, dt: stack.enter_context(nc.sbuf_tensor("sb_" + name, list(shape), dt))
        ident = T("ident_sb", [128, 128], BF16)
        ones_bf = T("ones_bf", [128, 64], BF16)
        sAB = ExitStack()
        TAB = lambda name, shape, dt: sAB.enter_context(nc.sbuf_tensor("sb_" + name, list(shape), dt))
        epsb = T("epsb", [128, 1], F32)
        c119 = T("c119", [128, 1], F32)
        slot_i32 = T("slot_i32", [128, NT, 4], mybir.dt.int32)
        g4n = T("g4n", [128, NT, 4], F32)
        actT = TAB("actT", [128, 4, 2048], BF16)
        mixA = TAB("mixA", [64, 8, 2048], BF16)

        def rms_tiles(x_ap_of, ntiles, g_b, dstT, dst_col0, pfx, xt_res, stats, hn_bufs, ps_tr,
                      pre=None, post=None):
            junk, ssq, rstd = stats

            def stage1(i):
                if pre is not None:
                    pre(i)
                xa, xres = x_ap_of(i)
                P.op("act", lambda E, xa=xa, i=i: E.activation(out=junk[:], in_=xa, func=AF.Square,
                                                                accum_out=ssq[:, i:i + 1]),
                     reads=[xres], writes=[pfx + "junk", (pfx + "ssq", i)])
                P.op("act", lambda E, i=i: E.activation(out=rstd[:, i:i + 1], in_=ssq[:, i:i + 1], func=AF.Sqrt,
                                                        bias=epsb[:], scale=1.0 / D),
                     reads=[(pfx + "ssq", i), "epsb"], writes=[(pfx + "rstd", i)])
                P.op("dve", lambda E, i=i: E.reciprocal(out=rstd[:, i:i + 1], in_=rstd[:, i:i + 1]),
                     reads=[(pfx + "rstd", i)], writes=[(pfx + "rstd", i)])
                hb = hn_bufs[i % len(hn_bufs)]
                hres = (pfx + "hn", i % len(hn_bufs))
                P.op("dve", lambda E, xa=xa, i=i, hb=hb: E.scalar_tensor_tensor(
                    out=hb[:], in0=xa, scalar=rstd[:, i:i + 1], in1=g_b[:], op0=ALU.mult, op1=ALU.mult),
                     reads=[xres, (pfx + "rstd", i), "gains"], writes=[hres])

            def stage2(i):
                hb = hn_bufs[i % len(hn_bufs)]
                hres = (pfx + "hn", i % len(hn_bufs))
                pt = ps_tr[i % len(ps_tr)]
                pres = (pfx + "pstr", i % len(ps_tr))
                for kc in range(8):
                    P.op("pe", lambda E, kc=kc, hb=hb, pt=pt: E.transpose(pt[:, kc, :], hb[:, kc * 128:(kc + 1) * 128],
                                                                          ident[:]),
                         reads=[hres, "ident"], writes=[pres])
                c0 = dst_col0 + 128 * i
                P.op("act", lambda E, pt=pt, c0=c0: E.copy(out=dstT[:, :, c0:c0 + 128], in_=pt[:]),
                     reads=[pres], writes=[("T", c0 // 128)])
                if post is not None:
                    post(i, hb, hres)

            stage1(0)
            for i in range(ntiles):
                if i + 1 < ntiles:
                    stage1(i + 1)
                stage2(i)

        P.dma("pool", ident[:], ident_in[:, :], writes=["ident"], key=P.ckey())
        P.op("pool", lambda E: E.memset(ones_bf[:], 1.0), writes=["ones"])
        P.op("pool", lambda E: E.memset(epsb[:], EPS), writes=["epsb"])
        P.op("pool", lambda E: E.memset(c119[:], 7.0 * 1.702), writes=["c119"])
        zero_jobs = list(range(NE * CAP // 1024))

        with ExitStack() as sA:
            TA = lambda name, shape, dt: sA.enter_context(nc.sbuf_tensor("sb_" + name, list(shape), dt))
            hnT = TA("hnT", [128, 8, 4096], BF16)

            with ExitStack() as s1:
                T1 = lambda name, shape, dt: s1.enter_context(nc.sbuf_tensor("sb_" + name, list(shape), dt))
                g_b = T1("g_b", [128, D], F32)
                NXT = 4
                xts = [T1("xt%d" % i, [128, D], F32) for i in range(NXT)]
                hns = [T1("hn%d" % i, [128, D], BF16) for i in range(4)]
                junk = T1("junk", [128, D], F32)
                ssq = T1("ssq", [128, 32], F32)
                rstd = T1("rstd", [128, 32], F32)
                ps_tr = [s1.enter_context(nc.psum_tensor("pstrA%d" % i, [128, 8, 128], BF16)) for i in range(4)]
                P.dma("sp", g_b[:], g_mix_in[:, :], writes=["gains"], key=P.ckey())
                def ld(i):
                    P.dma("sp", xts[i % NXT][:], xh[128 * i:128 * (i + 1), :], writes=[("xt", i % NXT)],
                          key=("xt", i % NXT))

                zt = T1("zeros", [128, 8192], BF16)
                P.op("pool", lambda E: E.memset(zt[:], 0.0), writes=["zt"])

                def pre(i):
                    if i == 0:
                        ld(0)
                        ld(1)
                        ld(2)
                    if i + 3 < 32:
                        ld(i + 3)
                    if i >= 2 and zero_jobs:
                        c = zero_jobs.pop()
                        P.dma("sp", xs[1024 * c:1024 * (c + 1), :].rearrange("(p r) d -> p (r d)", p=128), zt[:],
                              reads=["zt"], key=("zx", c % 4))
                rms_tiles(lambda i: (xts[i % NXT][:], ("xt", i % NXT)), 32, g_b, hnT, 0, "A1", None,
                          (junk, ssq, rstd), hns, ps_tr, pre=pre)
                P.emit()

            with ExitStack() as s2:
                T2 = lambda name, shape, dt: s2.enter_context(nc.sbuf_tensor("sb_" + name, list(shape), dt))
                wpc = T2("wpc", [128, 8, 512], BF16)
                wpl = T2("wpl", [128, 4, 128], BF16)
                psc = T2("psc", [128, 4], F32)
                icn = T2("icn", [128, 4, 16], F32)
                u = T2("u", [128, 2064], F32)
                sa = T2("sa", [128, 2064], F32)
                sb = T2("sb", [128, 2064], F32)
                pooled = T2("pooled", [128, 2048], BF16)
                t16 = T2("t16", [128, 16], F32)
                ps = [s2.enter_context(nc.psum_tensor("psA2_%d" % i, [128, 512], F32)) for i in range(2)]
                P.dma("pool", wpc[:], w_in_v[:, :, 0:512], writes=["wpc"], key="wpc")
                P.dma("pool", wpl[:], w_pool.rearrange("g c d -> c g d"), writes=["wpl"], key="wpl")
                P.dma("sp", psc[:], pscale_in[:, :], writes=["psc"], key=P.ckey())
                P.dma("sp", icn[:], invcnt_in.rearrange("p (g t) -> p g t", g=4), writes=["icn"], key=P.ckey())
                pi = 0
                for g in range(4):
                    w = (2, 4, 8, 16)[g]
                    for blk in range(5):
                        pp = ps[pi % 2]
                        pres = ("psA2", pi % 2)
                        pi += 1
                        if blk == 0:
                            c0, n, o0 = 2032, 16, 0
                        else:
                            c0, n, o0 = 2048 + 512 * (blk - 1), 512, 16 + 512 * (blk - 1)
                        for kc in range(8):
                            P.op("pe", lambda E, pp=pp, kc=kc, g=g, c0=c0, n=n: E.matmul(
                                pp[:, 0:n], lhsT=wpc[:, kc, g * 128:(g + 1) * 128], rhs=hnT[:, kc, c0:c0 + n],
                                start=(kc == 0), stop=(kc == 7)), reads=["wpc", "hnT"], writes=[pres])
                        P.op("act", lambda E, pp=pp, n=n, o0=o0: E.copy(out=u[:, o0:o0 + n], in_=pp[:, 0:n]),
                             reads=[pres], writes=["u"])
                    src, srcres = u, "u"
                    sh = 1
                    bufs = [(sa, "sa"), (sb, "sb")]
                    bi = 0
                    while sh < w:
                        dst, dres = bufs[bi % 2]
                        bi += 1
                        P.op("dve", lambda E, dst=dst, src=src, sh=sh: E.tensor_tensor(
                            out=dst[:, sh:2064], in0=src[:, sh:2064], in1=src[:, 0:2064 - sh], op=ALU.add),
                             reads=[srcres], writes=[dres])
                        src, srcres = dst, dres
                        sh *= 2
                    P.op("dve", lambda E, src=src, w=w: E.scalar_tensor_tensor(
                        out=pooled[:], in0=src[:, 16:2064], scalar=1.0 / w, in1=u[:, 16:2064],
                        op0=ALU.mult, op1=ALU.subtract), reads=[srcres, "u"], writes=["pooled"])
                    P.op("dve", lambda E, src=src, g=g: E.tensor_tensor(
                        out=t16[:], in0=src[:, 16:32], in1=icn[:, g, :], op=ALU.mult),
                         reads=[srcres, "icn"], writes=["t16"])
                    P.op("dve", lambda E: E.tensor_tensor(out=pooled[:, 0:16], in0=t16[:], in1=u[:, 16:32],
                                                          op=ALU.subtract),
                         reads=["t16", "u", "pooled"], writes=["pooled"])
                    for blk in range(4):
                        pp = ps[pi % 2]
                        pres = ("psA2", pi % 2)
                        pi += 1
                        P.op("pe", lambda E, pp=pp, g=g, blk=blk: E.matmul(
                            pp[:], lhsT=wpl[:, g, :], rhs=pooled[:, 512 * blk:512 * (blk + 1)], start=True, stop=True),
                             reads=["wpl", "pooled"], writes=[pres])
                        P.op("dve", lambda E, pp=pp, g=g, blk=blk: E.tensor_scalar(
                            out=actT[:, g, 512 * blk:512 * (blk + 1)], in0=pp[:], scalar1=psc[:, g:g + 1],
                            scalar2=None, op0=ALU.mult), reads=[pres, "psc"], writes=[("mixT", g, blk)])
                P.emit()

            with ExitStack() as s3:
                T3 = lambda name, shape, dt: s3.enter_context(nc.sbuf_tensor("sb_" + name, list(shape), dt))
                btabs = [T3("btab%d" % i, [128, 3, 2, 256], BF16) for i in range(2)]
                bhalos = [T3("bhalo%d" % i, [128, 3, 2, 128], BF16) for i in range(2)]
                wqs = [T3("wq%d" % i, [128, 8, 128], BF16) for i in range(2)]
                wks = [T3("wk%d" % i, [128, 8, 128], BF16) for i in range(2)]
                wvs = [T3("wv%d" % i, [128, 8, 128], BF16) for i in range(2)]
                QT = T3("QTz", [128, 2, 2048], BF16)
                KT = T3("KT", [128, 4096], BF16)
                VT = T3("VT", [128, 4096], BF16)
                V = T3("V", [128, 69, 2, 65], BF16)
                acc = T3("acc", [65, 2, 2048], F32)
                onesf = T3("onesf", [65, 64], F32)
                NSB, NOB = 4, 3
                Pb = [T3("Pb%d" % i, [128, 2, 256], BF16) for i in range(NSB)]
                ps_tr = s3.enter_context(nc.psum_tensor("pstrV", [128, 8, 128], BF16))
                ps_s = [s3.enter_context(nc.psum_tensor("pss%d" % i, [128, 2, 256], F32)) for i in range(NSB)]
                ps_o = [s3.enter_context(nc.psum_tensor("pso%d" % i, [128, 2, 256], F32)) for i in range(NOB)]
                ps_pr = [t[:].rearrange("p h q -> p (h q)") for t in ps_o]
                btab_v = btab_in.rearrange("p (b h q) -> p b h q", b=3, h=8)
                bhalo_v = bhalo_in.rearrange("p (b h q) -> p b h q", b=3, h=8)
                P.op("dve", lambda E: E.memset(V[:], 1.0), writes=["V"])
                P.op("dve", lambda E: E.memset(QT[:], 0.0), writes=["QT"])
                P.op("dve", lambda E: E.memset(onesf[:], 1.0), writes=["onesf"])

                def tok_slice(start, dil, n=128):
                    return slice(start, start + (n - 1) * dil + 1, dil)

                ktiles = []
                for j in range(15, 32):
                    ks = tok_slice(128 * j, 1)
                    if j == 15:
                        ktiles.append((0, ks, tok_slice(0, 1), 128, "halo", 0))
                    elif j == 31:
                        ktiles.append((0, ks, tok_slice(128 * 15, 1), 128, "tab", 0))
                    else:
                        ktiles.append((0, ks, tok_slice(128 * (j - 16), 1, 256), 256, "tab", 0))
                for n in range(3, 8):
                    for r in range(4):
                        ks = tok_slice(512 * n + r, 4)
                        if n == 3:
                            ktiles.append((1, ks, tok_slice(r, 4), 128, "halo", 0))
                        elif n == 7:
                            ktiles.append((1, ks, tok_slice(512 * 3 + r, 4), 128, "tab", 0))
                        else:
                            ktiles.append((1, ks, tok_slice(512 * (n - 4) + r, 4, 256), 256, "tab", 0))
                for n in range(2):
                    for r in range(16):
                        ks = tok_slice(2048 * n + r, 16)
                        ktiles.append((2, ks, tok_slice(r, 16), 128, "halo" if n == 0 else "tab", 0))
                assert len(ktiles) == 69

                def load_hp(hp):
                    cq = 512 + 128 * hp
                    w = hp % 2
                    P.dma("pool", wqs[w][:], w_in_v[:, :, cq:cq + 128], writes=[("wq", w)], key=("wq", w))
                    P.dma("pool", wks[w][:], w_in_v[:, :, 512 + cq:512 + cq + 128], writes=[("wk", w)], key=("wk", w))
                    P.dma("pool", wvs[w][:], w_in_v[:, :, 1024 + cq:1024 + cq + 128], writes=[("wv", w)], key=("wv", w))
                    P.dma("pool", btabs[w][:], btab_v[:, :, 2 * hp:2 * hp + 2, :], writes=[("btab", w)], key=("btab", w))
                    P.dma("pool", bhalos[w][:], bhalo_v[:, :, 2 * hp:2 * hp + 2, :], writes=[("bhalo", w)],
                          key=("bhalo", w))

                conv_jobs = []
                for e in range(NCONV):
                    for r in range(8):
                        conv_jobs.append((wgu_bf[e][128 * r:128 * (r + 1), :], w_gu[e][128 * r:128 * (r + 1), :]))
                    for r in range(8):
                        conv_jobs.append((wdn_bf[e][128 * r:128 * (r + 1), :], w_down[e][128 * r:128 * (r + 1), :]))
                conv_jobs.reverse()
                conv_n = [0]

                def conv_issue():
                    if conv_jobs:
                        dst, src = conv_jobs.pop()
                        P.dma("pool", dst, src, key=("cv", conv_n[0] % 10))
                        conv_n[0] += 1

                ppi = 0
                load_hp(0)
                for hp in range(4):
                    if hp + 1 < 4:
                        load_hp(hp + 1)
                    w = hp % 2
                    wq, wk, wv, btab, bhalo = wqs[w], wks[w], wvs[w], btabs[w], bhalos[w]
                    P.op("dve", lambda E: E.memset(acc[:], 0.0), writes=["acc"])
                    for (wt, wres, dst, dres, t0, nblk, scale) in ((wq, ("wq", w), QT, "QT", 2048, 4, 0.125),
                                                                    (wk, ("wk", w), KT, "KT", 0, 8, None),
                                                                    (wv, ("wv", w), VT, "VT", 0, 8, None)):
                        for blk in range(nblk):
                            pp = ps_pr[ppi % NOB]
                            pres = ("pso", ppi % NOB)
                            ppi += 1
                            for kc in range(8):
                                P.op("pe", lambda E, pp=pp, wt=wt, kc=kc, c0=t0 + 512 * blk: E.matmul(
                                    pp[:], lhsT=wt[:, kc, :], rhs=hnT[:, kc, c0:c0 + 512], start=(kc == 0),
                                    stop=(kc == 7)), reads=[wres, "hnT"], writes=[pres])
                            if scale is not None:
                                for h2 in range(2):
                                    P.op("act", lambda E, pp=pp, dst=dst, blk=blk, scale=scale, h2=h2: E.mul(
                                        out=dst[64 * h2:64 * (h2 + 1), h2, 512 * blk:512 * (blk + 1)],
                                        in_=pp[64 * h2:64 * (h2 + 1), :], mul=scale),
                                         reads=[pres], writes=[dres])
                            else:
                                P.op("act", lambda E, pp=pp, dst=dst, blk=blk: E.copy(
                                    out=dst[:, 512 * blk:512 * (blk + 1)], in_=pp[:]), reads=[pres], writes=[dres])
                    for t0 in range(0, 69, 8):
                        nt = min(8, 69 - t0)
                        for k in range(nt):
                            P.op("pe", lambda E, k=k, sl=ktiles[t0 + k][1]: E.transpose(ps_tr[:, k, :], VT[:, sl], ident[:]),
                                 reads=["VT", "ident"], writes=["pstrV"])
                        P.op("dve", lambda E, t0=t0, nt=nt: E.tensor_copy(
                            out=V[:, t0:t0 + nt, :, 0:64],
                            in_=ps_tr[:, 0:nt, :].rearrange("p t (h d) -> p t h d", h=2)),
                             reads=["pstrV"], writes=["V"])

                    def emit_bias(ti):
                        br, ks, qs, nq, kind, c0 = ktiles[ti]
                        b = ti % 2
                        for h2 in range(2):
                            if kind == "halo":
                                bt = bhalo[:, br, h2, :]
                            else:
                                bt = btab[:, br, h2, c0:c0 + nq]
                            P.op("pe", lambda E, b=b, bt=bt, nq=nq, h2=h2: E.matmul(
                                ps_s[b][:, h2, 0:nq], lhsT=ident[:], rhs=bt, start=(h2 == 0), stop=False,
                                skip_group_check=True),
                                 reads=["ident", "btab", "bhalo"], writes=[("pss", b)])

                    def emit_QK(ti):
                        br, ks, qs, nq, kind, c0 = ktiles[ti]
                        b = ti % 2
                        for h2 in range(2):
                            r0 = 64 * h2
                            P.op("pe", lambda E, b=b, h2=h2, nq=nq, ks=ks, qs=qs, r0=r0: E.matmul(
                                ps_s[b][:, h2, 0:nq], lhsT=KT[r0:r0 + 64, ks], rhs=QT[r0:r0 + 64, qs], start=False,
                                stop=(h2 == 1), tile_position=(r0, 0), skip_group_check=True),
                                 reads=["KT", "QT"], writes=[("pss", b)])
                        P.op("act", lambda E, b=b, nq=nq: E.activation(out=Pb[b][:, :, 0:nq], in_=ps_s[b][:, :, 0:nq],
                                                                      func=AF.Exp),
                             reads=[("pss", b)], writes=[("Pb", b)])

                    def emit_PV(ti):
                        br, ks, qs, nq, kind, c0 = ktiles[ti]
                        b = ti % NSB
                        ob = ti % NOB
                        for h2 in range(2):
                            P.op("pe", lambda E, h2=h2, b=b, ob=ob, nq=nq, ti=ti: E.matmul(
                                ps_o[ob][0:65, h2, 0:nq], lhsT=V[:, ti, h2, :], rhs=Pb[b][:, h2, 0:nq], start=True,
                                stop=True), reads=["V", ("Pb", b)], writes=[("pso", ob)])
                        P.op("dve", lambda E, ob=ob, qs=qs, nq=nq: E.tensor_tensor(
                            out=acc[:, :, qs], in0=acc[:, :, qs], in1=ps_o[ob][0:65, :, 0:nq], op=ALU.add),
                             reads=[("pso", ob), "acc"], writes=["acc"])

                    def emit_S_old(ti):
                        br, ks, qs, nq, kind, c0 = ktiles[ti]
                        b = ti % NSB
                        out = ps_s[b][:, :, 0:nq]
                        P.op("pe", lambda E, out=out, ks=ks, qs=qs: E.matmul(
                            out, lhsT=KT[:, ks], rhs=QT[:, :, qs], start=True, stop=False),
                             reads=["KT", "QT"], writes=[("pss", b)])
                        if kind == "halo":
                            bt = bhalo[:, br, :, :]
                        else:
                            bt = btab[:, br, :, c0:c0 + nq]
                        P.op("pe", lambda E, out=out, bt=bt: E.matmul(
                            out, lhsT=ident[:], rhs=bt, start=False, stop=True),
                             reads=["ident", ("btab", w), ("bhalo", w)], writes=[("pss", b)])
                        P.op("act", lambda E, b=b, nq=nq: E.activation(out=Pb[b][:, :, 0:nq], in_=ps_s[b][:, :, 0:nq],
                                                                      func=AF.Exp),
                             reads=[("pss", b)], writes=[("Pb", b)])

                    if OPT_ATT_NEW:
                        emit_bias(0)
                        emit_QK(0)
                        for ti in range(len(ktiles)):
                            if ti + 1 < len(ktiles):
                                emit_bias(ti + 1)
                            emit_PV(ti)
                            if ti + 1 < len(ktiles):
                                emit_QK(ti + 1)
                    else:
                        LA = NSB - 1
                        for t in range(LA):
                            emit_S_old(t)
                        for ti in range(len(ktiles)):
                            if ti + LA < len(ktiles):
                                emit_S_old(ti + LA)
                            emit_PV(ti)
                            if ti % 3 != 2:
                                conv_issue()
                    for h2 in range(2):
                        h = 2 * hp + h2
                        if OPT_ACT_RECIP:
                            P.op("act", lambda E, h2=h2: E.activation(out=acc[64:65, h2, :], in_=acc[64:65, h2, :], func=AF.Ln),
                                 reads=["acc"], writes=["acc"])
                            P.op("act", lambda E, h2=h2: E.activation(out=acc[64:65, h2, :], in_=acc[64:65, h2, :],
                                                                      func=AF.Exp, scale=-1.0),
                                 reads=["acc"], writes=["acc"])
                        else:
                            P.op("dve", lambda E, h2=h2: E.reciprocal(out=acc[64:65, h2, :], in_=acc[64:65, h2, :]),
                                 reads=["acc"], writes=["acc"])
                        for blk in range(4):
                            pp = ps_pr[ppi % NOB]
                            pres = ("pso", ppi % NOB)
                            ppi += 1
                            P.op("pe", lambda E, pp=pp, blk=blk, h2=h2: E.matmul(
                                pp[0:64, :], lhsT=onesf[64:65, :], rhs=acc[64:65, h2, 512 * blk:512 * (blk + 1)],
                                start=True, stop=True, tile_position=(64, 0)), reads=["onesf", "acc"], writes=[pres])
                            P.op("dve", lambda E, pp=pp, blk=blk, h=h, h2=h2: E.tensor_tensor(
                                out=mixA[:, h, 512 * blk:512 * (blk + 1)], in0=acc[0:64, h2, 512 * blk:512 * (blk + 1)],
                                in1=pp[0:64, :], op=ALU.mult), reads=[pres, "acc"], writes=[("mixA", h, blk)])
                while conv_jobs:
                    conv_issue()
                P.emit(defer_cv=True)
        if debug:
            P.dma("sp", dbg["mixT"].rearrange("p (c t) -> p c t", c=8)[:, 0:4, :], actT[:], key="dbg")
            P.emit()
            P.dma("sp", dbg["mixA"].rearrange("p (c t) -> p c t", c=8), mixA[:], key="dbg")
            P.emit()

        with ExitStack() as sR:
            TR = lambda name, shape, dt: sR.enter_context(nc.sbuf_tensor("sb_" + name, list(shape), dt))
            x1 = TR("x1", [128, NT, D], F32)
            g_b = TR("g_b2", [128, D], F32)
            junk = TR("junk2", [128, D], F32)
            ssq = TR("ssq2", [128, NT], F32)
            rstd = TR("rstd2", [128, NT], F32)
            hns = [TR("hnb%d" % i, [128, D], BF16) for i in range(4)]

            with ExitStack() as sB:
                TB = lambda name, shape, dt: sB.enter_context(nc.sbuf_tensor("sb_" + name, list(shape), dt))
                wo = TB("wo", [128, 4, D], BF16)
                woA = TB("woA", [64, 8, D], BF16)
                ps = [sB.enter_context(nc.psum_tensor("psB%d" % i, [128, 512], F32)) for i in range(4)]
                P.dma("pool", wo[:], w_out_v[:, 0:4, :], writes=["wo"], key="wo")
                P.dma("pool", woA[:], w_out[512:1024, :].rearrange("(h d) n -> d h n", d=64), writes=["wo"], key="wk")
                for i in range(NT):
                    P.dma("sp", x1[:, i, :], xh[2048 + 128 * i:2048 + 128 * (i + 1), :], writes=[("x1", i)],
                          key=("x1", i % 4))
                pi = 0
                for i in range(NT):
                    for half in range(2):
                        pp = ps[pi % 4]
                        pres = ("psB", pi % 4)
                        pi += 1
                        for kc in range(4):
                            P.op("pe", lambda E, pp=pp, kc=kc, i=i, half=half: E.matmul(
                                pp[:], lhsT=actT[:, kc, 128 * i:128 * (i + 1)], rhs=wo[:, kc, 512 * half:512 * (half + 1)],
                                start=(kc == 0), stop=False), reads=["wo", "mixT"], writes=[pres])
                        for h in range(8):
                            P.op("pe", lambda E, pp=pp, h=h, i=i, half=half: E.matmul(
                                pp[:], lhsT=mixA[:, h, 128 * i:128 * (i + 1)], rhs=woA[:, h, 512 * half:512 * (half + 1)],
                                start=False, stop=(h == 7)), reads=["wo", "mixT"], writes=[pres])
                        P.op("dve", lambda E, pp=pp, i=i, half=half: E.tensor_tensor(
                            out=x1[:, i, 512 * half:512 * (half + 1)], in0=x1[:, i, 512 * half:512 * (half + 1)],
                            in1=pp[:], op=ALU.add), reads=[pres, ("x1", i)], writes=[("x1", i)])
                P.emit(defer_cv=True)
            if debug:
                P.dma("sp", dbg["x1"].rearrange("p (c t) -> p c t", c=NT), x1[:], key="dbg")
                P.emit()

            with ExitStack() as sC1:
                TC1 = lambda name, shape, dt: sC1.enter_context(nc.sbuf_tensor("sb_" + name, list(shape), dt))
                hn2T = TC1("hn2T", [128, 8, 2048], BF16)
                gates = TC1("gates", [128, NT, NE], F32)
                gates_bf = TC1("gates_bf", [128, NT, NE], BF16)
                gT = TC1("gT", [NE, NT, 128], BF16)
                bdn = TC1("bdn", [NE, D], BF16)
                wr = TC1("wr", [128, 8, NE], BF16)
                brb = TC1("brb", [128, NE], F32)
                ltri = TC1("ltri", [128, 128], BF16)
                ones128 = TC1("ones128", [128, 128], BF16)
                iota4 = TC1("iota4", [128, 4, NE], F32)
                ebase = TC1("ebase", [128, NE], F32)
                carry = TC1("carry", [128, NE], F32)
                lg = TC1("lg", [128, NE], F32)
                m8 = TC1("m8", [128, 8], F32)
                idx8 = TC1("idx8", [128, 8], mybir.dt.uint32)
                ef = TC1("ef", [128, 4], F32)
                negm = TC1("negm", [128, 1], F32)
                ex = TC1("ex", [128, NE], F32)
                msk = TC1("msk", [128, NE], F32)
                mskb = TC1("mskb", [128, NE], BF16)
                ssum = TC1("ssum", [128, 1], F32)
                posf = TC1("posf", [128, NE], F32)
                ovf = TC1("ovf", [128, NE], F32)
                oh4 = TC1("oh4", [128, 4, NE], F32)
                pr4 = TC1("pr4", [128, 4, NE], F32)
                slotf = TC1("slotf", [128, 4], F32)
                g4 = TC1("g4", [128, 4], F32)
                ps_tr = [sC1.enter_context(nc.psum_tensor("pstrC%d" % i, [128, 8, 128], BF16)) for i in range(2)]
                ps_l = sC1.enter_context(nc.psum_tensor("psl", [128, NE], F32))
                ps_pos = sC1.enter_context(nc.psum_tensor("pspos", [128, NE], F32))
                ps_cnt = sC1.enter_context(nc.psum_tensor("pscnt", [128, NE], F32))
                ps_g = sC1.enter_context(nc.psum_tensor("psgT", [NE, 128], BF16))
                ps_b = [sC1.enter_context(nc.psum_tensor("psbd%d" % i, [128, 512], F32)) for i in range(2)]
                P.dma("sp", g_b[:], g_ffn_in[:, :], writes=["gains"], key=P.ckey())
                P.dma("pool", wr[:], w_router_v, writes=["wr"], key="wr")
                P.dma("sp", brb[:], brouter_in[:, :], writes=["brb"], key=P.ckey())
                P.dma("pool", bdn[:], b_down[:, :], writes=["bdn"], key=P.ckey())
                P.dma("pool", ltri[:], ltri_in[:, :], writes=["ltri"], key=P.ckey())
                P.dma("sp", iota4[:], iota4_in.rearrange("p (k e) -> p k e", k=4), writes=["iota4"], key=P.ckey())
                P.dma("sp", ebase[:], ebase_in[:, :], writes=["ebase"], key=P.ckey())
                P.op("pool", lambda E: E.memset(ones128[:], 1.0), writes=["ones128"])
                P.op("pool", lambda E: E.memset(carry[:], 0.0), writes=["carry"])

                def route(i, hb, hres):
                    for kc in range(8):
                        P.op("pe", lambda E, kc=kc, i=i: E.matmul(
                            ps_l[:], lhsT=hn2T[:, kc, 128 * i:128 * (i + 1)], rhs=wr[:, kc, :],
                            start=(kc == 0), stop=(kc == 7)), reads=["wr", ("T", i)], writes=["psl"])
                    P.op("dve", lambda E: E.tensor_tensor(out=lg[:], in0=ps_l[:], in1=brb[:], op=ALU.add),
                         reads=["psl", "brb"], writes=["lg"])
                    P.op("dve", lambda E: E.max(out=m8[:], in_=lg[:]), reads=["lg"], writes=["m8"])
                    P.op("dve", lambda E: E.max_index(out=idx8[:], in_max=m8[:], in_values=lg[:]),
                         reads=["lg", "m8"], writes=["idx8"])
                    P.op("dve", lambda E: E.tensor_copy(out=ef[:], in_=idx8[:, 0:4]), reads=["idx8"], writes=["ef"])
                    P.op("dve", lambda E: E.tensor_scalar(out=negm[:], in0=m8[:, 0:1], scalar1=-1.0, scalar2=None,
                                                          op0=ALU.mult), reads=["m8"], writes=["negm"])
                    P.op("dve", lambda E: E.tensor_scalar(out=msk[:], in0=lg[:], scalar1=m8[:, 3:4], scalar2=None,
                                                          op0=ALU.is_ge), reads=["lg", "m8"], writes=["msk"])
                    P.op("dve", lambda E: E.tensor_copy(out=mskb[:], in_=msk[:]), reads=["msk"], writes=["mskb"])
                    P.op("act", lambda E: E.activation(out=ex[:], in_=lg[:], func=AF.Exp, bias=negm[:], scale=1.0),
                         reads=["lg", "negm"], writes=["ex"])
                    P.op("dve", lambda E: E.tensor_tensor(out=ex[:], in0=ex[:], in1=msk[:], op=ALU.mult),
                         reads=["ex", "msk"], writes=["ex"])
                    P.op("dve", lambda E: E.reduce_sum(out=ssum[:], in_=ex[:], axis=mybir.AxisListType.X),
                         reads=["ex"], writes=["ssum"])
                    P.op("dve", lambda E: E.reciprocal(out=ssum[:], in_=ssum[:]), reads=["ssum"], writes=["ssum"])
                    P.op("dve", lambda E, i=i: E.tensor_scalar(out=gates[:, i, :], in0=ex[:], scalar1=ssum[:, 0:1],
                                                               scalar2=None, op0=ALU.mult),
                         reads=["ex", "ssum"], writes=[("gates", i)])
                    P.op("dve", lambda E, i=i: E.tensor_copy(out=gates_bf[:, i, :], in_=gates[:, i, :]),
                         reads=[("gates", i)], writes=[("gates_bf", i)])
                    P.op("pe", lambda E, i=i: E.transpose(ps_g[:], gates_bf[:, i, :], ident[:]),
                         reads=[("gates_bf", i), "ident"], writes=["psgT"])
                    P.op("act", lambda E, i=i: E.copy(out=gT[:, i, :], in_=ps_g[:]), reads=["psgT"],
                         writes=[("gT", i)])
                    P.op("pe", lambda E: E.matmul(ps_pos[:], lhsT=ltri[:], rhs=mskb[:], start=True, stop=True),
                         reads=["ltri", "mskb"], writes=["pspos"])
                    P.op("pe", lambda E: E.matmul(ps_cnt[:], lhsT=ones128[:], rhs=mskb[:], start=True, stop=True),
                         reads=["ones128", "mskb"], writes=["pscnt"])
                    P.op("dve", lambda E: E.tensor_tensor(out=posf[:], in0=ps_pos[:], in1=carry[:], op=ALU.add),
                         reads=["pspos", "carry"], writes=["posf"])
                    P.op("dve", lambda E: E.tensor_tensor(out=carry[:], in0=ps_cnt[:], in1=carry[:], op=ALU.add),
                         reads=["pscnt", "carry", "posf"], writes=["carry"])
                    P.op("dve", lambda E: E.tensor_scalar(out=ovf[:], in0=posf[:], scalar1=float(CAP), scalar2=BIGSLOT,
                                                          op0=ALU.is_ge, op1=ALU.mult), reads=["posf"], writes=["ovf"])
                    P.op("dve", lambda E: E.tensor_tensor(out=posf[:], in0=posf[:], in1=ebase[:], op=ALU.add),
                         reads=["posf", "ebase", "ovf"], writes=["posf"])
                    P.op("dve", lambda E: E.tensor_tensor(out=posf[:], in0=posf[:], in1=ovf[:], op=ALU.add),
                         reads=["posf", "ovf"], writes=["posf"])
                    P.op("dve", lambda E: E.tensor_tensor(
                        out=oh4[:], in0=iota4[:], in1=ef[:].unsqueeze(2).to_broadcast([128, 4, NE]), op=ALU.is_equal),
                         reads=["iota4", "ef"], writes=["oh4"])
                    P.op("dve", lambda E: E.tensor_tensor(
                        out=pr4[:], in0=oh4[:], in1=posf[:].unsqueeze(1).to_broadcast([128, 4, NE]), op=ALU.mult),
                         reads=["oh4", "posf"], writes=["pr4"])
                    P.op("dve", lambda E: E.reduce_sum(out=slotf[:], in_=pr4[:], axis=mybir.AxisListType.X),
                         reads=["pr4"], writes=["slotf"])
                    P.op("dve", lambda E, i=i: E.tensor_copy(out=slot_i32[:, i, :], in_=slotf[:]),
                         reads=["slotf"], writes=[("slot", i)])
                    P.op("dve", lambda E, i=i: E.tensor_tensor(
                        out=pr4[:], in0=oh4[:], in1=gates[:, i, :].unsqueeze(1).to_broadcast([128, 4, NE]), op=ALU.mult),
                         reads=["oh4", ("gates", i), "pr4"], writes=["pr4"])
                    P.op("dve", lambda E: E.reduce_sum(out=g4[:], in_=pr4[:], axis=mybir.AxisListType.X),
                         reads=["pr4"], writes=["g4"])
                    P.op("dve", lambda E, i=i: E.tensor_scalar(out=g4n[:, i, :], in0=g4[:], scalar1=-1.0, scalar2=None,
                                                               op0=ALU.mult), reads=["g4"], writes=[("g4n", i)])
                    for k in range(4):
                        P.op("pool", lambda E, i=i, k=k, hb=hb: E.indirect_dma_start(
                            out=xs[:, :], out_offset=bass.IndirectOffsetOnAxis(ap=slot_i32[:, i, k:k + 1], axis=0),
                            in_=hb[:], in_offset=None, bounds_check=P.bc_reg(E, NE * CAP - 1), oob_is_err=False),
                             reads=[hres, ("slot", i)], writes=[], dma_key=("scat", i % 2, k))

                rms_tiles(lambda i: (x1[:, i, :], ("x1", i)), NT, g_b, hn2T, 0, "C1", None,
                          (junk, ssq, rstd), hns, ps_tr, post=route)
                for i in range(NT):
                    for half in range(2):
                        pp = ps_b[(2 * i + half) % 2]
                        pres = ("psbd", (2 * i + half) % 2)
                        P.op("pe", lambda E, pp=pp, i=i, half=half: E.matmul(
                            pp[:], lhsT=gT[:, i, :], rhs=bdn[:, 512 * half:512 * (half + 1)], start=True, stop=True),
                             reads=[("gT", i), "bdn"], writes=[pres])
                        P.op("dve", lambda E, pp=pp, i=i, half=half: E.tensor_tensor(
                            out=x1[:, i, 512 * half:512 * (half + 1)], in0=x1[:, i, 512 * half:512 * (half + 1)],
                            in1=pp[:], op=ALU.add), reads=[pres, ("x1", i)], writes=[("x1", i)])
                    P.dma("sp", x1_spill[:, D * i:D * (i + 1)], x1[:, i, :], reads=[("x1", i)], key=("spill", i % 4))
                P.emit()
                if debug:
                    P.dma("sp", dbg["gates"].rearrange("p (c t) -> p c t", c=NT), gates[:], key="dbg")
                    P.emit()

        sAB.close()
        with ExitStack() as sC2:
            TC2 = lambda name, shape, dt: sC2.enter_context(nc.sbuf_tensor("sb_" + name, list(shape), dt))
            NRING = 8
            ring = [TC2("ring%d" % i, [128, 8, 512], BF16) for i in range(NRING)]
            bgu = TC2("bgu", [128, NE, 16], F32)
            xes = [TC2("xe%d" % i, [128, CAP // 128, D], BF16) for i in range(2)]
            xeTs = [TC2("xeT%d" % i, [128, 8, CAP], BF16) for i in range(2)]
            act_es = [TC2("act_e%d" % i, [128, 8, CAP], BF16) for i in range(2)]
            rs = [TC2("r%d" % i, [128, CAP], F32) for i in range(2)]
            sgs = [TC2("sg%d" % i, [128, CAP], F32) for i in range(2)]
            ucs = [TC2("uc%d" % i, [128, CAP], F32) for i in range(2)]
            yts = [TC2("yt%d" % i, [128, D], F32) for i in range(3)]
            ps_gu = [sC2.enter_context(nc.psum_tensor("psgu%d" % i, [128, 512], F32)) for i in range(4)]
            ps_d = [sC2.enter_context(nc.psum_tensor("psd%d" % i, [128, 512], F32)) for i in range(2)]
            ps_tr = [sC2.enter_context(nc.psum_tensor("pstrE%d" % i, [128, 8, 128], BF16)) for i in range(2)]
            P.dma("sp", bgu[:], bgu_in.rearrange("p (e c) -> p e c", e=NE), writes=["bgu"], key=P.ckey())
            P.op("dve", lambda E: E.tensor_scalar(out=bgu[:, :, 0:8], in0=bgu[:, :, 0:8], scalar1=-1.0, scalar2=7.0,
                                                  op0=ALU.mult, op1=ALU.add), reads=["bgu"], writes=["bgu"])
            P.op("dve", lambda E: E.tensor_scalar(out=bgu[:, :, 8:16], in0=bgu[:, :, 8:16], scalar1=1.0, scalar2=None,
                                                  op0=ALU.add), reads=["bgu"], writes=["bgu"])
            pieces = []
            for e in range(NE):
                for j in range(2):
                    pieces.append((e, "g", j))
                    pieces.append((e, "u", j))
                for half in range(2):
                    pieces.append((e, "dn", half))

            def piece_dma(n):
                e, kind, j = pieces[n]
                slot = n % NRING
                wg_e = wgu_bf[e] if e < NCONV else w_gu[e]
                wd_e = wdn_bf[e] if e < NCONV else w_down[e]
                for part in range(2):
                    if kind in ("g", "u"):
                        c0 = (0 if kind == "g" else 1024) + 512 * j
                        src = wg_e.rearrange("(kc p) f -> p kc f", p=128)[:, 4 * part:4 * (part + 1), c0:c0 + 512]
                    else:
                        src = wd_e.rearrange("(kc p) n -> p kc n", p=128)[
                            :, 4 * part:4 * (part + 1), 512 * j:512 * (j + 1)]
                    dst = ring[slot][:, 4 * part:4 * (part + 1), :]
                    P.dma("pool", dst, src, writes=[("ring", slot, part)], key=("ring", slot, part))

            def xe_load(e):
                b = e % 2
                P.dma("sp", xes[b][:], xs[e * CAP:(e + 1) * CAP, :].rearrange("(j p) d -> p j d", p=128),
                      writes=[("xe", b)], key=("xe", b))

            LOOK = NRING - 2
            for n in range(min(LOOK, len(pieces))):
                piece_dma(n)
            xe_load(0)
            gi = 0
            di = 0
            ei = 0
            ti = 0
            yi = 0
            for n, (e, kind, j) in enumerate(pieces):
                if n + LOOK < len(pieces):
                    piece_dma(n + LOOK)
                slot = n % NRING
                rg = ring[slot]
                b = e % 2
                xeT, act_e = xeTs[b], act_es[b]
                if kind == "g" and j == 0:
                    if e + 1 < NE:
                        xe_load(e + 1)
                    for jj in range(CAP // 128):
                        pt, ptres = ps_tr[ti % 2], ("pstrE", ti % 2)
                        ti += 1
                        for kc in range(8):
                            P.op("pe", lambda E, pt=pt, kc=kc, jj=jj, b=b: E.transpose(
                                pt[:, kc, :], xes[b][:, jj, 128 * kc:128 * (kc + 1)], ident[:]),
                                 reads=[("xe", b), "ident"], writes=[ptres])
                        P.op("act", lambda E, pt=pt, jj=jj, xeT=xeT: E.copy(out=xeT[:, :, 128 * jj:128 * (jj + 1)], in_=pt[:]),
                             reads=[ptres], writes=[("xeT", b)])
                if kind == "g":
                    continue
                if kind == "u":
                    slot_g = (n - 1) % NRING
                    rgg = ring[slot_g]
                    for c in range(4):
                        fc = 4 * j + c
                        pg, pgres = ps_gu[gi % 4], ("psgu", gi % 4)
                        gi += 1
                        pu, pures = ps_gu[gi % 4], ("psgu", gi % 4)
                        gi += 1
                        for (pp, pres, wt, wslot) in ((pg, pgres, rgg, slot_g), (pu, pures, rg, slot)):
                            for kc in range(8):
                                P.op("pe", lambda E, pp=pp, wt=wt, kc=kc, c=c, xeT=xeT: E.matmul(
                                    pp[:, 0:CAP], lhsT=wt[:, kc, 128 * c:128 * (c + 1)],
                                    rhs=xeT[:, kc, :], start=(kc == 0), stop=(kc == 7)),
                                     reads=[("ring", wslot, kc // 4), ("xeT", b)], writes=[pres])
                        k2 = ei % 2
                        ei += 1
                        r, sg, uc = rs[k2], sgs[k2], ucs[k2]
                        P.op("act", lambda E, pg=pg, r=r, e=e, fc=fc: E.activation(
                            out=r[:], in_=pg[:, 0:CAP], func=AF.Relu, bias=bgu[:, e, fc:fc + 1], scale=-1.0),
                             reads=[pgres, "bgu"], writes=[("r", k2)])
                        P.op("act", lambda E, r=r, sg=sg: E.activation(out=sg[:], in_=r[:], func=AF.Sigmoid, bias=c119[:],
                                                                       scale=-1.702),
                             reads=[("r", k2), "c119"], writes=[("sg", k2)])
                        P.op("dve", lambda E, r=r, sg=sg: E.scalar_tensor_tensor(
                            out=r[:], in0=r[:], scalar=7.0, in1=sg[:], op0=ALU.subtract, op1=ALU.mult),
                             reads=[("r", k2), ("sg", k2)], writes=[("r", k2)])
                        P.op("dve", lambda E, pu=pu, uc=uc, e=e, fc=fc: E.tensor_scalar(
                            out=uc[:], in0=pu[:, 0:CAP], scalar1=bgu[:, e, 8 + fc:9 + fc], scalar2=8.0, op0=ALU.add,
                            op1=ALU.min), reads=[pures, "bgu"], writes=[("uc", k2)])
                        P.op("dve", lambda E, r=r, uc=uc, fc=fc, act_e=act_e: E.scalar_tensor_tensor(
                            out=act_e[:, fc, :], in0=uc[:], scalar=-6.0, in1=r[:], op0=ALU.max, op1=ALU.mult),
                             reads=[("r", k2), ("uc", k2)], writes=[("act_e", b, fc)])
                else:
                    half = j
                    for jj in range(CAP // 128):
                        pp, pres = ps_d[di % 2], ("psd", di % 2)
                        di += 1
                        for fc in range(8):
                            P.op("pe", lambda E, pp=pp, rg=rg, fc=fc, jj=jj, act_e=act_e: E.matmul(
                                pp[:], lhsT=act_e[:, fc, 128 * jj:128 * (jj + 1)], rhs=rg[:, fc, :],
                                start=(fc == 0), stop=(fc == 7)),
                                 reads=[("ring", slot, fc // 4), ("act_e", b, fc)], writes=[pres])
                        yt = yts[jj]
                        P.op("act", lambda E, pp=pp, yt=yt, half=half: E.copy(out=yt[:, 512 * half:512 * (half + 1)],
                                                                              in_=pp[:]),
                             reads=[pres], writes=[("yt", jj, half)])
                        if half == 1:
                            P.dma("sp", ys[e * CAP + 128 * jj:e * CAP + 128 * (jj + 1), :], yt[:],
                                  reads=[("yt", jj, 0), ("yt", jj, 1)], key=("ys", jj))
            P.emit()

        with ExitStack() as sR:
            TR = lambda name, shape, dt: sR.enter_context(nc.sbuf_tensor("sb_" + name, list(shape), dt))
            x1 = TR("x1b", [128, NT, D], F32)
            g_b = TR("g_b3", [128, D], F32)
            junk = TR("junk3", [128, D], F32)
            ssq = TR("ssq3", [128, NT], F32)
            rstd = TR("rstd3", [128, NT], F32)
            hns = [TR("hnc%d" % i, [128, D], BF16) for i in range(4)]
            with ExitStack() as sD:
                TD = lambda name, shape, dt: sD.enter_context(nc.sbuf_tensor("sb_" + name, list(shape), dt))
                hn3T = TD("hn3T", [128, 8, 2048], BF16)
                wpg = TD("wpg", [128, 8, D], BF16)
                wpp = TD("wpp", [128, 2, D], BF16)
                gfin = TD("gfin", [128, D], F32)
                pts = [TD("pt%d" % i, [128, 256], BF16) for i in range(NT)]
                pTs = [TD("pT%d" % i, [128, 2, 128], BF16) for i in range(2)]
                sgs = [TD("sgD%d" % i, [128, 512], F32) for i in range(2)]
                outs = [TD("outD%d" % i, [128, D], F32) for i in range(2)]
                ssq2 = TD("ssqD", [128, NT], F32)
                rstd2 = TD("rstdD", [128, NT], F32)
                ps_tr = [sD.enter_context(nc.psum_tensor("pstrD%d" % i, [128, 8, 128], BF16)) for i in range(3)]
                ps_pt = sD.enter_context(nc.psum_tensor("pspt", [128, 2, 128], BF16))
                ps_g = [sD.enter_context(nc.psum_tensor("psDg%d" % i, [128, 512], F32)) for i in range(2)]
                ps_p = [sD.enter_context(nc.psum_tensor("psDp%d" % i, [128, 512], F32)) for i in range(2)]
                P.dma("sp", g_b[:], g_ple_in[:, :], writes=["gains"], key=P.ckey())
                P.dma("sp", gfin[:], g_fin_in[:, :], writes=["gfin"], key=P.ckey())
                P.dma("pool", wpg[:], w_pg_v, writes=["wpg"], key="wpg")
                P.dma("pool", wpp[:], w_pp_v, writes=["wpp"], key="wpp")
                for i in range(NT):
                    P.dma("pool", pts[i][:], p_in[128 * i:128 * (i + 1), :], writes=[("pt", i)], key=("pt", i % 2))
                yks = [TD("yk%d" % i, [128, D], F32) for i in range(8)]
                for i in range(8):
                    P.op("pool", lambda E, i=i: E.memset(yks[i][:], 0.0), writes=[("yk", i)])
                for i in range(NT):
                    P.dma("sp", x1[:, i, :], x1_spill[:, D * i:D * (i + 1)], writes=[("x1", i)], key=("x1", i % 4))

                def combine_tile(i):
                    for k in range(4):
                        q = (4 * i + k) % 8
                        P.op("pool", lambda E, i=i, k=k, q=q: E.indirect_dma_start(
                            out=yks[q][:], out_offset=None, in_=ys[:, :],
                            in_offset=bass.IndirectOffsetOnAxis(ap=slot_i32[:, i, k:k + 1], axis=0),
                            bounds_check=P.bc_reg(E, NE * CAP - 1), oob_is_err=False),
                             reads=[], writes=[("yk", q)], dma_key=("yk", q))
                        P.op("dve", lambda E, i=i, k=k, q=q: E.scalar_tensor_tensor(
                            out=x1[:, i, :], in0=yks[q][:], scalar=g4n[:, i, k:k + 1], in1=x1[:, i, :],
                            op0=ALU.mult, op1=ALU.add), reads=[("yk", q), ("x1", i)], writes=[("x1", i)])

                def pre_combine(i):
                    if debug:
                        return
                    if i == 0:
                        combine_tile(0)
                    if i + 1 < NT:
                        combine_tile(i + 1)

                if debug:
                    for i in range(NT):
                        combine_tile(i)
                if debug:
                    P.emit()
                if debug:
                    P.dma("sp", dbg["x2"].rearrange("p (c t) -> p c t", c=NT), x1[:], key="dbg")
                    P.emit()

                pi_box = [0]

                def ple_tile(i, hb_unused, hres_unused):
                    b = i % 2
                    for c in range(2):
                        P.op("pe", lambda E, i=i, c=c: E.transpose(ps_pt[:, c, :], pts[i][:, 128 * c:128 * (c + 1)], ident[:]),
                             reads=[("pt", i), "ident"], writes=["pspt"])
                    P.op("act", lambda E, b=b: E.copy(out=pTs[b][:], in_=ps_pt[:]), reads=["pspt"], writes=[("pT", b)])
                    for half in range(2):
                        k2 = pi_box[0] % 2
                        pi_box[0] += 1
                        pg, pgres = ps_g[k2], ("psDg", k2)
                        pq, pqres = ps_p[k2], ("psDp", k2)
                        for kc in range(8):
                            P.op("pe", lambda E, pg=pg, kc=kc, i=i, half=half: E.matmul(
                                pg[:], lhsT=hn3T[:, kc, 128 * i:128 * (i + 1)], rhs=wpg[:, kc, 512 * half:512 * (half + 1)],
                                start=(kc == 0), stop=(kc == 7)), reads=["wpg", ("T", i)], writes=[pgres])
                        for c in range(2):
                            P.op("pe", lambda E, pq=pq, c=c, b=b, half=half: E.matmul(
                                pq[:], lhsT=pTs[b][:, c, :], rhs=wpp[:, c, 512 * half:512 * (half + 1)],
                                start=(c == 0), stop=(c == 1)), reads=["wpp", ("pT", b)], writes=[pqres])
                        sg = sgs[k2]
                        P.op("act", lambda E, pg=pg, sg=sg: E.activation(out=sg[:], in_=pg[:], func=AF.Sigmoid),
                             reads=[pgres], writes=[("sgD", k2)])
                        P.op("dve", lambda E, pq=pq, sg=sg: E.tensor_tensor(out=sg[:], in0=sg[:], in1=pq[:], op=ALU.mult),
                             reads=[pqres, ("sgD", k2)], writes=[("sgD", k2)])
                        P.op("dve", lambda E, sg=sg, i=i, half=half: E.tensor_tensor(
                            out=x1[:, i, 512 * half:512 * (half + 1)], in0=x1[:, i, 512 * half:512 * (half + 1)],
                            in1=sg[:], op=ALU.add), reads=[("sgD", k2), ("x1", i)], writes=[("x1", i)])
                    ob = outs[b]
                    P.op("act", lambda E, i=i: E.activation(out=junk[:], in_=x1[:, i, :], func=AF.Square,
                                                            accum_out=ssq2[:, i:i + 1]),
                         reads=[("x1", i)], writes=["Djunk2", ("ssqD", i)])
                    P.op("act", lambda E, i=i: E.activation(out=rstd2[:, i:i + 1], in_=ssq2[:, i:i + 1], func=AF.Sqrt,
                                                            bias=epsb[:], scale=1.0 / D),
                         reads=[("ssqD", i), "epsb"], writes=[("rstdD", i)])
                    P.op("dve", lambda E, i=i: E.reciprocal(out=rstd2[:, i:i + 1], in_=rstd2[:, i:i + 1]),
                         reads=[("rstdD", i)], writes=[("rstdD", i)])
                    P.op("dve", lambda E, i=i, ob=ob: E.scalar_tensor_tensor(
                        out=ob[:], in0=x1[:, i, :], scalar=rstd2[:, i:i + 1], in1=gfin[:], op0=ALU.mult, op1=ALU.mult),
                         reads=[("x1", i), ("rstdD", i), "gfin"], writes=[("outD", b)])
                    P.dma("sp", y[128 * i:128 * (i + 1), :], ob[:], reads=[("outD", b)], key=("outD", b))
                def post_skewed(i, hb, hres):
                    if i >= 1:
                        ple_tile(i - 1, None, None)

                rms_tiles(lambda i: (x1[:, i, :], ("x1", i)), NT, g_b, hn3T, 0, "D", None,
                          (junk, ssq, rstd), hns, ps_tr, pre=pre_combine, post=post_skewed)
                ple_tile(NT - 1, None, None)
                P.emit()
    return nc


def _t5_bucket_np(dist):
    n = np.maximum(dist, 1).astype(np.float32)
    large = 16 + (np.log(n / 16) / math.log(2048 / 16) * 16).astype(np.int32)
    large = np.minimum(large, 31)
    return np.where(dist < 16, dist, large)


def _bias_tables(rel_bias, first_half):
    ki = np.arange(128)[:, None]
    qi = np.arange(128)[None, :]
    btab = np.full((128, 3, 8, 256), NEG, np.float32)
    for br, (window, dil) in enumerate(BRANCHES):
        relp = 128 + qi - ki
        idxp = _t5_bucket_np(np.maximum(relp, 0) * dil)
        relc = qi - ki
        idxc = _t5_bucket_np(np.maximum(relc, 0) * dil)
        for h in range(8):
            bp = rel_bias[idxp, h]
            bc = rel_bias[idxc, h]
            btab[:, br, h, 128:256] = np.where((relp >= 0) & (relp <= 128), bp, NEG)
            btab[:, br, h, 0:128] = np.where((relc >= 0) & (relc <= 128), bc, NEG)
    bhalo = btab[:, :, :, 128:256].copy()
    if first_half:
        bhalo[:] = NEG
    return btab.reshape(128, -1), np.ascontiguousarray(bhalo).reshape(128, -1)


_NC_CACHE = {}


def _prepare_in_maps(inputs):
    f = lambda a: np.ascontiguousarray(np.asarray(a, dtype=np.float32))
    x = f(inputs["x"])
    p = f(inputs["p"])[0]
    rb = f(inputs["rel_bias"])
    bc = lambda v: np.ascontiguousarray(np.broadcast_to(f(v).reshape(1, -1), (128, f(v).size)))
    shared = {
        "ident": np.eye(128, dtype=np.float32),
        "ltri": np.triu(np.ones((128, 128), np.float32), 1),
        "iota4": np.ascontiguousarray(np.broadcast_to(np.tile(np.arange(NE, dtype=np.float32), 4)[None, :], (128, 4 * NE))),
        "ebase": np.ascontiguousarray(np.broadcast_to((np.arange(NE, dtype=np.float32) * CAP)[None, :], (128, NE))),
        "g_mix_b": bc(inputs["g_mix"][0]),
        "g_ffn_b": bc(inputs["g_ffn"][0]),
        "g_ple_b": bc(inputs["g_ple"][0]),
        "g_fin_b": bc(inputs["g_final"]),
        "w_in": f(inputs["w_in"])[0],
        "w_pool": f(inputs["w_pool"])[0],
        "pscale": np.ascontiguousarray(f(inputs["pool_scale"])[0].reshape(4, 128).T),
        "w_out": f(inputs["w_out"])[0],
        "w_router": f(inputs["w_router"])[0],
        "b_router_b": bc(inputs["b_router"][0]),
        "w_gate_up": f(inputs["w_gate_up"])[0],
        "bgu": np.ascontiguousarray(f(inputs["b_gate_up"])[0].reshape(NE, 16, 128).transpose(2, 0, 1)).reshape(128, -1),
        "w_down": f(inputs["w_down"])[0],
        "b_down": f(inputs["b_down"])[0],
        "w_ple_gate": f(inputs["w_ple_gate"])[0],
        "w_ple_proj": f(inputs["w_ple_proj"])[0],
    }
    tabs = {fh: _bias_tables(rb, fh) for fh in (True, False)}
    in_maps = []
    for c in range(NCORES):
        b, half = c // 2, c % 2
        base = half * S_OWN
        xh = np.zeros((4096, D), np.float32)
        if half == 1:
            xh[0:2048] = x[b, 0:2048]
        xh[2048:4096] = x[b, base:base + S_OWN]
        pos = base + np.arange(16)
        invcnt = np.stack([1.0 / np.minimum(pos + 1, w) for w in (2, 4, 8, 16)]).astype(np.float32)
        m = dict(shared)
        m["xh"] = xh
        m["p"] = np.ascontiguousarray(p[b, base:base + S_OWN])
        m["btab"], m["bhalo"] = tabs[half == 0]
        m["invcnt"] = np.ascontiguousarray(np.broadcast_to(invcnt.reshape(1, -1), (128, 64)))
        in_maps.append(m)
    return in_maps


def kernel(**inputs):
    in_maps = _prepare_in_maps(inputs)
    if "nc" not in _NC_CACHE:
        _NC_CACHE["nc"] = build_program(debug=False)
    nc = _NC_CACHE["nc"]
    res = run_bass_kernel_spmd(nc, in_maps, core_ids=list(range(NCORES)))
    out = np.zeros((4, 4096, D), np.float32)
    for c in range(NCORES):
        b, half = c // 2, c % 2
        out[b, half * S_OWN:(half + 1) * S_OWN] = np.asarray(res.results[c]["y"], dtype=np.float32)
    return out
```
